# Optimizing a Trainium2 kernel written in Bass

```python
import math
import jax
import jax.numpy as jnp
from jax import lax
import numpy as np

D_MODEL = 1024
BATCH = 4
SEQ = 8192
DEPTH = 4

GRID_W = 64
CTX_LEN = 256
N_MOD = 6
EPS = 1e-6
SSD_HEADS = 16
SSD_HEAD_DIM = 64
SSD_INNER = SSD_HEADS * SSD_HEAD_DIM
SSD_GROUPS = 2
SSD_STATE = 128
SSD_BC = SSD_GROUPS * SSD_STATE
SSD_CONV = 5
SSD_CONV_DIM = SSD_INNER + 2 * SSD_BC
SSD_CHUNK = 128
MLA_HEADS = 8
MLA_Q_RANK = 384
MLA_KV_RANK = 256
MLA_NOPE = 128
MLA_ROPE = 64
MLA_V = 128
MLA_SCALE = (MLA_NOPE + MLA_ROPE) ** -0.5
ATTN_BLOCK = 128
ROPE_BASE = 10000.0
HYB_SPLITS = (SSD_INNER, SSD_CONV_DIM, SSD_HEADS, MLA_Q_RANK, MLA_KV_RANK, MLA_ROPE)
HYB_IN = SSD_INNER + SSD_CONV_DIM + SSD_HEADS + MLA_Q_RANK + MLA_KV_RANK + MLA_ROPE
HYB_CAT = SSD_INNER + MLA_HEADS * MLA_V
GM_CHUNK = 128
GM_INNER = 2 * D_MODEL
GM_GROUPS = 8
PEER_HEADS = 8
PEER_NKEYS = 128
PEER_EXPERTS = PEER_NKEYS * PEER_NKEYS
PEER_DKEY = 256
PEER_TOPK = 16
PEER_BLOCK = 128

kernel_name = 'hybrid_ssd_mla_gmlp_peer_dit'


def _rmsnorm(x, g):
    xf = x.astype(jnp.float32)
    y = xf * lax.rsqrt(jnp.mean(xf * xf, axis=-1, keepdims=True) + EPS)
    return y.astype(x.dtype) * g


def _layernorm(x, g, b):
    xf = x.astype(jnp.float32)
    mu = jnp.mean(xf, axis=-1, keepdims=True)
    var = jnp.mean(jnp.square(xf - mu), axis=-1, keepdims=True)
    return ((xf - mu) * lax.rsqrt(var + EPS)).astype(x.dtype) * g + b


def _modulate(h, shift, scale):
    return h * (1 + scale) + shift


def _split_cols(p, sizes):
    out, start = [], 0
    for s in sizes:
        out.append(p[..., start:start + s])
        start += s
    return out


def _axial_rope(n_lat):
    rows = n_lat // GRID_W
    row = jnp.broadcast_to(jnp.arange(rows, dtype=jnp.float32)[:, None], (rows, GRID_W)).reshape(-1)
    col = jnp.broadcast_to(jnp.arange(GRID_W, dtype=jnp.float32)[None, :], (rows, GRID_W)).reshape(-1)
    n_freq = MLA_ROPE // 4
    inv = ROPE_BASE ** (-jnp.arange(n_freq, dtype=jnp.float32) / n_freq)
    ang = jnp.concatenate([row[:, None] * inv, col[:, None] * inv], axis=-1)
    return jnp.cos(ang), jnp.sin(ang)


def _apply_rope(x, cos, sin):
    half = x.shape[-1] // 2
    x1, x2 = x[..., :half], x[..., half:]
    cos = cos.astype(x.dtype)
    sin = sin.astype(x.dtype)
    return jnp.concatenate([x1 * cos - x2 * sin, x1 * sin + x2 * cos], axis=-1)


def _dwconv_centred(x, w, b):
    k = w.shape[-1]
    kern = jnp.transpose(w)[:, None, :].astype(x.dtype)
    y = lax.conv_general_dilated(x, kern, window_strides=(1,), padding=[(k // 2, k // 2)],
                                 dimension_numbers=('NWC', 'WIO', 'NWC'),
                                 feature_group_count=x.shape[-1])
    return y + b


def _ssd_scan(xs, dt, a, bm, cm, h0):
    b, l = xs.shape[:2]
    nc, q = l // SSD_CHUNK, SSD_CHUNK
    g, e = SSD_GROUPS, SSD_HEADS // SSD_GROUPS
    dt = dt.astype(jnp.float32)
    da = (dt * a).reshape(b, nc, q, g, e)
    xd = (xs * dt[..., None]).reshape(b, nc, q, g, e, SSD_HEAD_DIM)
    bm = bm.reshape(b, nc, q, g, SSD_STATE)
    cm = cm.reshape(b, nc, q, g, SSD_STATE)
    cum = jnp.cumsum(da, axis=2)
    scan_order = jnp.tril(jnp.ones((q, q), bool))[None, None, :, :, None, None]
    seg = cum[:, :, :, None] - cum[:, :, None, :]
    decay_in = jnp.exp(jnp.where(scan_order, seg, -jnp.inf))
    cb = jnp.einsum('bctgn,bcsgn->bctsg', cm, bm)
    y_diag = jnp.einsum('bctsge,bcsgep->bctgep', cb[..., None] * decay_in, xd)
    to_end = jnp.exp(cum[:, :, -1:] - cum)
    states = jnp.einsum('bcsgn,bcsgep->bcgepn', bm, xd * to_end[..., None])
    chunk_decay = jnp.exp(cum[:, :, -1])

    def step(h, inp):
        st, dec = inp
        return h * dec[..., None, None] + st, h

    h_fin, h_in = lax.scan(step, h0, (jnp.moveaxis(states, 1, 0), jnp.moveaxis(chunk_decay, 1, 0)))
    h_in = jnp.moveaxis(h_in, 0, 1)
    y_off = jnp.einsum('bctgn,bcgepn->bctgep', cm, h_in) * jnp.exp(cum)[..., None]
    return (y_diag + y_off).reshape(b, l, SSD_HEADS, SSD_HEAD_DIM), h_fin


def _ssd_inputs(xbc_raw, conv_w, conv_b):
    xbc = jax.nn.silu(_dwconv_centred(xbc_raw, conv_w, conv_b))
    b, l, _ = xbc.shape
    xs = xbc[..., :SSD_INNER].reshape(b, l, SSD_HEADS, SSD_HEAD_DIM)
    bm = xbc[..., SSD_INNER:SSD_INNER + SSD_BC].reshape(b, l, SSD_GROUPS, SSD_STATE)
    cm = xbc[..., SSD_INNER + SSD_BC:].reshape(b, l, SSD_GROUPS, SSD_STATE)
    return xs, bm, cm


def _ssd_bidir(ctx_in, lat_in, a_log, dt_bias):
    b = lat_in[0].shape[0]
    ys_c, ys_l = [], []
    for d in range(2):
        a = -jnp.exp(a_log[d].astype(jnp.float32))
        c_in, l_in = ctx_in, lat_in
        if d == 1:
            c_in = tuple(t[:, ::-1] for t in ctx_in)
            l_in = tuple(t[:, ::-1] for t in lat_in)
        h0 = jnp.zeros((b, SSD_GROUPS, SSD_HEADS // SSD_GROUPS, SSD_HEAD_DIM, SSD_STATE), jnp.float32)
        yc, hc = _ssd_scan(c_in[0], jax.nn.softplus(c_in[3] + dt_bias[d]), a, c_in[1], c_in[2], h0)
        yl, _ = _ssd_scan(l_in[0], jax.nn.softplus(l_in[3] + dt_bias[d]), a, l_in[1], l_in[2], hc)
        if d == 1:
            yc, yl = yc[:, ::-1], yl[:, ::-1]
        ys_c.append(yc)
        ys_l.append(yl)
    return ys_c[0] + ys_c[1], ys_l[0] + ys_l[1]


def _ssd_out(y, xs, z, d_skip, norm_g):
    b, l = z.shape[:2]
    y = (y + d_skip[:, None] * xs).reshape(b, l, SSD_INNER).astype(z.dtype) * jax.nn.silu(z)
    y = _rmsnorm(y.reshape(b, l, SSD_GROUPS, SSD_INNER // SSD_GROUPS), norm_g.reshape(SSD_GROUPS, -1))
    return y.reshape(b, l, SSD_INNER)


def _mla_project(cq, ckv, kr, q_norm_g, w_qb, kv_norm_g, w_kvb, rope):
    b, l, _ = cq.shape
    q = (_rmsnorm(cq, q_norm_g) @ w_qb).reshape(b, l, MLA_HEADS, MLA_NOPE + MLA_ROPE)
    kv = (_rmsnorm(ckv, kv_norm_g) @ w_kvb).reshape(b, l, MLA_HEADS, MLA_NOPE + MLA_V)
    q_nope, q_rope = q[..., :MLA_NOPE], q[..., MLA_NOPE:]
    k_nope, v = kv[..., :MLA_NOPE], kv[..., MLA_NOPE:]
    if rope is not None:
        cos, sin = rope
        q_rope = _apply_rope(q_rope, cos[:, None, :], sin[:, None, :])
        kr = _apply_rope(kr, cos, sin)
    return q_nope, q_rope, k_nope, kr, v


def _attend(qn, qr, kn, kr, v):
    s = jnp.einsum('bqhd,bkhd->bhqk', qn, kn) + jnp.einsum('bqhr,bkr->bhqk', qr, kr)
    p = jax.nn.softmax(s.astype(jnp.float32) * MLA_SCALE, axis=-1).astype(v.dtype)
    return jnp.einsum('bhqk,bkhd->bqhd', p, v)


def _latent_attention(qn, qr, kn, kr, v):
    b, l = qn.shape[:2]
    nb = l // ATTN_BLOCK
    blocks = lambda t: jnp.moveaxis(t.reshape(b, nb, ATTN_BLOCK, *t.shape[2:]), 1, 0)
    out = lax.map(lambda qs: _attend(qs[0], qs[1], kn, kr, v), (blocks(qn), blocks(qr)))
    return jnp.moveaxis(out, 0, 1).reshape(b, l, MLA_HEADS * MLA_V)


def _hybrid_mix(h_c, h_l, rope, w_in, conv_w, conv_b, a_log, dt_bias, d_skip, ssd_norm_g,
                q_norm_g, w_qb, kv_norm_g, w_kvb, w_out, ctx_out):
    z_c, xbc_c, dt_c, cq_c, ckv_c, kr_c = _split_cols(h_c @ w_in, HYB_SPLITS)
    z_l, xbc_l, dt_l, cq_l, ckv_l, kr_l = _split_cols(h_l @ w_in, HYB_SPLITS)
    xs_c, b_c, c_c = _ssd_inputs(xbc_c, conv_w, conv_b)
    xs_l, b_l, c_l = _ssd_inputs(xbc_l, conv_w, conv_b)
    ys_c, ys_l = _ssd_bidir((xs_c, b_c, c_c, dt_c), (xs_l, b_l, c_l, dt_l), a_log, dt_bias)
    qn_c, qr_c, kn_c, krc, v_c = _mla_project(cq_c, ckv_c, kr_c, q_norm_g, w_qb, kv_norm_g, w_kvb, None)
    qn_l, qr_l, kn_l, krl, v_l = _mla_project(cq_l, ckv_l, kr_l, q_norm_g, w_qb, kv_norm_g, w_kvb, rope)
    att_l = _latent_attention(qn_l, qr_l, jnp.concatenate([kn_c, kn_l], axis=1),
                              jnp.concatenate([krc, krl], axis=1), jnp.concatenate([v_c, v_l], axis=1))
    y_l = jnp.concatenate([_ssd_out(ys_l, xs_l, z_l, d_skip, ssd_norm_g), att_l], axis=-1) @ w_out
    if not ctx_out:
        return None, y_l
    b, lc = h_c.shape[:2]
    att_c = _attend(qn_c, qr_c, kn_c, krc, v_c).reshape(b, lc, MLA_HEADS * MLA_V)
    y_c = jnp.concatenate([_ssd_out(ys_c, xs_c, z_c, d_skip, ssd_norm_g), att_c], axis=-1) @ w_out
    return y_c, y_l


def _chunk_gmlp(h, w_in, ln_g, ln_b, ws, bs, w_out):
    b, l, _ = h.shape
    u, v = jnp.split(jax.nn.gelu(h @ w_in, approximate=False), 2, axis=-1)
    v = _layernorm(v, ln_g, ln_b).reshape(b, l // GM_CHUNK, GM_CHUNK, GM_GROUPS, GM_INNER // GM_GROUPS)
    sv = jnp.einsum('gts,bcsgd->bctgd', ws, v) + jnp.transpose(bs)[:, :, None]
    return (u * sv.reshape(b, l, GM_INNER)) @ w_out


def _peer(h, wq, k1, k2, u_tab, v_tab):
    b, l, d = h.shape
    n_tok = b * l
    t = h.reshape(n_tok, d)
    half = PEER_DKEY // 2
    q = (t @ wq).reshape(n_tok, PEER_HEADS, PEER_DKEY)
    s1 = jnp.einsum('thd,hkd->thk', q[..., :half], k1)
    s2 = jnp.einsum('thd,hkd->thk', q[..., half:], k2)
    v1, i1 = lax.top_k(s1, PEER_TOPK)
    v2, i2 = lax.top_k(s2, PEER_TOPK)
    cand_s = (v1[..., :, None] + v2[..., None, :]).reshape(n_tok, PEER_HEADS, PEER_TOPK * PEER_TOPK)
    cand_i = (i1[..., :, None] * PEER_NKEYS + i2[..., None, :]).reshape(n_tok, PEER_HEADS, PEER_TOPK * PEER_TOPK)
    top_s, j = lax.top_k(cand_s, PEER_TOPK)
    idx = jnp.take_along_axis(cand_i, j, axis=-1)
    gate = jax.nn.softmax(top_s.astype(jnp.float32), axis=-1).astype(h.dtype)
    nb = n_tok // PEER_BLOCK

    def expert_block(args):
        tb, ib, gb = args
        act = jax.nn.gelu(jnp.einsum('phkd,pd->phk', jnp.take(u_tab, ib, axis=0), tb), approximate=False)
        return jnp.einsum('phk,phkd->pd', gb * act, jnp.take(v_tab, ib, axis=0))

    out = lax.map(expert_block, (t.reshape(nb, PEER_BLOCK, d),
                                 idx.reshape(nb, PEER_BLOCK, PEER_HEADS, PEER_TOPK),
                                 gate.reshape(nb, PEER_BLOCK, PEER_HEADS, PEER_TOPK)))
    return out.reshape(b, l, d)


def setup_inputs(seed: int = 0) -> dict:
    key = jax.random.key(seed)
    ks = iter(jax.random.split(key, 40))
    nrm = lambda shape, std: std * jax.random.normal(next(ks), shape, jnp.float32)
    ne, no = (DEPTH + 1) // 2, DEPTH // 2
    D = D_MODEL
    x = nrm((BATCH, SEQ, D), 1.0)
    c = nrm((BATCH, D), 1.0)
    ctx = nrm((BATCH, CTX_LEN, D), 1.0)
    c_ctx = nrm((D,), 1.0)
    ada_w = nrm((DEPTH, D, N_MOD * D), 0.02)
    ada_b = nrm((DEPTH, N_MOD * D), 0.02)
    norm1_g = 1.0 + nrm((DEPTH, D), 0.02)
    norm2_g = 1.0 + nrm((DEPTH, D), 0.02)
    hyb_w_in = nrm((ne, D, HYB_IN), D ** -0.5)
    ssd_conv_w = nrm((ne, SSD_CONV_DIM, SSD_CONV), SSD_CONV ** -0.5)
    ssd_conv_b = nrm((ne, SSD_CONV_DIM), 0.02)
    ssd_a_log = jnp.log(jax.random.uniform(next(ks), (ne, 2, SSD_HEADS), jnp.float32, 1.0, 16.0))
    dt0 = jnp.exp(jax.random.uniform(next(ks), (ne, 2, SSD_HEADS), jnp.float32, math.log(1e-3), math.log(1e-1)))
    ssd_dt_bias = dt0 + jnp.log(-jnp.expm1(-dt0))
    ssd_d = 1.0 + nrm((ne, SSD_HEADS), 0.02)
    ssd_norm_g = 1.0 + nrm((ne, SSD_INNER), 0.02)
    mla_q_norm_g = 1.0 + nrm((ne, MLA_Q_RANK), 0.02)
    mla_w_qb = nrm((ne, MLA_Q_RANK, MLA_HEADS * (MLA_NOPE + MLA_ROPE)), MLA_Q_RANK ** -0.5)
    mla_kv_norm_g = 1.0 + nrm((ne, MLA_KV_RANK), 0.02)
    mla_w_kvb = nrm((ne, MLA_KV_RANK, MLA_HEADS * (MLA_NOPE + MLA_V)), MLA_KV_RANK ** -0.5)
    hyb_w_out = nrm((ne, HYB_CAT, D), HYB_CAT ** -0.5)
    gm_w_in = nrm((no, D, 2 * GM_INNER), D ** -0.5)
    gm_ln_g = 1.0 + nrm((no, GM_INNER), 0.02)
    gm_ln_b = nrm((no, GM_INNER), 0.02)
    gm_ws = nrm((no, GM_GROUPS, GM_CHUNK, GM_CHUNK), GM_CHUNK ** -0.5)
    gm_bs = 1.0 + nrm((no, GM_GROUPS, GM_CHUNK), 0.02)
    gm_w_out = nrm((no, GM_INNER, D), GM_INNER ** -0.5)
    peer_wq = nrm((DEPTH, D, PEER_HEADS * PEER_DKEY), D ** -0.5)
    peer_k1 = nrm((DEPTH, PEER_HEADS, PEER_NKEYS, PEER_DKEY // 2), (PEER_DKEY // 2) ** -0.5)
    peer_k2 = nrm((DEPTH, PEER_HEADS, PEER_NKEYS, PEER_DKEY // 2), (PEER_DKEY // 2) ** -0.5)
    peer_u = nrm((DEPTH, PEER_EXPERTS, D), D ** -0.5)
    peer_v = nrm((DEPTH, PEER_EXPERTS, D), PEER_HEADS ** -0.5)
    final_norm_g = 1.0 + nrm((D,), 0.02)
    return {'x': x, 'c': c, 'ctx': ctx, 'c_ctx': c_ctx, 'ada_w': ada_w, 'ada_b': ada_b,
            'norm1_g': norm1_g, 'norm2_g': norm2_g, 'hyb_w_in': hyb_w_in, 'ssd_conv_w': ssd_conv_w,
            'ssd_conv_b': ssd_conv_b, 'ssd_a_log': ssd_a_log, 'ssd_dt_bias': ssd_dt_bias, 'ssd_d': ssd_d,
            'ssd_norm_g': ssd_norm_g, 'mla_q_norm_g': mla_q_norm_g, 'mla_w_qb': mla_w_qb,
            'mla_kv_norm_g': mla_kv_norm_g, 'mla_w_kvb': mla_w_kvb, 'hyb_w_out': hyb_w_out,
            'gm_w_in': gm_w_in, 'gm_ln_g': gm_ln_g, 'gm_ln_b': gm_ln_b, 'gm_ws': gm_ws, 'gm_bs': gm_bs,
            'gm_w_out': gm_w_out, 'peer_wq': peer_wq, 'peer_k1': peer_k1, 'peer_k2': peer_k2,
            'peer_u': peer_u, 'peer_v': peer_v, 'final_norm_g': final_norm_g}


def reference(x, c, ctx, c_ctx, ada_w, ada_b, norm1_g, norm2_g, hyb_w_in, ssd_conv_w, ssd_conv_b,
              ssd_a_log, ssd_dt_bias, ssd_d, ssd_norm_g, mla_q_norm_g, mla_w_qb, mla_kv_norm_g, mla_w_kvb,
              hyb_w_out, gm_w_in, gm_ln_g, gm_ln_b, gm_ws, gm_bs, gm_w_out, peer_wq, peer_k1, peer_k2,
              peer_u, peer_v, final_norm_g):
    rope = _axial_rope(x.shape[1])
    cond_lat = jax.nn.silu(c)
    cond_ctx = jax.nn.silu(c_ctx)
    xc = ctx
    for layer in range(DEPTH):
        i = layer // 2
        even = layer % 2 == 0
        keep_ctx = any(j % 2 == 0 for j in range(layer + 1, DEPTH))
        need_ctx = even or keep_ctx
        sh1, sc1, g1, sh2, sc2, g2 = jnp.split((cond_lat @ ada_w[layer] + ada_b[layer])[:, None, :], N_MOD, axis=-1)
        h_l = _modulate(_rmsnorm(x, norm1_g[layer]), sh1, sc1)
        if need_ctx:
            csh1, csc1, cg1, csh2, csc2, cg2 = jnp.split(cond_ctx @ ada_w[layer] + ada_b[layer], N_MOD, axis=-1)
            h_c = _modulate(_rmsnorm(xc, norm1_g[layer]), csh1, csc1)
        if even:
            y_c, y_l = _hybrid_mix(h_c, h_l, rope, hyb_w_in[i], ssd_conv_w[i], ssd_conv_b[i], ssd_a_log[i],
                                   ssd_dt_bias[i], ssd_d[i], ssd_norm_g[i], mla_q_norm_g[i], mla_w_qb[i],
                                   mla_kv_norm_g[i], mla_w_kvb[i], hyb_w_out[i], keep_ctx)
        else:
            y_l = _chunk_gmlp(h_l, gm_w_in[i], gm_ln_g[i], gm_ln_b[i], gm_ws[i], gm_bs[i], gm_w_out[i])
            y_c = _chunk_gmlp(h_c, gm_w_in[i], gm_ln_g[i], gm_ln_b[i], gm_ws[i], gm_bs[i], gm_w_out[i]) if keep_ctx else None
        x = x + g1 * y_l
        x = x + g2 * _peer(_modulate(_rmsnorm(x, norm2_g[layer]), sh2, sc2),
                           peer_wq[layer], peer_k1[layer], peer_k2[layer], peer_u[layer], peer_v[layer])
        if keep_ctx:
            xc = xc + cg1 * y_c
            xc = xc + cg2 * _peer(_modulate(_rmsnorm(xc, norm2_g[layer]), csh2, csc2),
                                  peer_wq[layer], peer_k1[layer], peer_k2[layer], peer_u[layer], peer_v[layer])
    return _rmsnorm(x, final_norm_g)
```

```python
import math
from contextlib import ExitStack
import numpy as np
import concourse.bass as bass
import concourse.mybir as mybir
from concourse.bass_utils import run_bass_kernel_spmd

F32 = mybir.dt.float32
BF16 = mybir.dt.bfloat16
U32 = mybir.dt.uint32
I32 = mybir.dt.int32
AF = mybir.ActivationFunctionType
ALU = mybir.AluOpType
AX = mybir.AxisListType

EPS = 1e-6
NCORES = 8


class Prog:
    def __init__(self, name):
        self.nc = bass.Bass("TRN2", target_bir_lowering=False)
        self.es = ExitStack()
        nc = self.nc
        self.E = {"pe": nc.tensor, "dve": nc.vector, "act": nc.scalar, "pool": nc.gpsimd, "sp": nc.sync}
        self.sem = {k: self.es.enter_context(nc.semaphore("s_" + k)) for k in self.E}
        self.cnt = {k: 0 for k in self.E}
        self.seen = {k: {} for k in self.E}
        nds = 24
        self.dsem = [self.es.enter_context(nc.semaphore(f"d{i}")) for i in range(nds)]
        self.dcnt = [0] * nds
        self.dnext = 0
        self.lastw = {}
        self.readers = {}
        self.uid = 0
        self.out_toks = []
        self.ccsem = self.es.enter_context(nc.semaphore("ccsem"))
        self.cccnt = 0
        self.scopes = []
        self.prefix = ""

    def push(self, prefix):
        self.scopes.append(ExitStack())
        self.prefix = prefix

    def pop(self):
        self.barrier()
        self.scopes.pop().close()
        self.prefix = ""

    def barrier(self):
        for e in self.E:
            for f in self.E:
                if f != e and self.cnt[f] > 0:
                    self._wait(e, ("e", f, self.cnt[f]))
            for j in range(len(self.dsem)):
                if self.dcnt[j] > 0:
                    self._wait(e, ("d", j, 16 * self.dcnt[j]))
            if self.cccnt > 0:
                self._wait(e, ("c", 0, self.cccnt))

    def cc(self, fn, reads=(), writes=()):
        self._deps("pool", reads, writes)
        ins = fn(self.E["pool"])
        self.cccnt += 1
        ins.then_inc(self.ccsem)
        self._record(("c", 0, self.cccnt), reads, writes)

    def sb(self, shape, dtype=F32, name=None):
        self.uid += 1
        es = self.scopes[-1] if self.scopes else self.es
        return es.enter_context(self.nc.sbuf_tensor(self.prefix + (name or f"t_{self.uid}"), list(shape), dtype))

    def ps(self, shape=(128, 512), dtype=F32, name=None):
        self.uid += 1
        return self.es.enter_context(self.nc.psum_tensor(name or f"p_{self.uid}", list(shape), dtype))

    def dram(self, name, shape, dtype=F32, kind="ExternalInput"):
        return self.nc.dram_tensor(name, list(shape), dtype, kind=kind).ap()

    def _wait(self, eng, tok):
        if tok[0] == "e":
            _, e2, c = tok
            if e2 == eng and eng == "pe":
                return
            key = ("e", e2)
            sem = self.sem[e2]
        elif tok[0] == "c":
            _, _, c = tok
            key = ("c", 0)
            sem = self.ccsem
        else:
            _, j, c = tok
            key = ("d", j)
            sem = self.dsem[j]
        if self.seen[eng].get(key, 0) >= c:
            return
        self.E[eng].wait_ge(sem, c)
        self.seen[eng][key] = c

    def _norm(self, keys):
        pf = self.prefix
        if not pf:
            return list(keys)
        return [k[len(pf):] if isinstance(k, str) and k.startswith(pf) else k for k in keys]

    def _deps(self, eng, reads, writes):
        reads, writes = self._norm(reads), self._norm(writes)
        for k in reads:
            t = self.lastw.get(k)
            if t is not None:
                self._wait(eng, t)
        for k in writes:
            t = self.lastw.get(k)
            if t is not None:
                self._wait(eng, t)
            for t in self.readers.get(k, {}).values():
                self._wait(eng, t)

    def _record(self, tok, reads, writes):
        reads, writes = self._norm(reads), self._norm(writes)
        for k in writes:
            self.lastw[k] = tok
            self.readers[k] = {}
        for k in reads:
            if k in writes:
                continue
            self.readers.setdefault(k, {})[tok[:2]] = tok

    def op(self, eng, fn, reads=(), writes=()):
        self._deps(eng, reads, writes)
        ins = fn(self.E[eng])
        self.cnt[eng] += 1
        ins.then_inc(self.sem[eng], 1)
        self._record(("e", eng, self.cnt[eng]), reads, writes)

    def dma(self, fn, reads=(), writes=(), q="sp", is_out=False):
        j = self.dnext
        self.dnext = (self.dnext + 1) % len(self.dsem)
        if self.dcnt[j] > 0:
            self._wait(q, ("d", j, 16 * self.dcnt[j]))
        self._deps(q, reads, writes)
        ins = fn(self.E[q])
        self.dcnt[j] += 1
        ins.then_inc(self.dsem[j], 16)
        tok = ("d", j, 16 * self.dcnt[j])
        self._record(tok, reads, writes)
        if is_out:
            self.out_toks.append(tok)
        return tok

    def finish(self):
        for j in range(len(self.dsem)):
            if self.dcnt[j] > 0:
                self._wait("sp", ("d", j, 16 * self.dcnt[j]))
        if self.cccnt > 0:
            self._wait("sp", ("c", 0, self.cccnt))
        self.es.close()
        return self.nc

    def load(self, dst_ap, src_ap, dkey, skey=None, q="sp"):
        self.dma(lambda e: e.dma_start(out=dst_ap, in_=src_ap), reads=[skey] if skey else [], writes=[dkey], q=q)

    def store(self, dst_ap, src_ap, skey, dkey=None, q="sp"):
        self.dma(lambda e: e.dma_start(out=dst_ap, in_=src_ap), reads=[skey], writes=[dkey] if dkey else [],
                 q=q, is_out=True)


class Ring:
    def __init__(self, items):
        self.items = items
        self.i = 0

    def get(self):
        it = self.items[self.i]
        self.i = (self.i + 1) % len(self.items)
        return it


def _run(nc, in_maps, ncores=NCORES):
    res = run_bass_kernel_spmd(nc, in_maps, core_ids=list(range(ncores)))
    return res.results


def build_peer(NT, kinds, final=False):
    p = Prog("peer")
    T = NT * 128
    x = p.dram("x", [T, 1024])
    rep = p.dram("rep", [2, 4, 128, 1024])
    wq = p.dram("wqd", [1024, 2048])
    k1T = p.dram("k1T", [128, 8, 128])
    k2T = p.dram("k2T", [128, 8, 128])
    utab = p.dram("utab", [16384, 1024])
    vtab = p.dram("vtab", [16384, 1024])
    ident_d = p.dram("identd", [128, 128])
    iota_d = p.dram("iotad", [128, 16])
    y = p.dram("y", [T, 1024], kind="ExternalOutput")
    if final:
        gfd = p.dram("gfd", [128, 1024])
        gf = p.sb([128, 1024], F32, "gf")
        p.load(gf[:], gfd[:, :], "gf")

    ident = p.sb([128, 128], F32, "ident")
    iota16 = p.sb([128, 16], F32, "iota16")
    wq_sb = p.sb([128, 8, 2048], BF16, "wq")
    k1_sb = p.sb([128, 8, 128], F32, "k1")
    k2_sb = p.sb([128, 8, 128], F32, "k2")
    A_rep = [p.sb([128, 1024], F32, f"A{s}") for s in range(2)]
    B_rep = [p.sb([128, 1024], F32, f"B{s}") for s in range(2)]
    G2_rep = [p.sb([128, 1024], F32, f"G{s}") for s in range(2)]
    NG = 3
    gbuf = [p.sb([128, 4, 1024], F32, f"gb{i}") for i in range(NG)]
    gring = Ring(list(range(NG)))
    W = [p.sb([128, 2048], F32, f"W{i}") for i in range(4)]
    xt = p.sb([128, 1024], F32, "xt")
    tt = p.sb([128, 1024], F32, "tt")
    acc = p.sb([128, 1024], F32, "acc")
    tT = p.sb([128, 8, 128], BF16, "tT")
    small = p.sb([128, 64], F32, "small")
    v16 = p.sb([128, 16, 16], F32, "v16")
    i16u = p.sb([128, 16, 16], U32, "i16u")
    i16f = p.sb([128, 16, 16], F32, "i16f")
    ts16 = p.sb([128, 8, 16], F32, "ts16")
    j16u = p.sb([128, 8, 16], U32, "j16u")
    jhu = p.sb([128, 8, 16], U32, "jhu")
    jlu = p.sb([128, 8, 16], U32, "jlu")
    jhi = p.sb([128, 8, 16], F32, "jhi")
    jlo = p.sb([128, 8, 16], F32, "jlo")
    sel1 = p.sb([128, 8, 16], F32, "sel1")
    sel2 = p.sb([128, 8, 16], F32, "sel2")
    eidf = p.sb([128, 128], F32, "eidf")
    eidu = p.sb([128, 128], I32, "eidu")
    gate = p.sb([128, 8, 16], F32, "gate")
    aval = p.sb([128, 128], F32, "aval")
    wval = p.sb([128, 128], F32, "wval")
    psum = [p.ps([128, 512], F32, f"ps{i}") for i in range(8)]

    p.load(ident[:], ident_d[:, :], "ident")
    p.load(iota16[:], iota_d[:, :], "iota16")
    p.load(k1_sb[:], k1T[:, :, :], "k1")
    p.load(k2_sb[:], k2T[:, :, :], "k2")
    wq_v = wq.rearrange("(k p) n -> p k n", p=128)
    for k in range(8):
        for hlf in range(2):
            gi = gring.get()
            p.load(gbuf[gi][:, 0:1, :], wq_v[:, k:k + 1, hlf * 1024:(hlf + 1) * 1024], f"gb{gi}")
            p.op("dve", lambda e, gi=gi, k=k, hlf=hlf: e.tensor_copy(
                out=wq_sb[:, k, hlf * 1024:(hlf + 1) * 1024], in_=gbuf[gi][:, 0, :]),
                reads=[f"gb{gi}"], writes=["wq"])
    for s in range(2):
        gi = gring.get()
        p.load(gbuf[gi][:, 0:4, :], rep[s].rearrange("f p n -> p f n"), f"gb{gi}")
        p.op("dve", lambda e, gi=gi, s=s: e.scalar_tensor_tensor(
            out=A_rep[s][:], in0=gbuf[gi][:, 1, :], scalar=1.0, in1=gbuf[gi][:, 0, :], op0=ALU.add, op1=ALU.mult),
            reads=[f"gb{gi}"], writes=[f"A{s}"])
        p.op("dve", lambda e, gi=gi, s=s: e.tensor_copy(out=B_rep[s][:], in_=gbuf[gi][:, 2, :]),
             reads=[f"gb{gi}"], writes=[f"B{s}"])
        p.op("dve", lambda e, gi=gi, s=s: e.tensor_copy(out=G2_rep[s][:], in_=gbuf[gi][:, 3, :]),
             reads=[f"gb{gi}"], writes=[f"G{s}"])

    for ti in range(NT):
        s = kinds[ti]
        r0 = ti * 128
        p.load(xt[:], x[r0:r0 + 128, :], "xt")
        p.op("act", lambda e: e.activation(out=tt[:], in_=xt[:], func=AF.Square, accum_out=small[:, 0:1]),
             reads=["xt"], writes=["tt", "small"])
        p.op("dve", lambda e: e.tensor_scalar(out=small[:, 1:2], in0=small[:, 0:1], scalar1=1.0 / 1024, scalar2=EPS,
                                              op0=ALU.mult, op1=ALU.add), reads=["small"], writes=["small"])
        p.op("act", lambda e: e.activation(out=small[:, 3:4], in_=small[:, 1:2], func=AF.Sqrt), reads=["small"], writes=["small"])
        p.op("dve", lambda e: e.reciprocal(out=small[:, 2:3], in_=small[:, 3:4]), reads=["small"], writes=["small"])
        p.op("dve", lambda e, s=s: e.scalar_tensor_tensor(out=tt[:], in0=xt[:], scalar=small[:, 2:3], in1=A_rep[s][:],
                                                          op0=ALU.mult, op1=ALU.mult),
             reads=["xt", "small", f"A{s}"], writes=["tt"])
        p.op("dve", lambda e, s=s: e.tensor_tensor(out=tt[:], in0=tt[:], in1=B_rep[s][:], op=ALU.add),
             reads=[f"B{s}"], writes=["tt"])
        for half in range(2):
            pb = psum[half]
            for c in range(4):
                k = half * 4 + c
                p.op("pe", lambda e, pb=pb, c=c, k=k: e.transpose(out=pb[:, c * 128:(c + 1) * 128],
                                                                  in_=tt[:, k * 128:(k + 1) * 128], identity=ident[:]),
                     reads=["tt", "ident"], writes=[pb.name])
            p.op("act", lambda e, pb=pb, half=half: e.activation(
                out=tT[:, half * 4:(half + 1) * 4, :], in_=pb[:].rearrange("p (c t) -> p c t", c=4), func=AF.Copy),
                reads=[pb.name], writes=["tT"])
        qT = W[0]
        for jb in range(4):
            pb = psum[2 + jb]
            for jj in range(4):
                j = jb * 4 + jj
                for k in range(8):
                    p.op("pe", lambda e, pb=pb, jj=jj, j=j, k=k: e.matmul(
                        pb[:, jj * 128:(jj + 1) * 128], lhsT=wq_sb[:, k, j * 128:(j + 1) * 128], rhs=tT[:, k, :],
                        start=(k == 0), stop=(k == 7)), reads=["wq", "tT"], writes=[pb.name])
            p.op("act", lambda e, pb=pb, jb=jb: e.activation(out=qT[:, jb * 512:(jb + 1) * 512], in_=pb[:], func=AF.Copy),
                 reads=[pb.name], writes=["W0"])
        S = W[1]
        for gb in range(4):
            pb = psum[(6 + gb) % 8]
            for gg in range(4):
                g = gb * 4 + gg
                h, half = g // 2, g % 2
                ksb = k1_sb if half == 0 else k2_sb
                p.op("pe", lambda e, pb=pb, gg=gg, g=g, h=h, ksb=ksb: e.matmul(
                    pb[:, gg * 128:(gg + 1) * 128], lhsT=qT[:, g * 128:(g + 1) * 128], rhs=ksb[:, h, :],
                    start=True, stop=True), reads=["W0", "k1", "k2"], writes=[pb.name])
            p.op("act", lambda e, pb=pb, gb=gb: e.activation(out=S[:, gb * 512:(gb + 1) * 512], in_=pb[:], func=AF.Copy),
                 reads=[pb.name], writes=["W1"])
        S2 = W[2]
        for g in range(16):
            sl = slice(g * 128, (g + 1) * 128)
            p.op("dve", lambda e, g=g, sl=sl: e.max(out=v16[:, g, 0:8], in_=S[:, sl]), reads=["W1"], writes=["v16"])
            p.op("dve", lambda e, g=g, sl=sl: e.max_index(out=i16u[:, g, 0:8], in_max=v16[:, g, 0:8], in_values=S[:, sl]),
                 reads=["W1", "v16"], writes=["i16u"])
            p.op("dve", lambda e, g=g, sl=sl: e.match_replace(out=S2[:, sl], in_to_replace=v16[:, g, 0:8],
                                                              in_values=S[:, sl], imm_value=-1e30),
                 reads=["W1", "v16"], writes=["W2"])
            p.op("dve", lambda e, g=g, sl=sl: e.max(out=v16[:, g, 8:16], in_=S2[:, sl]), reads=["W2"], writes=["v16"])
            p.op("dve", lambda e, g=g, sl=sl: e.max_index(out=i16u[:, g, 8:16], in_max=v16[:, g, 8:16], in_values=S2[:, sl]),
                 reads=["W2", "v16"], writes=["i16u"])
        p.op("dve", lambda e: e.tensor_copy(out=i16f[:], in_=i16u[:]), reads=["i16u"], writes=["i16f"])
        v4 = v16[:].rearrange("p (h two) k -> p h two k", two=2)
        cs = W[3][:].rearrange("p (h i j) -> p h i j", h=8, i=16)
        p.op("dve", lambda e: e.tensor_tensor(out=cs, in0=v4[:, :, 0, :].unsqueeze(3).to_broadcast([128, 8, 16, 16]),
                                              in1=v4[:, :, 1, :].unsqueeze(2).to_broadcast([128, 8, 16, 16]), op=ALU.add),
             reads=["v16"], writes=["W3"])
        cs2 = W[0]
        for h in range(8):
            sl = slice(h * 256, (h + 1) * 256)
            p.op("dve", lambda e, h=h, sl=sl: e.max(out=ts16[:, h, 0:8], in_=W[3][:, sl]), reads=["W3"], writes=["ts16"])
            p.op("dve", lambda e, h=h, sl=sl: e.max_index(out=j16u[:, h, 0:8], in_max=ts16[:, h, 0:8], in_values=W[3][:, sl]),
                 reads=["W3", "ts16"], writes=["j16u"])
            p.op("dve", lambda e, h=h, sl=sl: e.match_replace(out=cs2[:, sl], in_to_replace=ts16[:, h, 0:8],
                                                              in_values=W[3][:, sl], imm_value=-1e30),
                 reads=["W3", "ts16"], writes=["W0"])
            p.op("dve", lambda e, h=h, sl=sl: e.max(out=ts16[:, h, 8:16], in_=cs2[:, sl]), reads=["W0"], writes=["ts16"])
            p.op("dve", lambda e, h=h, sl=sl: e.max_index(out=j16u[:, h, 8:16], in_max=ts16[:, h, 8:16], in_values=cs2[:, sl]),
                 reads=["W0", "ts16"], writes=["j16u"])
        p.op("dve", lambda e: e.tensor_scalar(out=jhu[:], in0=j16u[:], scalar1=4, scalar2=None, op0=ALU.logical_shift_right),
             reads=["j16u"], writes=["jhu"])
        p.op("dve", lambda e: e.tensor_scalar(out=jlu[:], in0=j16u[:], scalar1=15, scalar2=None, op0=ALU.bitwise_and),
             reads=["j16u"], writes=["jlu"])
        p.op("dve", lambda e: e.tensor_copy(out=jhi[:], in_=jhu[:]), reads=["jhu"], writes=["jhi"])
        p.op("dve", lambda e: e.tensor_copy(out=jlo[:], in_=jlu[:]), reads=["jlu"], writes=["jlo"])
        i4 = i16f[:].rearrange("p (h two) k -> p h two k", two=2)
        eq = W[1][:].rearrange("p (h r k) -> p h r k", h=8, r=16)
        for which, (jsel, dst) in enumerate(((jhi, sel1), (jlo, sel2))):
            for h in range(8):
                p.op("dve", lambda e, h=h, jsel=jsel: e.tensor_tensor(
                    out=eq[:, h], in0=jsel[:, h, :].unsqueeze(2).to_broadcast([128, 16, 16]),
                    in1=iota16[:].unsqueeze(1).to_broadcast([128, 16, 16]), op=ALU.is_equal),
                    reads=[jsel.name, "iota16"], writes=["W1"])
                p.op("dve", lambda e, h=h, which=which: e.tensor_tensor(
                    out=eq[:, h], in0=eq[:, h], in1=i4[:, h, which, :].unsqueeze(1).to_broadcast([128, 16, 16]),
                    op=ALU.mult), reads=["i16f"], writes=["W1"])
            p.op("dve", lambda e, dst=dst: e.tensor_reduce(out=dst[:], in_=eq, axis=AX.X, op=ALU.add),
                 reads=["W1"], writes=[dst.name])
        p.op("dve", lambda e: e.scalar_tensor_tensor(out=eidf[:], in0=sel1[:].rearrange("p h r -> p (h r)"), scalar=128.0,
                                                     in1=sel2[:].rearrange("p h r -> p (h r)"), op0=ALU.mult, op1=ALU.add),
             reads=["sel1", "sel2"], writes=["eidf"])
        p.op("dve", lambda e: e.tensor_copy(out=eidu[:], in_=eidf[:]), reads=["eidf"], writes=["eidu"])
        p.op("dve", lambda e: e.tensor_tensor(out=gate[:], in0=ts16[:], in1=ts16[:, :, 0:1].to_broadcast([128, 8, 16]),
                                              op=ALU.subtract), reads=["ts16"], writes=["gate"])
        p.op("act", lambda e: e.activation(out=gate[:], in_=gate[:], func=AF.Exp), reads=[], writes=["gate"])
        p.op("dve", lambda e: e.tensor_reduce(out=small[:, 8:16], in_=gate[:], axis=AX.X, op=ALU.add),
             reads=["gate"], writes=["small"])
        p.op("dve", lambda e: e.reciprocal(out=small[:, 16:24], in_=small[:, 8:16]), reads=[], writes=["small"])
        p.op("dve", lambda e: e.tensor_tensor(out=gate[:], in0=gate[:],
                                              in1=small[:, 16:24].unsqueeze(2).to_broadcast([128, 8, 16]), op=ALU.mult),
             reads=["small"], writes=["gate"])
        for c in range(32):
            gi = gring.get()
            for ee in range(4):
                col = c * 4 + ee
                p.dma(lambda e, gi=gi, ee=ee, col=col: e.indirect_dma_start(
                    out=gbuf[gi][:, ee, :], out_offset=None, in_=utab[:, :],
                    in_offset=bass.IndirectOffsetOnAxis(ap=eidu[:, col:col + 1], axis=0)),
                    reads=["eidu"], writes=[f"gb{gi}"], q="pool")
            p.op("dve", lambda e, gi=gi: e.tensor_tensor(out=gbuf[gi][:], in0=gbuf[gi][:],
                                                         in1=tt[:].unsqueeze(1).to_broadcast([128, 4, 1024]), op=ALU.mult),
                 reads=["tt"], writes=[f"gb{gi}"])
            p.op("dve", lambda e, gi=gi, c=c: e.tensor_reduce(out=aval[:, c * 4:(c + 1) * 4], in_=gbuf[gi][:], axis=AX.X,
                                                              op=ALU.add), reads=[f"gb{gi}"], writes=["aval"])
        p.op("act", lambda e: e.activation(out=wval[:], in_=aval[:], func=AF.Gelu), reads=["aval"], writes=["wval"])
        p.op("dve", lambda e: e.tensor_tensor(out=wval[:], in0=wval[:], in1=gate[:].rearrange("p h r -> p (h r)"),
                                              op=ALU.mult), reads=["gate"], writes=["wval"])
        for c in range(32):
            gi = gring.get()
            for ee in range(4):
                col = c * 4 + ee
                p.dma(lambda e, gi=gi, ee=ee, col=col: e.indirect_dma_start(
                    out=gbuf[gi][:, ee, :], out_offset=None, in_=vtab[:, :],
                    in_offset=bass.IndirectOffsetOnAxis(ap=eidu[:, col:col + 1], axis=0)),
                    reads=["eidu"], writes=[f"gb{gi}"], q="pool")
            for ee in range(4):
                col = c * 4 + ee
                if col == 0:
                    p.op("dve", lambda e, gi=gi, ee=ee, col=col: e.tensor_scalar(
                        out=acc[:], in0=gbuf[gi][:, ee, :], scalar1=wval[:, col:col + 1], scalar2=None, op0=ALU.mult),
                        reads=[f"gb{gi}", "wval"], writes=["acc"])
                else:
                    p.op("dve", lambda e, gi=gi, ee=ee, col=col: e.scalar_tensor_tensor(
                        out=acc[:], in0=gbuf[gi][:, ee, :], scalar=wval[:, col:col + 1], in1=acc[:],
                        op0=ALU.mult, op1=ALU.add), reads=[f"gb{gi}", "wval"], writes=["acc"])
        p.op("dve", lambda e, s=s: e.tensor_tensor(out=acc[:], in0=acc[:], in1=G2_rep[s][:], op=ALU.mult),
             reads=[f"G{s}"], writes=["acc"])
        p.op("dve", lambda e: e.tensor_tensor(out=acc[:], in0=acc[:], in1=xt[:], op=ALU.add), reads=["xt"], writes=["acc"])
        if final:
            p.op("act", lambda e: e.activation(out=tt[:], in_=acc[:], func=AF.Square, accum_out=small[:, 32:33]),
                 reads=["acc"], writes=["tt", "small"])
            p.op("dve", lambda e: e.tensor_scalar(out=small[:, 33:34], in0=small[:, 32:33], scalar1=1.0 / 1024, scalar2=EPS,
                                                  op0=ALU.mult, op1=ALU.add), reads=[], writes=["small"])
            p.op("act", lambda e: e.activation(out=small[:, 34:35], in_=small[:, 33:34], func=AF.Sqrt), reads=[], writes=["small"])
            p.op("dve", lambda e: e.reciprocal(out=small[:, 35:36], in_=small[:, 34:35]), reads=[], writes=["small"])
            p.op("dve", lambda e: e.scalar_tensor_tensor(out=acc[:], in0=acc[:], scalar=small[:, 35:36], in1=gf[:],
                                                         op0=ALU.mult, op1=ALU.mult), reads=["small", "gf"], writes=["acc"])
        p.store(y[r0:r0 + 128, :], acc[:], "acc")
    return p.finish()


def load_w_bf16(p, dst, src, kc, n, stg, piece=2048):
    v = src.rearrange("(k p) n -> p k n", p=128)
    i = 0
    for k in range(kc):
        for c0 in range(0, n, piece):
            c1 = min(n, c0 + piece)
            st = stg[i % len(stg)]
            i += 1
            p.load(st[:, 0:c1 - c0], v[:, k, c0:c1], st.name)
            eng = "dve" if i % 2 else "act"
            if eng == "dve":
                p.op("dve", lambda e, st=st, k=k, c0=c0, c1=c1: e.tensor_copy(out=dst[:, k, c0:c1], in_=st[:, 0:c1 - c0]),
                     reads=[st.name], writes=[dst.name])
            else:
                p.op("act", lambda e, st=st, k=k, c0=c0, c1=c1: e.activation(out=dst[:, k, c0:c1], in_=st[:, 0:c1 - c0],
                                                                               func=AF.Copy),
                     reads=[st.name], writes=[dst.name])


def fm_rstd(p, src, kcs, n, ones, sq, pb, rstd, inv_n):
    k0, k1 = kcs
    p.op("act", lambda e: e.activation(out=sq[:, k0:k1, 0:n], in_=src[:, k0:k1, 0:n], func=AF.Square),
         reads=[src.name], writes=[sq.name])
    for k in range(k0, k1):
        p.op("pe", lambda e, k=k: e.matmul(pb[:, 0:n], lhsT=ones[:], rhs=sq[:, k, 0:n], start=(k == k0), stop=(k == k1 - 1)),
             reads=[sq.name, ones.name], writes=[pb.name])
    p.op("act", lambda e: e.activation(out=rstd[:, 0:n], in_=pb[:, 0:n], func=AF.Sqrt, scale=inv_n, bias=EPS),
         reads=[pb.name], writes=[rstd.name])
    p.op("dve", lambda e: e.reciprocal(out=rstd[:, 0:n], in_=rstd[:, 0:n]), reads=[], writes=[rstd.name])


def fm_norm_mod(p, xt, n, ones, sq, pb, rstd, A, B, hT):
    fm_rstd(p, xt, (0, 8), n, ones, sq, pb, rstd, 1.0 / 1024)
    p.op("dve", lambda e: e.tensor_tensor(out=sq[:, :, 0:n], in0=xt[:, :, 0:n],
                                          in1=rstd[:, 0:n].unsqueeze(1).to_broadcast([128, 8, n]), op=ALU.mult),
         reads=[xt.name, rstd.name], writes=[sq.name])
    for k in range(8):
        if k % 2 == 0:
            p.op("act", lambda e, k=k: e.activation(out=hT[:, k, 0:n], in_=sq[:, k, 0:n], func=AF.Identity,
                                                    scale=A[:, k:k + 1], bias=B[:, k:k + 1]),
                 reads=[sq.name, "modc"], writes=[hT.name])
        else:
            p.op("dve", lambda e, k=k: e.tensor_scalar(out=hT[:, k, 0:n], in0=sq[:, k, 0:n], scalar1=A[:, k:k + 1],
                                                       scalar2=B[:, k:k + 1], op0=ALU.mult, op1=ALU.add),
                 reads=[sq.name, "modc"], writes=[hT.name])


def load_modc(p, modd, nsets, nv):
    modc = p.sb([128, nsets, nv, 8], F32, "modc")
    p.load(modc[:], modd.rearrange("s v p c -> p s v c"), "modc")
    return modc


def build_ada(NL=4, NCOL=768):
    p = Prog("ada")
    cT = p.dram("cT", [128, 8, 5])
    w = p.dram("w", [NL, 1024, NCOL])
    b = p.dram("b", [NL, 5, NCOL])
    out = p.dram("out", [NL, 5, NCOL], kind="ExternalOutput")
    c_sb = p.sb([128, 8, 5], F32, "c_sb")
    s_sb = p.sb([128, 8, 5], F32, "s_sb")
    w_sb = [p.sb([128, 8, NCOL], F32, f"w_sb{i}") for i in range(2)]
    b_sb = p.sb([5, NL, NCOL], F32, "b_sb")
    o_sb = p.sb([5, NL, NCOL], F32, "o_sb")
    pbs = [p.ps([128, 512], F32, f"pa{i}") for i in range(4)]
    p.load(c_sb[:], cT[:, :, :], "c_sb")
    p.load(b_sb[:], b.rearrange("l r n -> r l n"), "b_sb")
    p.op("act", lambda e: e.activation(out=s_sb[:], in_=c_sb[:], func=AF.Silu), reads=["c_sb"], writes=["s_sb"])
    half = NCOL // 2
    for l in range(NL):
        ws = w_sb[l % 2]
        p.load(ws[:], w[l].rearrange("(k p) n -> p k n", p=128), ws.name)
        for hh in range(2):
            pb = pbs[(l * 2 + hh) % 4]
            for k in range(8):
                p.op("pe", lambda e, pb=pb, k=k, hh=hh, ws=ws: e.matmul(
                    pb[0:5, 0:half], lhsT=s_sb[:, k, :], rhs=ws[:, k, hh * half:(hh + 1) * half],
                    start=(k == 0), stop=(k == 7)), reads=["s_sb", ws.name], writes=[pb.name])
            p.op("dve", lambda e, pb=pb, l=l, hh=hh: e.tensor_tensor(
                out=o_sb[:, l, hh * half:(hh + 1) * half], in0=pb[0:5, 0:half], in1=b_sb[:, l, hh * half:(hh + 1) * half],
                op=ALU.add), reads=[pb.name, "b_sb"], writes=["o_sb"])
    p.store(out.rearrange("l r n -> r l n"), o_sb[:], "o_sb")
    return p.finish()


def build_inproj(blocks, NOB):
    p = Prog("inproj")
    T = max(t0 + n for t0, n, _ in blocks)
    xT = p.dram("xT", [1024, T])
    modd = p.dram("modd", [2, 2, 128, 8])
    gcol = p.dram("gcol", [128, 8])
    wd = p.dram("wd", [1024, NOB * 128])
    onesd = p.dram("onesd", [128, 128])
    proj = p.dram("proj", [NOB * 128, T], kind="ExternalOutput")
    ones = p.sb([128, 128], F32, "ones")
    p.load(ones[:], onesd[:, :], "ones")
    modc = load_modc(p, modd, 2, 2)
    g_sb = p.sb([128, 8], F32, "g_sb")
    p.load(g_sb[:], gcol[:, :], "g_sb")
    Acol = p.sb([128, 2, 8], F32, "Acol")
    for s in range(2):
        p.op("dve", lambda e, s=s: e.scalar_tensor_tensor(out=Acol[:, s, :], in0=modc[:, s, 0, :], scalar=1.0, in1=g_sb[:],
                                                          op0=ALU.add, op1=ALU.mult),
             reads=["modc", "g_sb"], writes=["modc"])
    stg = [p.sb([128, 2048], F32, f"stg{i}") for i in range(2)]
    w_sb = p.sb([128, 8, NOB * 128], BF16, "w_sb")
    load_w_bf16(p, w_sb, wd, 8, NOB * 128, stg)
    xts = [p.sb([128, 8, 512], F32, f"xt{i}") for i in range(2)]
    sq = p.sb([128, 8, 512], F32, "sq")
    rstd = p.sb([128, 512], F32, "rstd")
    hT = p.sb([128, 8, 512], BF16, "hT")
    outs = [p.sb([128, 512], F32, f"ot{i}") for i in range(4)]
    pbs = [p.ps([128, 512], F32, f"pp{i}") for i in range(8)]
    xv = xT.rearrange("(k p) t -> p k t", p=128)
    oi = 0
    for bi, (t0, n, s) in enumerate(blocks):
        xt = xts[bi % 2]
        p.load(xt[:, :, 0:n], xv[:, :, t0:t0 + n], xt.name)
        fm_norm_mod(p, xt, n, ones, sq, pbs[0], rstd, Acol[:, s, :], modc[:, s, 1, :], hT)
        for j in range(NOB):
            pb = pbs[1 + j % 7]
            for k in range(8):
                p.op("pe", lambda e, pb=pb, j=j, k=k: e.matmul(pb[:, 0:n], lhsT=w_sb[:, k, j * 128:(j + 1) * 128],
                                                               rhs=hT[:, k, 0:n], start=(k == 0), stop=(k == 7)),
                     reads=["w_sb", "hT"], writes=[pb.name])
            ot = outs[oi % 4]
            oi += 1
            if oi % 2:
                p.op("act", lambda e, pb=pb, ot=ot: e.activation(out=ot[:, 0:n], in_=pb[:, 0:n], func=AF.Copy),
                     reads=[pb.name], writes=[ot.name])
            else:
                p.op("dve", lambda e, pb=pb, ot=ot: e.tensor_copy(out=ot[:, 0:n], in_=pb[:, 0:n]),
                     reads=[pb.name], writes=[ot.name])
            p.store(proj[j * 128:(j + 1) * 128, t0:t0 + n], ot[:, 0:n], ot.name)
    return p.finish()


def build_ssd(NCH_CTX=2, NCH_LAT=64):
    p = Prog("ssd")
    NCH = NCH_CTX + NCH_LAT
    NTOK = NCH * 128
    xbcT = p.dram("xbcT", [768, NTOK])
    dtr = p.dram("dtr", [NTOK, 8])
    convw = p.dram("convw", [768, 5])
    convb = p.dram("convb", [128, 6])
    rep8 = p.dram("rep8", [128, 5, 8])
    cst = p.dram("cst", [6, 128, 128])
    yf = p.dram("yf", [NTOK, 512], kind="ExternalOutput")
    yb = p.dram("yb", [NTOK, 512], kind="ExternalOutput")

    cs = p.sb([128, 6, 128], F32, "cs")
    p.load(cs[:], cst.rearrange("c p n -> p c n"), "cs")
    ident, ones = cs[:, 0, :], cs[:, 5, :]
    mask01 = p.sb([128, 2, 128], F32, "mask01")
    p.op("dve", lambda e: e.tensor_copy(out=mask01[:], in_=cs[:, 1:3, :]), reads=["cs"], writes=["mask01"])
    dt_all = p.sb([128, NCH, 8], F32, "dt_all")
    p.load(dt_all[:], dtr.rearrange("(c p) h -> p c h", p=128), "dt_all")
    cw = p.sb([128, 6, 5], F32, "cw")
    p.load(cw[:], convw.rearrange("(k p) j -> p k j", p=128), "cw")
    cb = p.sb([128, 6], F32, "cb")
    p.load(cb[:], convb[:, :], "cb")
    r8 = p.sb([128, 5, 8], F32, "r8")
    p.load(r8[:], rep8[:, :, :], "r8")
    aneg = p.sb([128, 2, 8], F32, "aneg")
    p.op("act", lambda e: e.activation(out=aneg[:], in_=r8[:, 2:4, :], func=AF.Exp), reads=["r8"], writes=["aneg"])
    p.op("dve", lambda e: e.tensor_scalar(out=aneg[:], in0=aneg[:], scalar1=-1.0, scalar2=None, op0=ALU.mult),
         reads=[], writes=["aneg"])

    wins = [p.sb([128, 6, 132], F32, f"win{i}") for i in range(2)]
    xc = p.sb([128, 6, 128], F32, "xc")
    acc = p.sb([128, 6, 128], F32, "cacc")
    xs_tm = p.sb([128, 512], F32, "xs_tm")
    btm = p.sb([128, 128], F32, "btm")
    sm = p.sb([128, 96], F32, "sm")
    da_b = p.sb([128, 8, 128], F32, "da_b")
    xdw = p.sb([128, 512], F32, "xdw")
    xd = p.sb([128, 512], F32, "xd")
    cbm = p.sb([128, 128], F32, "cbm")
    arg = p.sb([128, 8, 128], F32, "arg")
    dec = p.sb([128, 8, 128], F32, "dec")
    ys = [p.sb([128, 512], F32, f"y{i}") for i in range(2)]
    ytmp = p.sb([128, 512], F32, "ytmp")
    H = p.sb([128, 512], F32, "H")
    P = [p.ps([128, 512], F32, f"pq{i}") for i in range(8)]
    xv = xbcT.rearrange("(k p) t -> p k t", p=128)

    it = 0
    for d in range(2):
        tri = cs[:, 1 + d, :]
        neg = cs[:, 3 + d, :]
        yout = yf if d == 0 else yb
        order = list(range(NCH)) if d == 0 else list(range(NCH_CTX - 1, -1, -1)) + list(range(NCH - 1, NCH_CTX - 1, -1))
        p.op("dve", lambda e: e.memset(H[:], 0.0), reads=[], writes=["H"])
        for c in order:
            s0, s1 = (0, NCH_CTX) if c < NCH_CTX else (NCH_CTX, NCH)
            win = wins[it % 2]
            yt = ys[it % 2]
            it += 1
            lo = 0 if c > s0 else 2
            hi = 132 if c < s1 - 1 else 130
            if lo or hi < 132:
                p.op("dve", lambda e, win=win: e.memset(win[:], 0.0), reads=[], writes=[win.name])
            p.load(win[:, :, lo:hi], xv[:, :, c * 128 - 2 + lo:c * 128 - 2 + hi], win.name)
            for k in range(6):
                for j in range(5):
                    if j == 0:
                        p.op("dve", lambda e, k=k, j=j, win=win: e.tensor_scalar(
                            out=acc[:, k, :], in0=win[:, k, j:j + 128], scalar1=cw[:, k, j:j + 1], scalar2=None, op0=ALU.mult),
                            reads=[win.name, "cw"], writes=["cacc"])
                    else:
                        p.op("dve", lambda e, k=k, j=j, win=win: e.scalar_tensor_tensor(
                            out=acc[:, k, :], in0=win[:, k, j:j + 128], scalar=cw[:, k, j:j + 1], in1=acc[:, k, :],
                            op0=ALU.mult, op1=ALU.add), reads=[win.name, "cw"], writes=["cacc"])
            for k in range(6):
                p.op("act", lambda e, k=k: e.activation(out=xc[:, k, :], in_=acc[:, k, :], func=AF.Silu, bias=cb[:, k:k + 1]),
                     reads=["cacc", "cb"], writes=["xc"])
            for k in range(4):
                p.op("pe", lambda e, k=k: e.transpose(out=P[0][:, k * 128:(k + 1) * 128], in_=xc[:, k, :], identity=ident),
                     reads=["xc", "cs"], writes=["pq0"])
            p.op("act", lambda e: e.activation(out=xs_tm[:], in_=P[0][:], func=AF.Copy), reads=["pq0"], writes=["xs_tm"])
            p.op("pe", lambda e: e.transpose(out=P[1][:, 0:128], in_=xc[:, 4, :], identity=ident),
                 reads=["xc", "cs"], writes=["pq1"])
            p.op("dve", lambda e: e.tensor_copy(out=btm[:], in_=P[1][:, 0:128]), reads=["pq1"], writes=["btm"])
            p.op("dve", lambda e, c=c, d=d: e.tensor_tensor(out=sm[:, 16:24], in0=dt_all[:, c, :], in1=r8[:, d, :], op=ALU.add),
                 reads=["dt_all", "r8"], writes=["sm"])
            p.op("act", lambda e: e.activation(out=sm[:, 16:24], in_=sm[:, 16:24], func=AF.Exp), reads=[], writes=["sm"])
            p.op("act", lambda e: e.activation(out=sm[:, 0:8], in_=sm[:, 16:24], func=AF.Ln, bias=1.0), reads=[], writes=["sm"])
            p.op("dve", lambda e, d=d: e.tensor_tensor(out=sm[:, 8:16], in0=sm[:, 0:8], in1=aneg[:, d, :], op=ALU.mult),
                 reads=["aneg"], writes=["sm"])
            p.op("pe", lambda e, tri=tri: e.matmul(P[1][:, 128:136], lhsT=tri, rhs=sm[:, 8:16], start=True, stop=True),
                 reads=["sm", "cs"], writes=["pq1"])
            p.op("pe", lambda e: e.matmul(P[1][:, 136:144], lhsT=ones, rhs=sm[:, 8:16], start=True, stop=True),
                 reads=["sm", "cs"], writes=["pq1"])
            p.op("dve", lambda e: e.tensor_copy(out=sm[:, 24:40], in_=P[1][:, 128:144]), reads=["pq1"], writes=["sm"])
            p.op("dve", lambda e: e.tensor_tensor(out=sm[:, 40:48], in0=sm[:, 32:40], in1=sm[:, 24:32], op=ALU.subtract),
                 reads=[], writes=["sm"])
            p.op("act", lambda e: e.activation(out=sm[:, 48:72], in_=sm[:, 24:48], func=AF.Exp), reads=[], writes=["sm"])
            p.op("dve", lambda e: e.tensor_scalar(out=sm[:, 72:80], in0=sm[:, 24:32], scalar1=-1.0, scalar2=None, op0=ALU.mult),
                 reads=[], writes=["sm"])
            p.op("dve", lambda e: e.tensor_tensor(out=sm[:, 80:88], in0=sm[:, 0:8], in1=sm[:, 64:72], op=ALU.mult),
                 reads=[], writes=["sm"])
            ecum, cdec = sm[:, 48:56], sm[:, 56:64]
            xs3 = xs_tm[:].rearrange("p (h q) -> p h q", h=8)
            p.op("dve", lambda e: e.tensor_tensor(out=xdw[:].rearrange("p (h q) -> p h q", h=8), in0=xs3,
                                                  in1=sm[:, 80:88].unsqueeze(2).to_broadcast([128, 8, 64]), op=ALU.mult),
                 reads=["xs_tm", "sm"], writes=["xdw"])
            p.op("dve", lambda e: e.tensor_tensor(out=xd[:].rearrange("p (h q) -> p h q", h=8), in0=xs3,
                                                  in1=sm[:, 0:8].unsqueeze(2).to_broadcast([128, 8, 64]), op=ALU.mult),
                 reads=["xs_tm", "sm"], writes=["xd"])
            p.op("dve", lambda e: e.tensor_copy(out=da_b[:], in_=sm[:, 8:16].unsqueeze(2).to_broadcast([128, 8, 128])),
                 reads=["sm"], writes=["da_b"])
            p.op("pe", lambda e: e.matmul(P[2][:], lhsT=btm[:], rhs=xdw[:], start=True, stop=True),
                 reads=["btm", "xdw"], writes=["pq2"])
            p.op("pe", lambda e: e.matmul(P[3][:], lhsT=xc[:, 5, :], rhs=H[:], start=True, stop=True),
                 reads=["xc", "H"], writes=["pq3"])
            p.op("pe", lambda e: e.matmul(P[1][:, 256:384], lhsT=xc[:, 4, :], rhs=xc[:, 5, :], start=True, stop=True),
                 reads=["xc"], writes=["pq1"])
            p.op("dve", lambda e, d=d: e.tensor_tensor(out=cbm[:], in0=P[1][:, 256:384], in1=mask01[:, d, :], op=ALU.mult),
                 reads=["pq1", "mask01"], writes=["cbm"])
            for hb in range(2):
                pb = P[4 + hb]
                for hh in range(4):
                    h = hb * 4 + hh
                    p.op("pe", lambda e, pb=pb, hh=hh, h=h, tri=tri: e.matmul(
                        pb[:, hh * 128:(hh + 1) * 128], lhsT=da_b[:, h, :], rhs=tri, start=True, stop=True),
                        reads=["da_b", "cs"], writes=[pb.name])
                p.op("dve", lambda e, pb=pb, hb=hb, neg=neg: e.tensor_tensor(
                    out=arg[:, hb * 4:(hb + 1) * 4, :], in0=pb[:].rearrange("p (h t) -> p h t", h=4),
                    in1=neg.unsqueeze(1).to_broadcast([128, 4, 128]), op=ALU.add),
                    reads=[pb.name, "cs"], writes=["arg"])
            for h in range(8):
                p.op("act", lambda e, h=h: e.activation(out=dec[:, h, :], in_=arg[:, h, :], func=AF.Exp, bias=sm[:, 72 + h:73 + h]),
                     reads=["arg", "sm"], writes=["dec"])
            p.op("dve", lambda e: e.tensor_tensor(out=dec[:], in0=dec[:], in1=cbm[:].unsqueeze(1).to_broadcast([128, 8, 128]),
                                                  op=ALU.mult), reads=["cbm"], writes=["dec"])
            for h in range(8):
                p.op("pe", lambda e, h=h: e.matmul(P[6][:, h * 64:(h + 1) * 64], lhsT=dec[:, h, :], rhs=xd[:, h * 64:(h + 1) * 64],
                                                   start=True, stop=True), reads=["dec", "xd"], writes=["pq6"])
            p.op("dve", lambda e: e.tensor_tensor(out=ytmp[:].rearrange("p (h q) -> p h q", h=8),
                                                  in0=P[3][:].rearrange("p (h q) -> p h q", h=8),
                                                  in1=ecum.unsqueeze(2).to_broadcast([128, 8, 64]), op=ALU.mult),
                 reads=["pq3", "sm"], writes=["ytmp"])
            p.op("dve", lambda e, yt=yt: e.tensor_tensor(out=yt[:], in0=ytmp[:], in1=P[6][:], op=ALU.add),
                 reads=["ytmp", "pq6"], writes=[yt.name])
            if d == 0:
                p.op("dve", lambda e: e.tensor_tensor(out=ytmp[:].rearrange("p (h q) -> p h q", h=8), in0=xs3,
                                                      in1=r8[:, 4, :].unsqueeze(2).to_broadcast([128, 8, 64]), op=ALU.mult),
                     reads=["xs_tm", "r8"], writes=["ytmp"])
                p.op("dve", lambda e, yt=yt: e.tensor_tensor(out=yt[:], in0=yt[:], in1=ytmp[:], op=ALU.add),
                     reads=["ytmp"], writes=[yt.name])
            p.store(yout[c * 128:(c + 1) * 128, :], yt[:], yt.name)
            p.op("dve", lambda e: e.tensor_tensor(out=H[:].rearrange("p (h q) -> p h q", h=8),
                                                  in0=H[:].rearrange("p (h q) -> p h q", h=8),
                                                  in1=cdec.unsqueeze(2).to_broadcast([128, 8, 64]), op=ALU.mult),
                 reads=["sm"], writes=["H"])
            p.op("dve", lambda e: e.tensor_tensor(out=H[:], in0=H[:], in1=P[2][:], op=ALU.add), reads=["pq2"], writes=["H"])
    return p.finish()


def build_attn(NCTX=256, NLAT=8192, NH=4, stop=99):
    p = Prog("attn")
    NTOK = NCTX + NLAT
    scale = (128 + 64) ** -0.5
    cqT = p.dram("cqT", [384, NTOK])
    ckvT = p.dram("ckvT", [256, NTOK])
    kr2 = p.dram("kr2", [2, 32, NTOK])
    cosd = p.dram("cosd", [32, NTOK])
    sind = p.dram("sind", [32, NTOK])
    wqn_d = p.dram("wqn", [384, NH * 128])
    wq1_d = p.dram("wq1", [384, NH * 32])
    wq2_d = p.dram("wq2", [384, NH * 32])
    wkn_d = p.dram("wkn", [256, NH * 128])
    wv_d = p.dram("wvd", [256, NH * 128])
    qg_d = p.dram("qg", [128, 3])
    kvg_d = p.dram("kvg", [128, 2])
    ones_d = p.dram("onesd", [128, 128])
    att = p.dram("att", [NH * 128, NTOK], kind="ExternalOutput")

    ones = p.sb([128, 128], F32, "ones")
    p.load(ones[:], ones_d[:, :], "ones")
    onesb = p.sb([128, 128], BF16, "onesb")
    p.op("dve", lambda e: e.tensor_copy(out=onesb[:], in_=ones[:]), reads=["ones"], writes=["onesb"])
    qg = p.sb([128, 3], F32, "qgs")
    p.load(qg[:], qg_d[:, :], "qgs")
    kvg = p.sb([128, 2], F32, "kvgs")
    p.load(kvg[:], kvg_d[:, :], "kvgs")
    stg = [p.sb([128, 2048], F32, f"stg{i}") for i in range(2)]
    wqn = p.sb([128, 3, NH * 128], BF16, "wqn_s")
    wq1 = p.sb([128, 3, NH * 32], BF16, "wq1_s")
    wq2 = p.sb([128, 3, NH * 32], BF16, "wq2_s")
    wkn = p.sb([128, 2, NH * 128], BF16, "wkn_s")
    wv = p.sb([128, 2, NH * 128], BF16, "wv_s")
    load_w_bf16(p, wqn, wqn_d, 3, NH * 128, stg)
    load_w_bf16(p, wq1, wq1_d, 3, NH * 32, stg)
    load_w_bf16(p, wq2, wq2_d, 3, NH * 32, stg)
    load_w_bf16(p, wkn, wkn_d, 2, NH * 128, stg)
    load_w_bf16(p, wv, wv_d, 2, NH * 128, stg)

    ckvn = p.sb([128, 2, NTOK], BF16, "ckvn")
    K1 = p.sb([32, NTOK], BF16, "K1")
    K2 = p.sb([32, NTOK], BF16, "K2")
    KnT = p.sb([128, NTOK], BF16, "KnT")
    Vh = p.sb([128, NTOK // 128, 128], BF16, "Vh")
    cqn = p.sb([128, 3, 512], BF16, "cqn")
    xin = p.sb([128, 3, 512], F32, "xin")
    sq = p.sb([128, 3, 512], F32, "sq")
    rstd = p.sb([128, 512], F32, "rstd")
    XA = p.sb([32, 512], F32, "XA")
    XB = p.sb([32, 512], F32, "XB")
    CS = p.sb([32, 512], F32, "CS")
    SN = p.sb([32, 512], F32, "SN")
    r1 = p.sb([32, 512], F32, "r1")
    r2 = p.sb([32, 512], F32, "r2")
    t1 = p.sb([32, 512], F32, "t1")
    t2 = p.sb([32, 512], F32, "t2")
    QnT = p.sb([128, 512], BF16, "QnT")
    Q1 = p.sb([32, 512], BF16, "Q1")
    Q2 = p.sb([32, 512], BF16, "Q2")
    NM = p.sb([128, 512], BF16, "NM")
    Pts = [p.sb([128, 512], BF16, f"Pt{i}") for i in range(3)]
    osb = p.sb([128, 512], F32, "osb")
    mx = p.sb([128, 8], F32, "mx")
    P = [p.ps([128, 512], F32, f"pw{i}") for i in range(8)]
    p.op("dve", lambda e: e.memset(mx[:], 0.0), reads=[], writes=["mx"])

    blocks = [(t0, min(512, NTOK - t0)) for t0 in range(0, NTOK, 512)]

    def rope32(n, o1, o2, pb):
        p.op("dve", lambda e: e.tensor_tensor(out=t1[:, 0:n], in0=XA[:, 0:n], in1=CS[:, 0:n], op=ALU.mult), reads=["XA", "CS"], writes=["t1"])
        p.op("dve", lambda e: e.tensor_tensor(out=t2[:, 0:n], in0=XB[:, 0:n], in1=SN[:, 0:n], op=ALU.mult), reads=["XB", "SN"], writes=["t2"])
        p.op("dve", lambda e: e.tensor_tensor(out=r1[:, 0:n], in0=t1[:, 0:n], in1=t2[:, 0:n], op=ALU.subtract), reads=["t1", "t2"], writes=["r1"])
        p.op("dve", lambda e: e.tensor_tensor(out=t1[:, 0:n], in0=XB[:, 0:n], in1=CS[:, 0:n], op=ALU.mult), reads=["XB", "CS"], writes=["t1"])
        p.op("dve", lambda e: e.tensor_tensor(out=t2[:, 0:n], in0=XA[:, 0:n], in1=SN[:, 0:n], op=ALU.mult), reads=["XA", "SN"], writes=["t2"])
        p.op("dve", lambda e: e.tensor_tensor(out=r2[:, 0:n], in0=t1[:, 0:n], in1=t2[:, 0:n], op=ALU.add), reads=["t1", "t2"], writes=["r2"])
        p.op("act", lambda e: e.activation(out=o1, in_=r1[:, 0:n], func=AF.Copy), reads=["r1"], writes=[o1.name])
        p.op("act", lambda e: e.activation(out=o2, in_=r2[:, 0:n], func=AF.Copy), reads=["r2"], writes=[o2.name])
        p.op("act", lambda e: e.activation(out=t1[:, 0:n], in_=r1[:, 0:n], func=AF.Square), reads=["r1"], writes=["t1"])
        p.op("act", lambda e: e.activation(out=t2[:, 0:n], in_=r2[:, 0:n], func=AF.Square), reads=["r2"], writes=["t2"])
        p.op("pe", lambda e: e.matmul(pb[:, 0:n], lhsT=ones[0:32, :], rhs=t1[:, 0:n], start=False, stop=False),
             reads=["t1", "ones"], writes=[pb.name])
        p.op("pe", lambda e: e.matmul(pb[:, 0:n], lhsT=ones[0:32, :], rhs=t2[:, 0:n], start=False, stop=True),
             reads=["t2", "ones"], writes=[pb.name])

    def load_rope_tables(t0, n):
        p.load(CS[:, 0:n], cosd[:, t0:t0 + n], "CS")
        p.load(SN[:, 0:n], sind[:, t0:t0 + n], "SN")

    def running_max(pb, n, col):
        p.op("dve", lambda e: e.tensor_reduce(out=mx[:, 2:3], in_=pb[:, 0:n], axis=AX.X, op=ALU.max),
             reads=[pb.name], writes=["mx"])
        p.op("dve", lambda e: e.tensor_tensor(out=mx[:, col:col + 1], in0=mx[:, col:col + 1], in1=mx[:, 2:3], op=ALU.max),
             reads=[], writes=["mx"])

    def norm_block(src, kc, gcol, dst_fn, inv, t0, n):
        p.load(xin[:, 0:kc, 0:n], src[:, :, t0:t0 + n], "xin")
        fm_rstd(p, xin, (0, kc), n, ones, sq, P[5], rstd, inv)
        p.op("dve", lambda e: e.tensor_tensor(out=sq[:, 0:kc, 0:n], in0=xin[:, 0:kc, 0:n],
                                              in1=rstd[:, 0:n].unsqueeze(1).to_broadcast([128, kc, n]), op=ALU.mult),
             reads=["xin", "rstd"], writes=["sq"])
        for k in range(kc):
            dst = dst_fn(k)
            p.op("act", lambda e, k=k, dst=dst: e.activation(out=dst, in_=sq[:, k, 0:n], func=AF.Identity, scale=gcol[:, k:k + 1]),
                 reads=["sq", gcol.name], writes=[dst.name])

    cqv = cqT.rearrange("(k p) t -> p k t", p=128)
    ckvv = ckvT.rearrange("(k p) t -> p k t", p=128)
    zero_ap = ones
    for (t0, n) in blocks:
        norm_block(ckvv, 2, kvg, lambda k: ckvn[:, k, t0:t0 + n], 1.0 / 256, t0, n)
        load_rope_tables(t0, n)
        p.load(XA[:, 0:n], kr2[0, :, t0:t0 + n], "XA")
        p.load(XB[:, 0:n], kr2[1, :, t0:t0 + n], "XB")
        p.op("act", lambda e: e.activation(out=t1[:, 0:n], in_=XA[:, 0:n], func=AF.Copy, scale=0.0), reads=["XA"], writes=["t1"])
        p.op("pe", lambda e: e.matmul(P[6][:, 0:n], lhsT=ones[0:32, :], rhs=t1[:, 0:n], start=True, stop=False),
             reads=["t1", "ones"], writes=["pw6"])
        rope32(n, K1[:, t0:t0 + n], K2[:, t0:t0 + n], P[6])
        running_max(P[6], n, 0)
    if stop == 0:
        return p.finish()

    qblocks = [(0, NCTX, NCTX // 128)] + [(NCTX + i * 512, 512, NTOK // 128) for i in range(NLAT // 512)]
    pi = 0
    for h in range(NH):
        p.op("dve", lambda e: e.memset(mx[:, 1:2], 0.0), reads=[], writes=["mx"])
        for (t0, n) in blocks:
            pb = P[5]
            for k in range(2):
                p.op("pe", lambda e, k=k: e.matmul(pb[:, 0:n], lhsT=wkn[:, k, h * 128:(h + 1) * 128], rhs=ckvn[:, k, t0:t0 + n],
                                                   start=(k == 0), stop=(k == 1)), reads=["wkn_s", "ckvn"], writes=[pb.name])
            p.op("act", lambda e: e.activation(out=KnT[:, t0:t0 + n], in_=pb[:, 0:n], func=AF.Copy), reads=[pb.name], writes=["KnT"])
            p.op("act", lambda e: e.activation(out=sq[:, 0, 0:n], in_=pb[:, 0:n], func=AF.Square), reads=[pb.name], writes=["sq"])
            p.op("pe", lambda e: e.matmul(P[6][:, 0:n], lhsT=ones[:], rhs=sq[:, 0, 0:n], start=True, stop=True),
                 reads=["sq", "ones"], writes=["pw6"])
            running_max(P[6], n, 1)
            pv = P[7]
            nt = n // 128
            for j in range(nt):
                for k in range(2):
                    p.op("pe", lambda e, j=j, k=k: e.matmul(
                        pv[:, j * 128:(j + 1) * 128], lhsT=ckvn[:, k, t0 + j * 128:t0 + (j + 1) * 128],
                        rhs=wv[:, k, h * 128:(h + 1) * 128], start=(k == 0), stop=(k == 1)),
                        reads=["wv_s", "ckvn"], writes=["pw7"])
            p.op("dve", lambda e, nt=nt: e.tensor_copy(out=Vh[:, t0 // 128:t0 // 128 + nt, :],
                                                      in_=pv[:, 0:nt * 128].rearrange("p (j d) -> p j d", j=nt)),
                 reads=["pw7"], writes=["Vh"])
        p.op("dve", lambda e: e.tensor_tensor(out=mx[:, 4:5], in0=mx[:, 0:1], in1=mx[:, 1:2], op=ALU.add), reads=[], writes=["mx"])
        p.op("act", lambda e: e.activation(out=mx[:, 4:5], in_=mx[:, 4:5], func=AF.Sqrt), reads=[], writes=["mx"])
        p.op("dve", lambda e: e.tensor_scalar(out=mx[:, 3:4], in0=mx[:, 4:5], scalar1=-1.0, scalar2=None, op0=ALU.mult),
             reads=[], writes=["mx"])
        if stop == 1:
            return p.finish()
        for (q0, n, nkt) in qblocks:
            norm_block(cqv, 3, qg, lambda k: cqn[:, k, 0:n], 1.0 / 384, q0, n)
            pb = P[5]
            for k in range(3):
                p.op("pe", lambda e, k=k: e.matmul(pb[:, 0:n], lhsT=wqn[:, k, h * 128:(h + 1) * 128], rhs=cqn[:, k, 0:n],
                                                   start=(k == 0), stop=(k == 2)), reads=["wqn_s", "cqn"], writes=[pb.name])
            p.op("act", lambda e: e.activation(out=QnT[:, 0:n], in_=pb[:, 0:n], func=AF.Copy), reads=[pb.name], writes=["QnT"])
            p.op("act", lambda e: e.activation(out=sq[:, 0, 0:n], in_=pb[:, 0:n], func=AF.Square), reads=[pb.name], writes=["sq"])
            for (wsb, pq, dst) in ((wq1, P[6], XA), (wq2, P[7], XB)):
                for k in range(3):
                    p.op("pe", lambda e, k=k, wsb=wsb, pq=pq: e.matmul(
                        pq[0:32, 0:n], lhsT=wsb[:, k, h * 32:(h + 1) * 32], rhs=cqn[:, k, 0:n],
                        start=(k == 0), stop=(k == 2)), reads=[wsb.name, "cqn"], writes=[pq.name])
                p.op("dve", lambda e, pq=pq, dst=dst: e.tensor_copy(out=dst[:, 0:n], in_=pq[0:32, 0:n]), reads=[pq.name], writes=[dst.name])
            load_rope_tables(q0, n)
            p.op("pe", lambda e: e.matmul(P[5][:, 0:n], lhsT=ones[:], rhs=sq[:, 0, 0:n], start=True, stop=False),
                 reads=["sq", "ones"], writes=["pw5"])
            rope32(n, Q1[:, 0:n], Q2[:, 0:n], P[5])
            p.op("act", lambda e: e.activation(out=rstd[:, 0:n], in_=P[5][:, 0:n], func=AF.Sqrt), reads=["pw5"], writes=["rstd"])
            p.op("dve", lambda e: e.tensor_scalar(out=NM[:, 0:n], in0=rstd[:, 0:n], scalar1=mx[:, 3:4], scalar2=None,
                                                  op0=ALU.mult), reads=["rstd", "mx"], writes=["NM"])
            if stop == 2:
                return p.finish()
            po, pd = P[3], P[4]
            for kt in range(nkt):
                ps = P[pi % 3]
                pt = Pts[pi % 3]
                pi += 1
                ks = slice(kt * 128, (kt + 1) * 128)
                p.op("pe", lambda e, ps=ps, ks=ks: e.matmul(ps[:, 0:n], lhsT=KnT[:, ks], rhs=QnT[:, 0:n], start=True, stop=False),
                     reads=["KnT", "QnT"], writes=[ps.name])
                p.op("pe", lambda e, ps=ps, ks=ks: e.matmul(ps[:, 0:n], lhsT=K1[:, ks], rhs=Q1[:, 0:n], start=False, stop=False),
                     reads=["K1", "Q1"], writes=[ps.name])
                p.op("pe", lambda e, ps=ps, ks=ks: e.matmul(ps[:, 0:n], lhsT=K2[:, ks], rhs=Q2[:, 0:n], start=False, stop=False),
                     reads=["K2", "Q2"], writes=[ps.name])
                p.op("pe", lambda e, ps=ps: e.matmul(ps[:, 0:n], lhsT=onesb[0:1, :], rhs=NM[0:1, 0:n], start=False, stop=True),
                     reads=["onesb", "NM"], writes=[ps.name])
                p.op("act", lambda e, ps=ps, pt=pt: e.activation(out=pt[:, 0:n], in_=ps[:, 0:n], func=AF.Exp, scale=scale),
                     reads=[ps.name], writes=[pt.name])
                p.op("pe", lambda e, pt=pt, kt=kt: e.matmul(po[:, 0:n], lhsT=Vh[:, kt, :], rhs=pt[:, 0:n],
                                                            start=(kt == 0), stop=(kt == nkt - 1)),
                     reads=["Vh", pt.name], writes=[po.name])
                p.op("pe", lambda e, pt=pt, kt=kt: e.matmul(pd[:, 0:n], lhsT=onesb[:], rhs=pt[:, 0:n],
                                                            start=(kt == 0), stop=(kt == nkt - 1)),
                     reads=["onesb", pt.name], writes=[pd.name])
            p.op("dve", lambda e: e.reciprocal(out=rstd[:, 0:n], in_=pd[:, 0:n]), reads=[pd.name], writes=["rstd"])
            p.op("dve", lambda e: e.tensor_tensor(out=osb[:, 0:n], in0=po[:, 0:n], in1=rstd[:, 0:n], op=ALU.mult),
                 reads=[po.name, "rstd"], writes=["osb"])
            p.store(att[h * 128:(h + 1) * 128, q0:q0 + n], osb[:, 0:n], "osb")
    return p.finish()


def build_outproj(blocks):
    p = Prog("outproj")
    T = max(t0 + n for t0, n, _ in blocks)
    yfT = p.dram("yfT", [1024, T])
    ybT = p.dram("ybT", [1024, T])
    zT = p.dram("zT", [1024, T])
    attT = p.dram("attT", [1024, T])
    xT = p.dram("xT", [1024, T])
    wd = p.dram("wd", [2048, 1024])
    ngd = p.dram("ngd", [128, 8])
    modd = p.dram("modd", [2, 1, 128, 8])
    onesd = p.dram("onesd", [128, 128])
    xo = p.dram("xo", [1024, T], kind="ExternalOutput")
    ones = p.sb([128, 128], F32, "ones")
    p.load(ones[:], onesd[:, :], "ones")
    modc = load_modc(p, modd, 2, 1)
    ng = p.sb([128, 8], F32, "ng")
    p.load(ng[:], ngd[:, :], "ng")
    stg = [p.sb([128, 2048], F32, f"stg{i}") for i in range(2)]
    w_sb = p.sb([128, 16, 1024], BF16, "w_sb")
    load_w_bf16(p, w_sb, wd, 16, 1024, stg)
    yf = p.sb([128, 8, 512], F32, "yf")
    yb = p.sb([128, 8, 512], F32, "yb")
    zt = p.sb([128, 8, 512], F32, "zt")
    at = p.sb([128, 8, 512], F32, "at")
    xt = p.sb([128, 8, 512], F32, "xt")
    sq = p.sb([128, 8, 512], F32, "sq")
    rs = [p.sb([128, 512], F32, f"rs{i}") for i in range(2)]
    cat = p.sb([128, 16, 512], BF16, "cat")
    pbs = [p.ps([128, 512], F32, f"pp{i}") for i in range(8)]
    v = lambda a: a.rearrange("(k p) t -> p k t", p=128)
    for (t0, n, s) in blocks:
        for (tile, src) in ((yf, yfT), (yb, ybT), (zt, zT), (at, attT), (xt, xT)):
            p.load(tile[:, :, 0:n], v(src)[:, :, t0:t0 + n], tile.name)
        p.op("dve", lambda e: e.tensor_tensor(out=yf[:, :, 0:n], in0=yf[:, :, 0:n], in1=yb[:, :, 0:n], op=ALU.add),
             reads=["yb"], writes=["yf"])
        p.op("act", lambda e: e.activation(out=zt[:, :, 0:n], in_=zt[:, :, 0:n], func=AF.Silu), reads=[], writes=["zt"])
        p.op("dve", lambda e: e.tensor_tensor(out=yf[:, :, 0:n], in0=yf[:, :, 0:n], in1=zt[:, :, 0:n], op=ALU.mult),
             reads=["zt"], writes=["yf"])
        for g in range(2):
            fm_rstd(p, yf, (g * 4, g * 4 + 4), n, ones, sq, pbs[g], rs[g], 1.0 / 512)
        for g in range(2):
            p.op("dve", lambda e, g=g: e.tensor_tensor(out=sq[:, g * 4:g * 4 + 4, 0:n], in0=yf[:, g * 4:g * 4 + 4, 0:n],
                                                       in1=rs[g][:, 0:n].unsqueeze(1).to_broadcast([128, 4, n]), op=ALU.mult),
                 reads=["yf", rs[g].name], writes=["sq"])
        for k in range(8):
            p.op("act", lambda e, k=k: e.activation(out=cat[:, k, 0:n], in_=sq[:, k, 0:n], func=AF.Identity, scale=ng[:, k:k + 1]),
                 reads=["sq", "ng"], writes=["cat"])
        p.op("dve", lambda e: e.tensor_copy(out=cat[:, 8:16, 0:n], in_=at[:, :, 0:n]), reads=["at"], writes=["cat"])
        for j in range(8):
            pb = pbs[2 + j % 6]
            for k in range(16):
                p.op("pe", lambda e, pb=pb, j=j, k=k: e.matmul(pb[:, 0:n], lhsT=w_sb[:, k, j * 128:(j + 1) * 128],
                                                               rhs=cat[:, k, 0:n], start=(k == 0), stop=(k == 15)),
                     reads=["w_sb", "cat"], writes=[pb.name])
            p.op("dve", lambda e, pb=pb, j=j, s=s: e.scalar_tensor_tensor(
                out=xt[:, j, 0:n], in0=pb[:, 0:n], scalar=modc[:, s, 0, j:j + 1], in1=xt[:, j, 0:n], op0=ALU.mult, op1=ALU.add),
                reads=[pb.name, "modc"], writes=["xt"])
        p.store(v(xo)[:, :, t0:t0 + n], xt[:, :, 0:n], "xt")
    return p.finish()


def build_gmlp(kinds):
    p = Prog("gmlp")
    NCHK = len(kinds)
    T = NCHK * 128
    xT = p.dram("xT", [1024, T])
    modd = p.dram("modd", [2, 3, 128, 8])
    gcol = p.dram("gcol", [128, 8])
    wind = p.dram("wind", [1024, 4096])
    woutd = p.dram("woutd", [2048, 1024])
    wsT = p.dram("wsT", [128, 8, 128])
    reps = p.dram("reps", [2, 128, 2048])
    bsr = p.dram("bsr", [128, 8, 128])
    onesd = p.dram("onesd", [128, 128])
    xo = p.dram("xo", [1024, T], kind="ExternalOutput")
    ones = p.sb([128, 128], F32, "ones")
    p.load(ones[:], onesd[:, :], "ones")
    modc = load_modc(p, modd, 2, 3)
    g_sb = p.sb([128, 8], F32, "g_sb")
    p.load(g_sb[:], gcol[:, :], "g_sb")
    Acol = p.sb([128, 2, 8], F32, "Acol")
    for s in range(2):
        p.op("dve", lambda e, s=s: e.scalar_tensor_tensor(out=Acol[:, s, :], in0=modc[:, s, 0, :], scalar=1.0, in1=g_sb[:],
                                                          op0=ALU.add, op1=ALU.mult),
             reads=["modc", "g_sb"], writes=["modc"])
    stg = [p.sb([128, 2048], F32, f"stg{i}") for i in range(2)]
    win = p.sb([128, 8, 4096], BF16, "win")
    wout = p.sb([128, 16, 1024], BF16, "wout")
    load_w_bf16(p, win, wind, 8, 4096, stg)
    load_w_bf16(p, wout, woutd, 16, 1024, stg)
    ws_sb = p.sb([128, 8, 128], F32, "ws_sb")
    p.load(ws_sb[:], wsT[:, :, :], "ws_sb")
    lng = p.sb([128, 2048], F32, "lng")
    lnb = p.sb([128, 2048], F32, "lnb")
    p.load(lng[:], reps[0], "lng")
    p.load(lnb[:], reps[1], "lnb")
    bs_sb = p.sb([128, 8, 128], F32, "bs_sb")
    p.load(bs_sb[:], bsr[:, :, :], "bs_sb")
    xts = [p.sb([128, 8, 128], F32, f"xt{i}") for i in range(2)]
    sq = p.sb([128, 8, 128], F32, "sq")
    rstd = p.sb([128, 128], F32, "rstd")
    hT = p.sb([128, 8, 128], BF16, "hT")
    uT = p.sb([128, 16, 128], F32, "uT")
    vt = p.sb([128, 2048], F32, "vt")
    junk = p.sb([128, 2048], F32, "junk")
    st = p.sb([128, 16], F32, "st")
    gated = p.sb([128, 16, 128], BF16, "gated")
    tmp = p.sb([128, 512], F32, "tmp")
    pbs = [p.ps([128, 512], F32, f"pp{i}") for i in range(8)]
    xv = xT.rearrange("(k p) t -> p k t", p=128)
    xov = xo.rearrange("(k p) t -> p k t", p=128)
    for ci, s in enumerate(kinds):
        t0 = ci * 128
        xt = xts[ci % 2]
        p.load(xt[:], xv[:, :, t0:t0 + 128], xt.name)
        fm_norm_mod(p, xt, 128, ones, sq, pbs[0], rstd, Acol[:, s, :], modc[:, s, 1, :], hT)
        for jb in range(4):
            pb = pbs[1 + jb % 3]
            for jj in range(4):
                j = jb * 4 + jj
                for k in range(8):
                    p.op("pe", lambda e, pb=pb, jj=jj, j=j, k=k: e.matmul(
                        pb[:, jj * 128:(jj + 1) * 128], lhsT=win[:, k, j * 128:(j + 1) * 128], rhs=hT[:, k, :],
                        start=(k == 0), stop=(k == 7)), reads=["win", "hT"], writes=[pb.name])
            p.op("act", lambda e, pb=pb, jb=jb: e.activation(out=uT[:, jb * 4:(jb + 1) * 4, :],
                                                            in_=pb[:].rearrange("p (j t) -> p j t", j=4), func=AF.Gelu),
                 reads=[pb.name], writes=["uT"])
        for cb in range(4):
            pb = pbs[4 + cb % 2]
            for k in range(8):
                p.op("pe", lambda e, pb=pb, cb=cb, k=k: e.matmul(
                    pb[:], lhsT=hT[:, k, :], rhs=win[:, k, 2048 + cb * 512:2048 + (cb + 1) * 512],
                    start=(k == 0), stop=(k == 7)), reads=["win", "hT"], writes=[pb.name])
            p.op("act", lambda e, pb=pb, cb=cb: e.activation(out=vt[:, cb * 512:(cb + 1) * 512], in_=pb[:], func=AF.Gelu),
                 reads=[pb.name], writes=["vt"])
        p.op("dve", lambda e: e.tensor_reduce(out=st[:, 0:1], in_=vt[:], axis=AX.X, op=ALU.add), reads=["vt"], writes=["st"])
        p.op("act", lambda e: e.activation(out=junk[:], in_=vt[:], func=AF.Square), reads=["vt"], writes=["junk"])
        p.op("dve", lambda e: e.tensor_reduce(out=st[:, 1:2], in_=junk[:], axis=AX.X, op=ALU.add), reads=["junk"], writes=["st"])
        p.op("dve", lambda e: e.tensor_scalar(out=st[:, 2:4], in0=st[:, 0:2], scalar1=1.0 / 2048, scalar2=None, op0=ALU.mult),
             reads=[], writes=["st"])
        p.op("dve", lambda e: e.tensor_tensor(out=st[:, 4:5], in0=st[:, 2:3], in1=st[:, 2:3], op=ALU.mult), reads=[], writes=["st"])
        p.op("dve", lambda e: e.tensor_tensor(out=st[:, 5:6], in0=st[:, 3:4], in1=st[:, 4:5], op=ALU.subtract), reads=[], writes=["st"])
        p.op("act", lambda e: e.activation(out=st[:, 6:7], in_=st[:, 5:6], func=AF.Sqrt, bias=EPS), reads=[], writes=["st"])
        p.op("dve", lambda e: e.reciprocal(out=st[:, 7:8], in_=st[:, 6:7]), reads=[], writes=["st"])
        p.op("dve", lambda e: e.tensor_scalar(out=vt[:], in0=vt[:], scalar1=st[:, 2:3], scalar2=st[:, 7:8], op0=ALU.subtract,
                                              op1=ALU.mult), reads=["st"], writes=["vt"])
        p.op("dve", lambda e: e.tensor_tensor(out=vt[:], in0=vt[:], in1=lng[:], op=ALU.mult), reads=["lng"], writes=["vt"])
        p.op("dve", lambda e: e.tensor_tensor(out=vt[:], in0=vt[:], in1=lnb[:], op=ALU.add), reads=["lnb"], writes=["vt"])
        for jb in range(4):
            pb = pbs[6 + jb % 2]
            for jj in range(4):
                j = jb * 4 + jj
                p.op("pe", lambda e, pb=pb, jj=jj, j=j: e.matmul(pb[:, jj * 128:(jj + 1) * 128], lhsT=vt[:, j * 128:(j + 1) * 128],
                                                                 rhs=ws_sb[:, j // 2, :], start=True, stop=True),
                     reads=["vt", "ws_sb"], writes=[pb.name])
            p.op("dve", lambda e, pb=pb, jb=jb: e.tensor_tensor(
                out=tmp[:].rearrange("p (g r t) -> p g r t", g=2, r=2), in0=pb[:].rearrange("p (g r t) -> p g r t", g=2, r=2),
                in1=bs_sb[:, jb * 2:jb * 2 + 2, :].unsqueeze(2).to_broadcast([128, 2, 2, 128]), op=ALU.add),
                reads=[pb.name, "bs_sb"], writes=["tmp"])
            p.op("dve", lambda e, jb=jb: e.tensor_tensor(out=gated[:, jb * 4:(jb + 1) * 4, :],
                                                         in0=tmp[:].rearrange("p (j t) -> p j t", j=4),
                                                         in1=uT[:, jb * 4:(jb + 1) * 4, :], op=ALU.mult),
                 reads=["tmp", "uT"], writes=["gated"])
        for jb in range(2):
            pb = pbs[1 + jb]
            for jj in range(4):
                j = jb * 4 + jj
                for k in range(16):
                    p.op("pe", lambda e, pb=pb, jj=jj, j=j, k=k: e.matmul(
                        pb[:, jj * 128:(jj + 1) * 128], lhsT=wout[:, k, j * 128:(j + 1) * 128], rhs=gated[:, k, :],
                        start=(k == 0), stop=(k == 15)), reads=["wout", "gated"], writes=[pb.name])
            for jj in range(4):
                j = jb * 4 + jj
                p.op("dve", lambda e, pb=pb, jj=jj, j=j, s=s: e.scalar_tensor_tensor(
                    out=xt[:, j, :], in0=pb[:, jj * 128:(jj + 1) * 128], scalar=modc[:, s, 2, j:j + 1], in1=xt[:, j, :],
                    op0=ALU.mult, op1=ALU.add), reads=[pb.name, "modc"], writes=[xt.name])
        p.store(xov[:, :, t0:t0 + 128], xt[:], xt.name)
    return p.finish()


def _colv(v, k=8):
    return np.ascontiguousarray(np.asarray(v, np.float32).reshape(k, 128).T)


def _rows(v, n=128):
    return np.ascontiguousarray(np.broadcast_to(np.asarray(v, np.float32)[None, :], (n, v.shape[0])))


def _rope_tables(n_lat, n_ctx):
    grid_w, n_freq = 64, 16
    rows = n_lat // grid_w
    row = np.broadcast_to(np.arange(rows, dtype=np.float32)[:, None], (rows, grid_w)).reshape(-1)
    col = np.broadcast_to(np.arange(grid_w, dtype=np.float32)[None, :], (rows, grid_w)).reshape(-1)
    inv = (np.float32(10000.0) ** (-np.arange(n_freq, dtype=np.float32) / np.float32(n_freq))).astype(np.float32)
    ang = np.concatenate([row[:, None] * inv, col[:, None] * inv], axis=-1).astype(np.float32)
    cos = np.concatenate([np.ones((n_ctx, 32), np.float32), np.cos(ang).astype(np.float32)], 0)
    sin = np.concatenate([np.zeros((n_ctx, 32), np.float32), np.sin(ang).astype(np.float32)], 0)
    return np.ascontiguousarray(cos.T), np.ascontiguousarray(sin.T)


def _ssd_consts():
    s = np.arange(128)[:, None]
    t = np.arange(128)[None, :]
    trif = (s <= t).astype(np.float32)
    trib = (s >= t).astype(np.float32)
    return np.stack([np.eye(128, dtype=np.float32), trif, trib, (1 - trif) * np.float32(-30000.0),
                     (1 - trib) * np.float32(-30000.0), np.ones((128, 128), np.float32)]).astype(np.float32)


def kernel_unfused(x, c, ctx, c_ctx, ada_w, ada_b, norm1_g, norm2_g, hyb_w_in, ssd_conv_w, ssd_conv_b,
           ssd_a_log, ssd_dt_bias, ssd_d, ssd_norm_g, mla_q_norm_g, mla_w_qb, mla_kv_norm_g, mla_w_kvb,
           hyb_w_out, gm_w_in, gm_ln_g, gm_ln_b, gm_ws, gm_bs, gm_w_out, peer_wq, peer_k1, peer_k2,
           peer_u, peer_v, final_norm_g):
    f32 = lambda a: np.ascontiguousarray(np.asarray(a, dtype=np.float32))
    x, c, ctx, c_ctx = f32(x), f32(c), f32(ctx), f32(c_ctx)
    B, L, D = x.shape
    LC = ctx.shape[1]
    depth = ada_w.shape[0]
    HL, HC = L // 2, LC // 2
    NTOK = LC + L
    ones128 = np.ones((128, 128), np.float32)
    ident = np.eye(128, dtype=np.float32)

    cv = np.concatenate([c, c_ctx[None]], 0)
    cT = np.ascontiguousarray(cv.T.reshape(8, 128, B + 1).transpose(1, 0, 2))
    ncol = 6 * D // NCORES
    ada_w, ada_b = f32(ada_w), f32(ada_b)
    ims = [dict(cT=cT, w=np.ascontiguousarray(ada_w[:, :, ci * ncol:(ci + 1) * ncol]),
                b=np.ascontiguousarray(np.broadcast_to(ada_b[:, None, ci * ncol:(ci + 1) * ncol], (depth, B + 1, ncol))))
           for ci in range(NCORES)]
    res = _run(build_ada(depth, ncol), ims)
    mod = np.concatenate([r["out"] for r in res], axis=2)
    mvec = lambda l, r, m: mod[l, r, m * D:(m + 1) * D]

    X = [np.concatenate([x[ci // 2, (ci % 2) * HL:(ci % 2 + 1) * HL], ctx[ci // 2, (ci % 2) * HC:(ci % 2 + 1) * HC]], 0)
         for ci in range(NCORES)]
    TPC = HL + HC
    blocks = [(t0, 512, 0) for t0 in range(0, HL, 512)] + [(HL, HC, 1)]
    kinds = [0] * (HL // 128) + [1] * (HC // 128)

    def to_batch(per_core):
        out = []
        for b in range(B):
            fb = np.empty((per_core[0].shape[0], NTOK), np.float32)
            for hf in range(2):
                fc = per_core[2 * b + hf]
                fb[:, LC + hf * HL:LC + (hf + 1) * HL] = fc[:, :HL]
                fb[:, hf * HC:(hf + 1) * HC] = fc[:, HL:]
            out.append(fb)
        return out

    def to_core(fb, hf):
        return np.ascontiguousarray(np.concatenate([fb[:, LC + hf * HL:LC + (hf + 1) * HL], fb[:, hf * HC:(hf + 1) * HC]], 1))

    nc_inproj = nc_ssd = nc_attn = nc_outproj = nc_gmlp = None
    cosT, sinT = _rope_tables(L, LC)
    for layer in range(depth):
        i = layer // 2
        XT = [np.ascontiguousarray(xc_.T) for xc_ in X]
        if layer % 2 == 0:
            if nc_inproj is None:
                nc_inproj = build_inproj(blocks, 26)
            W = np.zeros((D, 26 * 128), np.float32)
            W[:, :hyb_w_in.shape[2]] = hyb_w_in[i]
            ims = []
            for ci in range(NCORES):
                b = ci // 2
                modd = np.stack([np.stack([_colv(mvec(layer, r, 1)), _colv(mvec(layer, r, 0))]) for r in (b, B)])
                ims.append(dict(xT=XT[ci], modd=modd, gcol=_colv(norm1_g[layer]), wd=W, onesd=ones128))
            proj = [r["proj"] for r in _run(nc_inproj, ims)]
            Fb = to_batch(proj)
            if nc_ssd is None:
                nc_ssd = build_ssd(LC // 128, L // 128)
            cst = _ssd_consts()
            cw, cb = f32(ssd_conv_w[i]), f32(ssd_conv_b[i])
            ims = []
            for ci in range(NCORES):
                b, g = ci // 2, ci % 2
                rowsel = np.concatenate([1024 + g * 512 + np.arange(512), 2048 + g * 128 + np.arange(128),
                                         2304 + g * 128 + np.arange(128)])
                chsel = rowsel - 1024
                hs = slice(g * 8, (g + 1) * 8)
                rep8 = np.ascontiguousarray(np.broadcast_to(
                    np.stack([ssd_dt_bias[i][0, hs], ssd_dt_bias[i][1, hs], ssd_a_log[i][0, hs], ssd_a_log[i][1, hs],
                              ssd_d[i][hs]]).astype(np.float32)[None], (128, 5, 8)))
                ims.append(dict(xbcT=np.ascontiguousarray(Fb[b][rowsel]),
                                dtr=np.ascontiguousarray(Fb[b][2560 + g * 8:2560 + (g + 1) * 8].T),
                                convw=np.ascontiguousarray(cw[chsel]), convb=np.ascontiguousarray(cb[chsel].reshape(6, 128).T),
                                rep8=rep8, cst=cst))
            rs = _run(nc_ssd, ims)
            yfb = [np.ascontiguousarray(np.concatenate([rs[2 * b]["yf"], rs[2 * b + 1]["yf"]], 1).T) for b in range(B)]
            ybb = [np.ascontiguousarray(np.concatenate([rs[2 * b]["yb"], rs[2 * b + 1]["yb"]], 1).T) for b in range(B)]
            if nc_attn is None:
                nc_attn = build_attn(LC, L, 4)
            wqb_, wkvb_ = f32(mla_w_qb[i]), f32(mla_w_kvb[i])
            ims = []
            for ci in range(NCORES):
                b, hh = ci // 2, ci % 2
                heads = range(hh * 4, hh * 4 + 4)
                cat = lambda w, lo, hi, st: np.ascontiguousarray(np.concatenate([w[:, h * st + lo:h * st + hi] for h in heads], 1))
                ims.append(dict(cqT=np.ascontiguousarray(Fb[b][2576:2960]), ckvT=np.ascontiguousarray(Fb[b][2960:3216]),
                                kr2=np.ascontiguousarray(Fb[b][3216:3280].reshape(2, 32, NTOK)), cosd=cosT, sind=sinT,
                                wqn=cat(wqb_, 0, 128, 192), wq1=cat(wqb_, 128, 160, 192), wq2=cat(wqb_, 160, 192, 192),
                                wkn=cat(wkvb_, 0, 128, 256), wvd=cat(wkvb_, 128, 256, 256),
                                qg=_colv(mla_q_norm_g[i], 3), kvg=_colv(mla_kv_norm_g[i], 2), onesd=ones128))
            rs = _run(nc_attn, ims)
            attb = [np.concatenate([rs[2 * b]["att"], rs[2 * b + 1]["att"]], 0) for b in range(B)]
            del Fb
            if nc_outproj is None:
                nc_outproj = build_outproj(blocks)
            ims = []
            for ci in range(NCORES):
                b, hf = ci // 2, ci % 2
                modd = np.stack([_colv(mvec(layer, r, 2))[None] for r in (b, B)])
                ims.append(dict(yfT=to_core(yfb[b], hf), ybT=to_core(ybb[b], hf), zT=np.ascontiguousarray(proj[ci][0:1024]),
                                attT=to_core(attb[b], hf), xT=XT[ci], wd=f32(hyb_w_out[i]), ngd=_colv(ssd_norm_g[i]),
                                modd=modd, onesd=ones128))
            xo = [r["xo"] for r in _run(nc_outproj, ims)]
            del proj, yfb, ybb, attb
        else:
            if nc_gmlp is None:
                nc_gmlp = build_gmlp(kinds)
            reps = np.stack([_rows(gm_ln_g[i]), _rows(gm_ln_b[i])])
            bsr = np.ascontiguousarray(np.broadcast_to(f32(gm_bs[i])[None], (128, 8, 128)))
            wsT = np.ascontiguousarray(f32(gm_ws[i]).transpose(2, 0, 1))
            ims = []
            for ci in range(NCORES):
                b = ci // 2
                modd = np.stack([np.stack([_colv(mvec(layer, r, 1)), _colv(mvec(layer, r, 0)), _colv(mvec(layer, r, 2))])
                                 for r in (b, B)])
                ims.append(dict(xT=XT[ci], modd=modd, gcol=_colv(norm1_g[layer]), wind=f32(gm_w_in[i]), woutd=f32(gm_w_out[i]),
                                wsT=wsT, reps=reps, bsr=bsr, onesd=ones128))
            xo = [r["xo"] for r in _run(nc_gmlp, ims)]
        del XT
        final = layer == depth - 1
        nc_peer = build_peer(TPC // 128, kinds, final=final)
        k1T = np.ascontiguousarray(f32(peer_k1[layer]).transpose(2, 0, 1))
        k2T = np.ascontiguousarray(f32(peer_k2[layer]).transpose(2, 0, 1))
        iota16 = np.ascontiguousarray(np.broadcast_to(np.arange(16, dtype=np.float32)[None], (128, 16)))
        ut, vt_, wq_ = f32(peer_u[layer]), f32(peer_v[layer]), f32(peer_wq[layer])
        ims = []
        for ci in range(NCORES):
            b = ci // 2
            rep = np.stack([np.stack([_rows(norm2_g[layer]), _rows(mvec(layer, r, 4)), _rows(mvec(layer, r, 3)),
                                      _rows(mvec(layer, r, 5))]) for r in (b, B)])
            im = dict(x=np.ascontiguousarray(xo[ci].T), rep=rep, wqd=wq_, k1T=k1T, k2T=k2T, utab=ut, vtab=vt_,
                      identd=ident, iotad=iota16)
            if final:
                im["gfd"] = _rows(final_norm_g)
            ims.append(im)
        del xo
        X = [r["y"] for r in _run(nc_peer, ims)]
    out = np.empty((B, L, D), np.float32)
    for ci in range(NCORES):
        out[ci // 2, (ci % 2) * HL:(ci % 2 + 1) * HL] = X[ci][:HL]
    return out


def build_fused(HLB=8, depth=4):
    HL, HC = HLB * 512, 128
    TPC = HL + HC
    NTOK = 2 * TPC
    NKT = NTOK // 128
    ne, no = (depth + 1) // 2, depth // 2
    blocks = [(j * 512, 512, 0) for j in range(HLB)] + [(HL, HC, 1)]
    NBLK = HLB + 1
    p = Prog("fused")
    nc = p.nc
    D = p.dram
    xT0 = D("xT0", [1024, TPC])
    c2T = D("c2T", [128, 8, 2])
    ada_w = D("ada_w", [depth, 1024, 6144])
    ada_bc = D("ada_bc", [128, depth * 48])
    ada_br = D("ada_br", [depth, 128, 3072])
    n1g = D("n1g", [128, depth * 8])
    n2g_rep = D("n2g_rep", [depth, 128, 1024])
    m01d = D("m01", [128, 2])
    cstd = D("cst", [6, 128, 128])
    iotad = D("iotad", [128, 16])
    w_in = D("w_in", [ne, 1024, 3328])
    convw = D("convw", [ne, 768, 5])
    convb = D("convb", [ne, 128, 6])
    rep8 = D("rep8", [ne, 128, 5, 8])
    wqn_d = D("wqn", [ne, 384, 1024])
    wq1_d = D("wq1", [ne, 384, 256])
    wq2_d = D("wq2", [ne, 384, 256])
    wkn_d = D("wkn", [ne, 256, 1024])
    wv_d = D("wvd", [ne, 256, 1024])
    qg_d = D("qg", [ne, 128, 3])
    kvg_d = D("kvg", [ne, 128, 2])
    cosK = D("cosK", [32, NTOK])
    sinK = D("sinK", [32, NTOK])
    cosQ = D("cosQ", [32, TPC])
    sinQ = D("sinQ", [32, TPC])
    w_out = D("w_out", [ne, 2048, 1024])
    ssd_ng = D("ssd_ng", [ne, 128, 8])
    gm_win = D("gm_win", [no, 1024, 4096])
    gm_wout = D("gm_wout", [no, 2048, 1024])
    gm_wsT = D("gm_wsT", [no, 128, 8, 128])
    gm_reps = D("gm_reps", [no, 2, 128, 2048])
    gm_bsr = D("gm_bsr", [no, 128, 8, 128])
    pwq = D("pwq", [depth, 1024, 2048])
    pk1T = D("pk1T", [depth, 128, 8, 128])
    pk2T = D("pk2T", [depth, 128, 8, 128])
    putab = [D(f"putab{l_}", [16384, 1024]) for l_ in range(depth)]
    pvtab = [D(f"pvtab{l_}", [16384, 1024]) for l_ in range(depth)]
    gfd = D("gfd", [128, 1024])
    yout = D("y", [HL, 1024], kind="ExternalOutput")

    def scratch(name, shape):
        return nc.dram_tensor(name, list(shape), F32).ap()

    XS = [scratch("XSa", [1024, TPC]), scratch("XSb", [1024, TPC])]
    ZT = scratch("ZT", [1024, TPC])
    CQT = scratch("CQT", [384, TPC])
    ATT = scratch("ATT", [1024, TPC])
    BA = [scratch(f"BA{j}", [896, n]) for j, (_, n, _) in enumerate(blocks)]
    BB = [scratch(f"BB{j}", [1024, n]) for j, (_, n, _) in enumerate(blocks)]
    GA = [scratch(f"GA{j}", [2 * 896, n]) for j, (_, n, _) in enumerate(blocks)]
    GB = [scratch(f"GB{j}", [2 * 1024, n]) for j, (_, n, _) in enumerate(blocks)]
    NQ = 2 * HLB + 1
    qn = [512] * (2 * HLB) + [256]
    YB = [scratch(f"YB{q}", [512, qn[q]]) for q in range(NQ)]
    GY = [scratch(f"GY{q}", [1024, qn[q]]) for q in range(NQ)]
    UVB3 = nc.dram_tensor("UVB16", [16384, 2, 1024], BF16).ap()
    UVB = UVB3.rearrange("e w n -> e (w n)")
    rg = [[2 * b, 2 * b + 1] for b in range(NCORES // 2)]

    def allgather(src, dst, skey, dkey):
        p.cc(lambda e: e.collective_compute("AllGather", ALU.bypass, replica_groups=rg, ins=[src.opt()], outs=[dst.opt()]),
             reads=[skey], writes=[dkey])

    cs = p.sb([128, 6, 128], F32, "cs")
    p.load(cs[:], cstd.rearrange("c p n -> p c n"), "cs")
    ident, ones = cs[:, 0, :], cs[:, 5, :]
    onesT = p.sb([128, 128], F32, "ones")
    p.load(onesT[:], cstd[5], "ones")
    m01 = p.sb([128, 2], F32, "m01s")
    p.load(m01[:], m01d[:, :], "m01s")
    iota16 = p.sb([128, 16], F32, "iota16")
    p.load(iota16[:], iotad[:, :], "iota16")
    n1 = p.sb([128, depth * 8], F32, "n1")
    p.load(n1[:], n1g[:, :], "n1")
    s_sb = p.sb([128, 8, 2], F32, "s_sb")
    MC = p.sb([128, depth, 48, 2], F32, "MC")
    PS = [p.ps([128, 512], F32, f"ps{i}") for i in range(8)]

    p.push("adaph_")
    c_sb = p.sb([128, 8, 2], F32, "c_sb")
    p.load(c_sb[:], c2T[:, :, :], "c_sb")
    p.op("act", lambda e: e.activation(out=s_sb[:], in_=c_sb[:], func=AF.Silu), reads=["c_sb"], writes=["s_sb"])
    bc = p.sb([128, depth * 48], F32, "bc")
    p.load(bc[:], ada_bc[:, :], "bc")
    awt = [p.sb([128, 8, 512], F32, f"awt{i}") for i in range(2)]
    ai = 0
    for l in range(depth):
        pb = PS[l % 2]
        for cbk in range(12):
            wt = awt[ai % 2]
            ai += 1
            p.load(wt[:], ada_w[l].rearrange("(k p) n -> p k n", p=128)[:, :, cbk * 512:(cbk + 1) * 512], wt.name)
            for q in range(4):
                ck = cbk * 4 + q
                for kc in range(8):
                    p.op("pe", lambda e, pb=pb, wt=wt, q=q, ck=ck, kc=kc: e.matmul(
                        pb[:, ck * 2:ck * 2 + 2], lhsT=wt[:, kc, q * 128:(q + 1) * 128], rhs=s_sb[:, kc, :],
                        start=(kc == 0), stop=(kc == 7)), reads=[wt.name, "s_sb"], writes=[pb.name])
        p.op("dve", lambda e, pb=pb, l=l: e.tensor_tensor(
            out=MC[:, l], in0=pb[:, 0:96].rearrange("p (c s) -> p c s", s=2),
            in1=bc[:, l * 48:(l + 1) * 48].unsqueeze(2).to_broadcast([128, 48, 2]), op=ALU.add),
            reads=[pb.name, "bc"], writes=["MC"])
    p.pop()
    mcol = lambda l, m, s: MC[:, l, m * 8:(m + 1) * 8, s]

    def exch_rows(e, r, j):
        if e < 7:
            return GA[j][r * 896 + e * 128:r * 896 + (e + 1) * 128, :]
        return GB[j][r * 1024 + (e - 7) * 128:r * 1024 + (e - 6) * 128, :]

    def phase_inproj(l, i, src):
        p.push(f"e1{l}_")
        stg = [p.sb([128, 2048], F32, f"stg{k}") for k in range(2)]
        w_sb = p.sb([128, 8, 3328], BF16, "w_sb")
        load_w_bf16(p, w_sb, w_in[i], 8, 3328, stg)
        Acol = p.sb([128, 2, 8], F32, "Acol")
        for s in range(2):
            p.op("dve", lambda e, s=s: e.scalar_tensor_tensor(out=Acol[:, s, :], in0=mcol(l, 1, s), scalar=1.0,
                                                              in1=n1[:, l * 8:(l + 1) * 8], op0=ALU.add, op1=ALU.mult),
                 reads=["MC", "n1"], writes=["modc"])
        xts = [p.sb([128, 8, 512], F32, f"xt{k}") for k in range(2)]
        sq = p.sb([128, 8, 512], F32, "sq")
        rstd = p.sb([128, 512], F32, "rstd")
        hT = p.sb([128, 8, 512], BF16, "hT")
        outs = [p.sb([128, 512], F32, f"ot{k}") for k in range(4)]
        xv = src.rearrange("(k p) t -> p k t", p=128)
        oi = 0
        for bi, (t0, n, s) in enumerate(blocks):
            xt = xts[bi % 2]
            p.load(xt[:, :, 0:n], xv[:, :, t0:t0 + n], xt.name, skey="XS")
            fm_norm_mod(p, xt, n, onesT, sq, PS[0], rstd, Acol[:, s, :], mcol(l, 0, s), hT)
            for j in range(26):
                pb = PS[1 + j % 7]
                for k in range(8):
                    p.op("pe", lambda e, pb=pb, j=j, k=k: e.matmul(pb[:, 0:n], lhsT=w_sb[:, k, j * 128:(j + 1) * 128],
                                                                   rhs=hT[:, k, 0:n], start=(k == 0), stop=(k == 7)),
                         reads=["w_sb", "hT"], writes=[pb.name])
                ot = outs[oi % 4]
                oi += 1
                if oi % 2:
                    p.op("act", lambda e, pb=pb, ot=ot: e.activation(out=ot[:, 0:n], in_=pb[:, 0:n], func=AF.Copy),
                         reads=[pb.name], writes=[ot.name])
                else:
                    p.op("dve", lambda e, pb=pb, ot=ot: e.tensor_copy(out=ot[:, 0:n], in_=pb[:, 0:n]),
                         reads=[pb.name], writes=[ot.name])
                if j < 8:
                    dst, dk = ZT[j * 128:(j + 1) * 128, t0:t0 + n], "ZT"
                elif j < 11:
                    dst, dk = CQT[(j - 8) * 128:(j - 7) * 128, t0:t0 + n], "CQT"
                elif j < 18:
                    dst, dk = BA[bi][(j - 11) * 128:(j - 10) * 128, :], f"BA{bi}"
                else:
                    dst, dk = BB[bi][(j - 18) * 128:(j - 17) * 128, :], f"BB{bi}"
                p.dma(lambda e, dst=dst, ot=ot: e.dma_start(out=dst, in_=ot[:, 0:n]), reads=[ot.name], writes=[dk])
            allgather(BA[bi], GA[bi], f"BA{bi}", f"GA{bi}")
            allgather(BB[bi], GB[bi], f"BB{bi}", f"GB{bi}")
        p.pop()

    NCH = NTOK // 128
    LCH = NCH - 2

    def seq_segments(c, lo, hi):
        segs = []
        if c < 2:
            a, b = c * 128 - 2 + lo, c * 128 - 2 + hi
            t = a
            while t < b:
                r = t // 128
                e = min(b, (r + 1) * 128)
                segs.append((r, HLB, t - r * 128, e - r * 128, t - (c * 128 - 2)))
                t = e
        else:
            a, b = (c - 2) * 128 - 2 + lo, (c - 2) * 128 - 2 + hi
            t = a
            while t < b:
                r = t // HL
                j = (t - r * HL) // 512
                base = r * HL + j * 512
                e = min(b, base + 512)
                segs.append((r, j, t - base, e - base, t - ((c - 2) * 128 - 2)))
                t = e
        return segs

    def chunk_home(c):
        if c < 2:
            return (c, HLB, 0), (2 * HLB, c * 128)
        lc = c - 2
        t = lc * 128
        r = t // HL
        j = (t - r * HL) // 512
        return (r, j, t - r * HL - j * 512), (t // 512, t % 512)

    def phase_ssd(i):
        p.push(f"e2{i}_")
        mask01 = p.sb([128, 2, 128], F32, "mask01")
        p.op("dve", lambda e: e.tensor_copy(out=mask01[:], in_=cs[:, 1:3, :]), reads=["cs"], writes=["mask01"])
        cw = p.sb([128, 6, 5], F32, "cw")
        p.load(cw[:], convw[i].rearrange("(k p) j -> p k j", p=128), "cw")
        cb = p.sb([128, 6], F32, "cb")
        p.load(cb[:], convb[i], "cb")
        r8 = p.sb([128, 5, 8], F32, "r8")
        p.load(r8[:], rep8[i], "r8")
        aneg = p.sb([128, 2, 8], F32, "aneg")
        p.op("act", lambda e: e.activation(out=aneg[:], in_=r8[:, 2:4, :], func=AF.Exp), reads=["r8"], writes=["aneg"])
        p.op("dve", lambda e: e.tensor_scalar(out=aneg[:], in0=aneg[:], scalar1=-1.0, scalar2=None, op0=ALU.mult),
             reads=[], writes=["aneg"])
        wins = [[p.sb([128, 6, 132], F32, f"win{g}{k}") for k in range(2)] for g in range(2)]
        def two(shape, nm):
            return [p.sb(shape, F32, f"{nm}{q}") for q in range(2)]
        win_, dtT_, xc_, acc_ = two([128, 6, 132], "win"), two([16, 128], "dtT"), two([128, 6, 128], "xc"), two([128, 6, 128], "cacc")
        xs_tm_, btm_, sm_, da_b_ = two([128, 512], "xs_tm"), two([128, 128], "btm"), two([128, 128], "sm"), two([128, 8, 128], "da_b")
        xdw_, xd_, cbm_, arg_ = two([128, 512], "xdw"), two([128, 512], "xd"), two([128, 128], "cbm"), two([128, 8, 128], "arg")
        dec_, yt_, ytmp_ = two([128, 8, 128], "dec"), two([128, 512], "yt"), two([128, 512], "ytmp")
        yT_, yold_ = two([128, 4, 128], "yT"), two([128, 4, 128], "yold")
        H = p.sb([128, 512], F32, "H")
        P = PS
        it = 0
        for d in range(2):
            tri = cs[:, 1 + d, :]
            neg = cs[:, 3 + d, :]
            order = list(range(NCH)) if d == 0 else [1, 0] + list(range(NCH - 1, 1, -1))
            p.op("dve", lambda e: e.memset(H[:], 0.0), reads=[], writes=["H"])
            for c in order:
                par = it % 2
                KK = lambda nm: f"{nm}{par}"
                win, dtT, xc, acc = win_[par], dtT_[par], xc_[par], acc_[par]
                xs_tm, btm, sm, da_b = xs_tm_[par], btm_[par], sm_[par], da_b_[par]
                xdw, xd, cbm, arg = xdw_[par], xd_[par], cbm_[par], arg_[par]
                dec, yt, ytmp, yT, yold = dec_[par], yt_[par], ytmp_[par], yT_[par], yold_[par]
                s0, s1 = (0, 2) if c < 2 else (2, NCH)
                lo = 0 if c > s0 else 2
                hi = 132 if c < s1 - 1 else 130
                wg = [wins[g][it % 2] for g in range(2)]
                it += 1
                for g in range(2):
                    if lo or hi < 132:
                        p.op("dve", lambda e, w=wg[g]: e.memset(w[:], 0.0), reads=[], writes=[wg[g].name])
                    for (r, j, c0, c1, d0) in seq_segments(c, lo, hi):
                        if g == 0:
                            srcs = [(GA[j][r * 896:r * 896 + 768, c0:c1], 0, 6, f"GA{j}")]
                        else:
                            srcs = [(GA[j][r * 896 + 768:r * 896 + 896, c0:c1], 0, 1, f"GA{j}"),
                                    (GB[j][r * 1024:r * 1024 + 640, c0:c1], 1, 6, f"GB{j}")]
                        for (sap, k0, k1, sk) in srcs:
                            p.load(wg[g][:, k0:k1, d0:d0 + (c1 - c0)], sap.rearrange("(k p) t -> p k t", p=128), wg[g].name, skey=sk)
                p.op("dve", lambda e, w0=wg[0]: e.tensor_scalar(out=win[:], in0=w0[:], scalar1=m01[:, 0:1], scalar2=None, op0=ALU.mult),
                     reads=[wg[0].name, "m01s"], writes=[KK("win")])
                p.op("dve", lambda e, w1=wg[1]: e.scalar_tensor_tensor(out=win[:], in0=w1[:], scalar=m01[:, 1:2], in1=win[:],
                                                                      op0=ALU.mult, op1=ALU.add),
                     reads=[wg[1].name, "m01s"], writes=[KK("win")])
                (hr, hj, hc0), (yq, yc0) = chunk_home(c)
                p.load(dtT[:], GB[hj][hr * 1024 + 896 + 64:hr * 1024 + 896 + 80, hc0:hc0 + 128], KK("dtT"), skey=f"GB{hj}")
                for k in range(6):
                    for j in range(5):
                        if j == 0:
                            p.op("dve", lambda e, k=k, j=j: e.tensor_scalar(
                                out=acc[:, k, :], in0=win[:, k, j:j + 128], scalar1=cw[:, k, j:j + 1], scalar2=None, op0=ALU.mult),
                                reads=[KK("win"), "cw"], writes=[KK("cacc")])
                        else:
                            p.op("dve", lambda e, k=k, j=j: e.scalar_tensor_tensor(
                                out=acc[:, k, :], in0=win[:, k, j:j + 128], scalar=cw[:, k, j:j + 1], in1=acc[:, k, :],
                                op0=ALU.mult, op1=ALU.add), reads=[KK("win"), "cw"], writes=[KK("cacc")])
                for k in range(6):
                    p.op("act", lambda e, k=k: e.activation(out=xc[:, k, :], in_=acc[:, k, :], func=AF.Silu, bias=cb[:, k:k + 1]),
                         reads=[KK("cacc"), "cb"], writes=[KK("xc")])
                for k in range(4):
                    p.op("pe", lambda e, k=k: e.transpose(out=P[0][:, k * 128:(k + 1) * 128], in_=xc[:, k, :], identity=ident),
                         reads=[KK("xc"), "cs"], writes=["ps0"])
                p.op("act", lambda e: e.activation(out=xs_tm[:], in_=P[0][:], func=AF.Copy), reads=["ps0"], writes=[KK("xs_tm")])
                p.op("pe", lambda e: e.transpose(out=P[1][:, 0:128], in_=xc[:, 4, :], identity=ident),
                     reads=[KK("xc"), "cs"], writes=["ps1"])
                p.op("dve", lambda e: e.tensor_copy(out=btm[:], in_=P[1][:, 0:128]), reads=["ps1"], writes=[KK("btm")])
                p.op("pe", lambda e: e.transpose(out=P[1][:, 384:400], in_=dtT[:], identity=ident[0:16, 0:16]),
                     reads=[KK("dtT"), "cs"], writes=["ps1"])
                p.op("dve", lambda e: e.tensor_scalar(out=sm[:, 88:96], in0=P[1][:, 384:392], scalar1=m01[:, 0:1], scalar2=None,
                                                      op0=ALU.mult), reads=["ps1", "m01s"], writes=[KK("sm")])
                p.op("dve", lambda e: e.scalar_tensor_tensor(out=sm[:, 88:96], in0=P[1][:, 392:400], scalar=m01[:, 1:2],
                                                             in1=sm[:, 88:96], op0=ALU.mult, op1=ALU.add),
                     reads=["ps1", "m01s"], writes=[KK("sm")])
                p.op("dve", lambda e, d=d: e.tensor_tensor(out=sm[:, 16:24], in0=sm[:, 88:96], in1=r8[:, d, :], op=ALU.add),
                     reads=["r8"], writes=[KK("sm")])
                p.op("act", lambda e: e.activation(out=sm[:, 16:24], in_=sm[:, 16:24], func=AF.Exp), reads=[], writes=[KK("sm")])
                p.op("act", lambda e: e.activation(out=sm[:, 0:8], in_=sm[:, 16:24], func=AF.Ln, bias=1.0), reads=[], writes=[KK("sm")])
                p.op("dve", lambda e, d=d: e.tensor_tensor(out=sm[:, 8:16], in0=sm[:, 0:8], in1=aneg[:, d, :], op=ALU.mult),
                     reads=["aneg"], writes=[KK("sm")])
                p.op("pe", lambda e, tri=tri: e.matmul(P[1][:, 128:136], lhsT=tri, rhs=sm[:, 8:16], start=True, stop=True),
                     reads=[KK("sm"), "cs"], writes=["ps1"])
                p.op("pe", lambda e: e.matmul(P[1][:, 136:144], lhsT=ones, rhs=sm[:, 8:16], start=True, stop=True),
                     reads=[KK("sm"), "cs"], writes=["ps1"])
                p.op("dve", lambda e: e.tensor_copy(out=sm[:, 24:40], in_=P[1][:, 128:144]), reads=["ps1"], writes=[KK("sm")])
                p.op("dve", lambda e: e.tensor_tensor(out=sm[:, 40:48], in0=sm[:, 32:40], in1=sm[:, 24:32], op=ALU.subtract),
                     reads=[], writes=[KK("sm")])
                p.op("act", lambda e: e.activation(out=sm[:, 48:72], in_=sm[:, 24:48], func=AF.Exp), reads=[], writes=[KK("sm")])
                p.op("dve", lambda e: e.tensor_scalar(out=sm[:, 72:80], in0=sm[:, 24:32], scalar1=-1.0, scalar2=None, op0=ALU.mult),
                     reads=[], writes=[KK("sm")])
                p.op("dve", lambda e: e.tensor_tensor(out=sm[:, 80:88], in0=sm[:, 0:8], in1=sm[:, 64:72], op=ALU.mult),
                     reads=[], writes=[KK("sm")])
                ecum, cdec = sm[:, 48:56], sm[:, 56:64]
                xs3 = xs_tm[:].rearrange("p (h q) -> p h q", h=8)
                p.op("dve", lambda e: e.tensor_tensor(out=xdw[:].rearrange("p (h q) -> p h q", h=8), in0=xs3,
                                                      in1=sm[:, 80:88].unsqueeze(2).to_broadcast([128, 8, 64]), op=ALU.mult),
                     reads=[KK("xs_tm"), KK("sm")], writes=[KK("xdw")])
                p.op("dve", lambda e: e.tensor_tensor(out=xd[:].rearrange("p (h q) -> p h q", h=8), in0=xs3,
                                                      in1=sm[:, 0:8].unsqueeze(2).to_broadcast([128, 8, 64]), op=ALU.mult),
                     reads=[KK("xs_tm"), KK("sm")], writes=[KK("xd")])
                p.op("dve", lambda e: e.tensor_copy(out=da_b[:], in_=sm[:, 8:16].unsqueeze(2).to_broadcast([128, 8, 128])),
                     reads=[KK("sm")], writes=[KK("da_b")])
                p.op("pe", lambda e: e.matmul(P[2][:], lhsT=btm[:], rhs=xdw[:], start=True, stop=True),
                     reads=[KK("btm"), KK("xdw")], writes=["ps2"])
                p.op("pe", lambda e: e.matmul(P[3][:], lhsT=xc[:, 5, :], rhs=H[:], start=True, stop=True),
                     reads=[KK("xc"), "H"], writes=["ps3"])
                p.op("pe", lambda e: e.matmul(P[1][:, 256:384], lhsT=xc[:, 4, :], rhs=xc[:, 5, :], start=True, stop=True),
                     reads=[KK("xc")], writes=["ps1"])
                p.op("dve", lambda e, d=d: e.tensor_tensor(out=cbm[:], in0=P[1][:, 256:384], in1=mask01[:, d, :], op=ALU.mult),
                     reads=["ps1", "mask01"], writes=[KK("cbm")])
                for hb in range(2):
                    pb = P[4 + hb]
                    for hh in range(4):
                        h = hb * 4 + hh
                        p.op("pe", lambda e, pb=pb, hh=hh, h=h, tri=tri: e.matmul(
                            pb[:, hh * 128:(hh + 1) * 128], lhsT=da_b[:, h, :], rhs=tri, start=True, stop=True),
                            reads=[KK("da_b"), "cs"], writes=[pb.name])
                    p.op("dve", lambda e, pb=pb, hb=hb, neg=neg: e.tensor_tensor(
                        out=arg[:, hb * 4:(hb + 1) * 4, :], in0=pb[:].rearrange("p (h t) -> p h t", h=4),
                        in1=neg.unsqueeze(1).to_broadcast([128, 4, 128]), op=ALU.add),
                        reads=[pb.name, "cs"], writes=[KK("arg")])
                for h in range(8):
                    p.op("act", lambda e, h=h: e.activation(out=dec[:, h, :], in_=arg[:, h, :], func=AF.Exp, bias=sm[:, 72 + h:73 + h]),
                         reads=[KK("arg"), KK("sm")], writes=[KK("dec")])
                p.op("dve", lambda e: e.tensor_tensor(out=dec[:], in0=dec[:], in1=cbm[:].unsqueeze(1).to_broadcast([128, 8, 128]),
                                                      op=ALU.mult), reads=[KK("cbm")], writes=[KK("dec")])
                for h in range(8):
                    p.op("pe", lambda e, h=h: e.matmul(P[6][:, h * 64:(h + 1) * 64], lhsT=dec[:, h, :], rhs=xd[:, h * 64:(h + 1) * 64],
                                                       start=True, stop=True), reads=[KK("dec"), KK("xd")], writes=["ps6"])
                p.op("dve", lambda e: e.tensor_tensor(out=ytmp[:].rearrange("p (h q) -> p h q", h=8),
                                                      in0=P[3][:].rearrange("p (h q) -> p h q", h=8),
                                                      in1=ecum.unsqueeze(2).to_broadcast([128, 8, 64]), op=ALU.mult),
                     reads=["ps3", KK("sm")], writes=[KK("ytmp")])
                p.op("dve", lambda e: e.tensor_tensor(out=yt[:], in0=ytmp[:], in1=P[6][:], op=ALU.add),
                     reads=[KK("ytmp"), "ps6"], writes=[KK("yt")])
                if d == 0:
                    p.op("dve", lambda e: e.tensor_tensor(out=ytmp[:].rearrange("p (h q) -> p h q", h=8), in0=xs3,
                                                          in1=r8[:, 4, :].unsqueeze(2).to_broadcast([128, 8, 64]), op=ALU.mult),
                         reads=[KK("xs_tm"), "r8"], writes=[KK("ytmp")])
                    p.op("dve", lambda e: e.tensor_tensor(out=yt[:], in0=yt[:], in1=ytmp[:], op=ALU.add),
                         reads=[KK("ytmp")], writes=[KK("yt")])
                for k in range(4):
                    p.op("pe", lambda e, k=k: e.transpose(out=P[7][:, k * 128:(k + 1) * 128], in_=yt[:, k * 128:(k + 1) * 128],
                                                          identity=ident), reads=[KK("yt"), "cs"], writes=["ps7"])
                ydst = YB[yq].rearrange("(k p) t -> p k t", p=128)[:, :, yc0:yc0 + 128]
                if d == 0:
                    p.op("act", lambda e: e.activation(out=yT[:], in_=P[7][:].rearrange("p (k t) -> p k t", k=4), func=AF.Copy),
                         reads=["ps7"], writes=[KK("yT")])
                else:
                    p.load(yold[:], ydst, KK("yold"), skey=f"YB{yq}")
                    p.op("dve", lambda e: e.tensor_tensor(out=yT[:], in0=P[7][:].rearrange("p (k t) -> p k t", k=4), in1=yold[:],
                                                          op=ALU.add), reads=["ps7", KK("yold")], writes=[KK("yT")])
                p.dma(lambda e, ydst=ydst: e.dma_start(out=ydst, in_=yT[:]), reads=[KK("yT")], writes=[f"YB{yq}"])
                p.op("dve", lambda e: e.tensor_tensor(out=H[:].rearrange("p (h q) -> p h q", h=8),
                                                      in0=H[:].rearrange("p (h q) -> p h q", h=8),
                                                      in1=cdec.unsqueeze(2).to_broadcast([128, 8, 64]), op=ALU.mult),
                     reads=[KK("sm")], writes=["H"])
                p.op("dve", lambda e: e.tensor_tensor(out=H[:], in0=H[:], in1=P[2][:], op=ALU.add), reads=["ps2"], writes=["H"])
        for q in range(NQ):
            allgather(YB[q], GY[q], f"YB{q}", f"GY{q}")
        p.pop()
    return _fused_rest(locals())


class _NS:
    def __init__(self, d):
        self.__dict__.update(d)


def _fused_rest(L):
    v = _NS(L)
    p, nc, PS, cs, ident, ones, onesT, m01, MC, mcol, n1 = v.p, v.nc, v.PS, v.cs, v.ident, v.ones, v.onesT, v.m01, v.MC, v.mcol, v.n1
    HLB, HL, HC, TPC, NTOK, NKT, blocks, depth = v.HLB, v.HL, v.HC, v.TPC, v.NTOK, v.NKT, v.blocks, v.depth
    GA, GB, GY, ZT, CQT, ATT, XS = v.GA, v.GB, v.GY, v.ZT, v.CQT, v.ATT, v.XS
    scale = (128 + 64) ** -0.5

    def phase_attn(i):
        NH = 8
        p.push(f"e3{i}_")
        onesb = p.sb([128, 128], BF16, "onesb")
        p.op("dve", lambda e: e.tensor_copy(out=onesb[:], in_=onesT[:]), reads=["ones"], writes=["onesb"])
        qg = p.sb([128, 3], F32, "qgs")
        p.load(qg[:], v.qg_d[i], "qgs")
        kvg = p.sb([128, 2], F32, "kvgs")
        p.load(kvg[:], v.kvg_d[i], "kvgs")
        stg = [p.sb([128, 2048], F32, f"stg{k}") for k in range(2)]
        wqn = p.sb([128, 3, NH * 128], BF16, "wqn_s")
        wq1 = p.sb([128, 3, NH * 32], BF16, "wq1_s")
        wq2 = p.sb([128, 3, NH * 32], BF16, "wq2_s")
        wkn = p.sb([128, 2, NH * 128], BF16, "wkn_s")
        wv = p.sb([128, 2, NH * 128], BF16, "wv_s")
        load_w_bf16(p, wqn, v.wqn_d[i], 3, NH * 128, stg)
        load_w_bf16(p, wq1, v.wq1_d[i], 3, NH * 32, stg)
        load_w_bf16(p, wq2, v.wq2_d[i], 3, NH * 32, stg)
        load_w_bf16(p, wkn, v.wkn_d[i], 2, NH * 128, stg)
        load_w_bf16(p, wv, v.wv_d[i], 2, NH * 128, stg)
        ckvn = p.sb([128, 2, NTOK], BF16, "ckvn")
        KR = p.sb([128, NTOK], BF16, "KR")
        wq12 = p.sb([128, 3, NH, 64], BF16, "wq12")
        wq21 = p.sb([128, 3, NH, 64], BF16, "wq21")
        KnT = p.sb([128, NTOK], BF16, "KnT")
        Vh = p.sb([128, NKT, 128], BF16, "Vh")
        cqn = p.sb([128, 3, 512], BF16, "cqn")
        xin = p.sb([128, 3, 512], F32, "xin")
        sq = p.sb([128, 3, 512], F32, "sq")
        rstd = p.sb([128, 512], F32, "rstd")
        XA, XB, CS, SN, r1, t1, t2 = [p.sb([64, 512], F32, nm) for nm in ("XA", "XB", "CS", "SN", "r1", "t1", "t2")]
        QnT = p.sb([128, 512], BF16, "QnT")
        QR = p.sb([128, 512], BF16, "QR")
        Pts = [p.sb([128, 512], BF16, f"Pt{k}") for k in range(3)]
        osb = p.sb([128, 512], F32, "osb")
        mx = p.sb([128, 8], F32, "mx")
        P = PS
        p.op("dve", lambda e: e.memset(mx[:], 0.0), reads=[], writes=["mx"])
        p.op("dve", lambda e: e.memset(KR[:], 1.0), reads=[], writes=["KR"])
        w1v = wq1[:].rearrange("p k (h c) -> p k h c", h=NH)
        w2v = wq2[:].rearrange("p k (h c) -> p k h c", h=NH)
        p.op("dve", lambda e: e.tensor_copy(out=wq12[:, :, :, 0:32], in_=w1v), reads=["wq1_s"], writes=["wq12"])
        p.op("dve", lambda e: e.tensor_copy(out=wq12[:, :, :, 32:64], in_=w2v), reads=["wq2_s"], writes=["wq12"])
        p.op("dve", lambda e: e.tensor_copy(out=wq21[:, :, :, 0:32], in_=w2v), reads=["wq2_s"], writes=["wq21"])
        p.op("dve", lambda e: e.tensor_copy(out=wq21[:, :, :, 32:64], in_=w1v), reads=["wq1_s"], writes=["wq21"])

        def rope64(n, out_ap, pb):
            p.op("dve", lambda e: e.tensor_tensor(out=t1[:, 0:n], in0=XA[:, 0:n], in1=CS[:, 0:n], op=ALU.mult), reads=["XA", "CS"], writes=["t1"])
            p.op("dve", lambda e: e.tensor_tensor(out=t2[:, 0:n], in0=XB[:, 0:n], in1=SN[:, 0:n], op=ALU.mult), reads=["XB", "SN"], writes=["t2"])
            p.op("dve", lambda e: e.tensor_tensor(out=r1[:, 0:n], in0=t1[:, 0:n], in1=t2[:, 0:n], op=ALU.add), reads=["t1", "t2"], writes=["r1"])
            if out_ap is not None:
                p.op("act", lambda e: e.activation(out=out_ap, in_=r1[:, 0:n], func=AF.Copy), reads=["r1"], writes=[out_ap.name])
            p.op("act", lambda e: e.activation(out=t1[:, 0:n], in_=r1[:, 0:n], func=AF.Square), reads=["r1"], writes=["t1"])
            p.op("pe", lambda e: e.matmul(pb[:, 0:n], lhsT=onesT[0:64, :], rhs=t1[:, 0:n], start=False, stop=True),
                 reads=["t1", "ones"], writes=[pb.name])

        def load_tables(cosd, sind, t0, n):
            p.load(CS[0:32, 0:n], cosd[:, t0:t0 + n], "CS")
            p.load(CS[32:64, 0:n], cosd[:, t0:t0 + n], "CS")
            p.load(SN[0:32, 0:n], sind[:, t0:t0 + n], "SN")
            p.load(SN[32:64, 0:n], sind[:, t0:t0 + n], "SN")
            p.op("dve", lambda e: e.tensor_scalar(out=SN[0:32, 0:n], in0=SN[0:32, 0:n], scalar1=-1.0, scalar2=None, op0=ALU.mult),
                 reads=[], writes=["SN"])

        def running_max(pb, n, col):
            p.op("dve", lambda e: e.tensor_reduce(out=mx[:, 2:3], in_=pb[:, 0:n], axis=AX.X, op=ALU.max), reads=[pb.name], writes=["mx"])
            p.op("dve", lambda e: e.tensor_tensor(out=mx[:, col:col + 1], in0=mx[:, col:col + 1], in1=mx[:, 2:3], op=ALU.max),
                 reads=[], writes=["mx"])

        def norm_block(src_ap, skey, kc, gcol, dst_fn, inv, n):
            p.load(xin[:, 0:kc, 0:n], src_ap, "xin", skey=skey)
            fm_rstd(p, xin, (0, kc), n, onesT, sq, P[5], rstd, inv)
            p.op("dve", lambda e: e.tensor_tensor(out=sq[:, 0:kc, 0:n], in0=xin[:, 0:kc, 0:n],
                                                  in1=rstd[:, 0:n].unsqueeze(1).to_broadcast([128, kc, n]), op=ALU.mult),
                 reads=["xin", "rstd"], writes=["sq"])
            for k in range(kc):
                dst = dst_fn(k)
                p.op("act", lambda e, k=k, dst=dst: e.activation(out=dst, in_=sq[:, k, 0:n], func=AF.Identity, scale=gcol[:, k:k + 1]),
                     reads=["sq", gcol.name], writes=[dst.name])

        kblocks = [(0, HLB, 128), (1, HLB, 128)] + [(r, j, 512) for r in range(2) for j in range(HLB)]
        koff = 0
        for (r, j, n) in kblocks:
            t0 = koff
            koff += n
            norm_block(GB[j][r * 1024 + 640:r * 1024 + 896, :].rearrange("(k p) t -> p k t", p=128), f"GB{j}", 2, kvg,
                       lambda k, t0=t0, n=n: ckvn[:, k, t0:t0 + n], 1.0 / 256, n)
            load_tables(v.cosK, v.sinK, t0, n)
            krb = r * 1024 + 896
            p.load(XA[0:32, 0:n], GB[j][krb:krb + 32, :], "XA", skey=f"GB{j}")
            p.load(XA[32:64, 0:n], GB[j][krb + 32:krb + 64, :], "XA", skey=f"GB{j}")
            p.load(XB[0:32, 0:n], GB[j][krb + 32:krb + 64, :], "XB", skey=f"GB{j}")
            p.load(XB[32:64, 0:n], GB[j][krb:krb + 32, :], "XB", skey=f"GB{j}")
            p.op("act", lambda e: e.activation(out=t1[:, 0:n], in_=XA[:, 0:n], func=AF.Copy, scale=0.0), reads=["XA"], writes=["t1"])
            p.op("pe", lambda e: e.matmul(P[6][:, 0:n], lhsT=onesT[0:64, :], rhs=t1[:, 0:n], start=True, stop=False),
                 reads=["t1", "ones"], writes=["ps6"])
            rope64(n, KR[0:64, t0:t0 + n], P[6])
            running_max(P[6], n, 0)
        kb512 = [(t0, min(512, NTOK - t0)) for t0 in range(0, NTOK, 512)]
        qblocks = [(t0, n, NKT if s == 0 else 2) for (t0, n, s) in blocks]
        pi = 0
        cqv = CQT.rearrange("(k p) t -> p k t", p=128)
        for h in range(NH):
            p.op("dve", lambda e: e.memset(mx[:, 1:2], 0.0), reads=[], writes=["mx"])
            for (t0, n) in kb512:
                pb = P[5]
                for k in range(2):
                    p.op("pe", lambda e, k=k: e.matmul(pb[:, 0:n], lhsT=wkn[:, k, h * 128:(h + 1) * 128], rhs=ckvn[:, k, t0:t0 + n],
                                                       start=(k == 0), stop=(k == 1)), reads=["wkn_s", "ckvn"], writes=[pb.name])
                p.op("act", lambda e: e.activation(out=KnT[:, t0:t0 + n], in_=pb[:, 0:n], func=AF.Copy), reads=[pb.name], writes=["KnT"])
                p.op("act", lambda e: e.activation(out=sq[:, 0, 0:n], in_=pb[:, 0:n], func=AF.Square), reads=[pb.name], writes=["sq"])
                p.op("pe", lambda e: e.matmul(P[6][:, 0:n], lhsT=onesT[:], rhs=sq[:, 0, 0:n], start=True, stop=True),
                     reads=["sq", "ones"], writes=["ps6"])
                running_max(P[6], n, 1)
                pv = P[7]
                nt = n // 128
                for j in range(nt):
                    for k in range(2):
                        p.op("pe", lambda e, j=j, k=k: e.matmul(
                            pv[:, j * 128:(j + 1) * 128], lhsT=ckvn[:, k, t0 + j * 128:t0 + (j + 1) * 128],
                            rhs=wv[:, k, h * 128:(h + 1) * 128], start=(k == 0), stop=(k == 1)),
                            reads=["wv_s", "ckvn"], writes=["ps7"])
                p.op("dve", lambda e, nt=nt: e.tensor_copy(out=Vh[:, t0 // 128:t0 // 128 + nt, :],
                                                          in_=pv[:, 0:nt * 128].rearrange("p (j d) -> p j d", j=nt)),
                     reads=["ps7"], writes=["Vh"])
            p.op("dve", lambda e: e.tensor_tensor(out=mx[:, 4:5], in0=mx[:, 0:1], in1=mx[:, 1:2], op=ALU.add), reads=[], writes=["mx"])
            p.op("act", lambda e: e.activation(out=mx[:, 4:5], in_=mx[:, 4:5], func=AF.Sqrt), reads=[], writes=["mx"])
            p.op("dve", lambda e: e.tensor_scalar(out=mx[:, 3:4], in0=mx[:, 4:5], scalar1=-1.0, scalar2=None, op0=ALU.mult),
                 reads=[], writes=["mx"])
            for (q0, n, nkt) in qblocks:
                norm_block(cqv[:, :, q0:q0 + n], "CQT", 3, qg, lambda k, n=n: cqn[:, k, 0:n], 1.0 / 384, n)
                pb = P[5]
                for k in range(3):
                    p.op("pe", lambda e, k=k: e.matmul(pb[:, 0:n], lhsT=wqn[:, k, h * 128:(h + 1) * 128], rhs=cqn[:, k, 0:n],
                                                       start=(k == 0), stop=(k == 2)), reads=["wqn_s", "cqn"], writes=[pb.name])
                p.op("act", lambda e: e.activation(out=QnT[:, 0:n], in_=pb[:, 0:n], func=AF.Copy), reads=[pb.name], writes=["QnT"])
                p.op("act", lambda e: e.activation(out=sq[:, 0, 0:n], in_=pb[:, 0:n], func=AF.Square), reads=[pb.name], writes=["sq"])
                for (wsb, pq, dst) in ((wq12, P[6], XA), (wq21, P[7], XB)):
                    for k in range(3):
                        p.op("pe", lambda e, k=k, wsb=wsb, pq=pq: e.matmul(
                            pq[0:64, 0:n], lhsT=wsb[:, k, h, :], rhs=cqn[:, k, 0:n],
                            start=(k == 0), stop=(k == 2)), reads=[wsb.name, "cqn"], writes=[pq.name])
                    p.op("dve", lambda e, pq=pq, dst=dst: e.tensor_copy(out=dst[:, 0:n], in_=pq[0:64, 0:n]), reads=[pq.name], writes=[dst.name])
                load_tables(v.cosQ, v.sinQ, q0, n)
                p.op("pe", lambda e: e.matmul(P[5][:, 0:n], lhsT=onesT[:], rhs=sq[:, 0, 0:n], start=True, stop=False),
                     reads=["sq", "ones"], writes=["ps5"])
                rope64(n, None, P[5])
                p.op("act", lambda e: e.activation(out=rstd[:, 0:n], in_=P[5][:, 0:n], func=AF.Sqrt), reads=["ps5"], writes=["rstd"])
                p.op("dve", lambda e: e.tensor_scalar(out=QR[:, 0:n], in0=rstd[:, 0:n], scalar1=mx[:, 3:4], scalar2=None,
                                                      op0=ALU.mult), reads=["rstd", "mx"], writes=["QR"])
                p.op("act", lambda e: e.activation(out=QR[0:64, 0:n], in_=r1[:, 0:n], func=AF.Copy), reads=["r1"], writes=["QR"])
                po, pd = P[3], P[4]
                slots = {}

                def qk(kt):
                    nonlocal pi
                    ps, pt = P[pi % 3], Pts[pi % 3]
                    pi += 1
                    slots[kt] = (ps, pt)
                    ks = slice(kt * 128, (kt + 1) * 128)
                    p.op("pe", lambda e: e.matmul(ps[:, 0:n], lhsT=KnT[:, ks], rhs=QnT[:, 0:n], start=True, stop=False),
                         reads=["KnT", "QnT"], writes=[ps.name])
                    p.op("pe", lambda e: e.matmul(ps[:, 0:n], lhsT=KR[0:65, ks], rhs=QR[0:65, 0:n], start=False, stop=True),
                         reads=["KR", "QR"], writes=[ps.name])

                qk(0)
                for kt in range(nkt):
                    ps, pt = slots.pop(kt)
                    p.op("act", lambda e, ps=ps, pt=pt: e.activation(out=pt[:, 0:n], in_=ps[:, 0:n], func=AF.Exp, scale=scale),
                         reads=[ps.name], writes=[pt.name])
                    if kt + 1 < nkt:
                        qk(kt + 1)
                    p.op("pe", lambda e, pt=pt, kt=kt: e.matmul(po[:, 0:n], lhsT=Vh[:, kt, :], rhs=pt[:, 0:n],
                                                                start=(kt == 0), stop=(kt == nkt - 1)),
                         reads=["Vh", pt.name], writes=[po.name])
                    p.op("pe", lambda e, pt=pt, kt=kt: e.matmul(pd[:, 0:n], lhsT=onesb[:], rhs=pt[:, 0:n],
                                                                start=(kt == 0), stop=(kt == nkt - 1)),
                         reads=["onesb", pt.name], writes=[pd.name])
                p.op("dve", lambda e: e.reciprocal(out=rstd[:, 0:n], in_=pd[:, 0:n]), reads=[pd.name], writes=["rstd"])
                p.op("dve", lambda e: e.tensor_tensor(out=osb[:, 0:n], in0=po[:, 0:n], in1=rstd[:, 0:n], op=ALU.mult),
                     reads=[po.name, "rstd"], writes=["osb"])
                p.dma(lambda e, q0=q0, n=n: e.dma_start(out=ATT[h * 128:(h + 1) * 128, q0:q0 + n], in_=osb[:, 0:n]),
                      reads=["osb"], writes=["ATT"])
        p.pop()

    def phase_outproj(l, i, src, dst):
        p.push(f"e4{l}_")
        ng = p.sb([128, 8], F32, "ng")
        p.load(ng[:], v.ssd_ng[i], "ng")
        stg = [p.sb([128, 2048], F32, f"stg{k}") for k in range(2)]
        w_sb = p.sb([128, 16, 1024], BF16, "w_sb")
        load_w_bf16(p, w_sb, v.w_out[i], 16, 1024, stg)
        yf = p.sb([128, 8, 512], F32, "yf")
        yb = p.sb([128, 8, 512], F32, "yb")
        zt = p.sb([128, 8, 512], F32, "zt")
        at = p.sb([128, 8, 512], F32, "at")
        xt = p.sb([128, 8, 512], F32, "xt")
        sq = p.sb([128, 8, 512], F32, "sq")
        rs = [p.sb([128, 512], F32, f"rs{k}") for k in range(2)]
        cat = p.sb([128, 16, 512], BF16, "cat")
        vv = lambda a: a.rearrange("(k p) t -> p k t", p=128)
        for bi, (t0, n, s) in enumerate(blocks):
            if s == 0:
                ya, yk0 = vv(GY[bi])[:, :, 0:n], f"GY{bi}"
                ybb, yk1 = vv(GY[HLB + bi])[:, :, 0:n], f"GY{HLB + bi}"
            else:
                ya, yk0 = vv(GY[2 * HLB])[:, :, 0:128], f"GY{2 * HLB}"
                ybb, yk1 = vv(GY[2 * HLB])[:, :, 128:256], f"GY{2 * HLB}"
            p.load(yf[:, :, 0:n], ya, "yf", skey=yk0)
            p.load(yb[:, :, 0:n], ybb, "yb", skey=yk1)
            p.load(zt[:, :, 0:n], vv(ZT)[:, :, t0:t0 + n], "zt", skey="ZT")
            p.load(at[:, :, 0:n], vv(ATT)[:, :, t0:t0 + n], "at", skey="ATT")
            p.load(xt[:, :, 0:n], vv(src)[:, :, t0:t0 + n], "xt", skey="XS")
            p.op("dve", lambda e: e.tensor_scalar(out=yf[:, :, 0:n], in0=yf[:, :, 0:n], scalar1=m01[:, 0:1], scalar2=None, op0=ALU.mult),
                 reads=["m01s"], writes=["yf"])
            p.op("dve", lambda e: e.scalar_tensor_tensor(out=yf[:, :, 0:n], in0=yb[:, :, 0:n], scalar=m01[:, 1:2], in1=yf[:, :, 0:n],
                                                         op0=ALU.mult, op1=ALU.add), reads=["yb", "m01s"], writes=["yf"])
            p.op("act", lambda e: e.activation(out=zt[:, :, 0:n], in_=zt[:, :, 0:n], func=AF.Silu), reads=[], writes=["zt"])
            p.op("dve", lambda e: e.tensor_tensor(out=yf[:, :, 0:n], in0=yf[:, :, 0:n], in1=zt[:, :, 0:n], op=ALU.mult),
                 reads=["zt"], writes=["yf"])
            for g in range(2):
                fm_rstd(p, yf, (g * 4, g * 4 + 4), n, onesT, sq, PS[g], rs[g], 1.0 / 512)
            for g in range(2):
                p.op("dve", lambda e, g=g: e.tensor_tensor(out=sq[:, g * 4:g * 4 + 4, 0:n], in0=yf[:, g * 4:g * 4 + 4, 0:n],
                                                           in1=rs[g][:, 0:n].unsqueeze(1).to_broadcast([128, 4, n]), op=ALU.mult),
                     reads=["yf", rs[g].name], writes=["sq"])
            for k in range(8):
                p.op("act", lambda e, k=k: e.activation(out=cat[:, k, 0:n], in_=sq[:, k, 0:n], func=AF.Identity, scale=ng[:, k:k + 1]),
                     reads=["sq", "ng"], writes=["cat"])
            p.op("dve", lambda e: e.tensor_copy(out=cat[:, 8:16, 0:n], in_=at[:, :, 0:n]), reads=["at"], writes=["cat"])
            for j in range(8):
                pb = PS[2 + j % 6]
                for k in range(16):
                    p.op("pe", lambda e, pb=pb, j=j, k=k: e.matmul(pb[:, 0:n], lhsT=w_sb[:, k, j * 128:(j + 1) * 128],
                                                                   rhs=cat[:, k, 0:n], start=(k == 0), stop=(k == 15)),
                         reads=["w_sb", "cat"], writes=[pb.name])
                p.op("dve", lambda e, pb=pb, j=j, s=s: e.scalar_tensor_tensor(
                    out=xt[:, j, 0:n], in0=pb[:, 0:n], scalar=MC[:, l, 16 + j, s:s + 1], in1=xt[:, j, 0:n], op0=ALU.mult, op1=ALU.add),
                    reads=[pb.name, "MC"], writes=["xt"])
            p.dma(lambda e, t0=t0, n=n: e.dma_start(out=vv(dst)[:, :, t0:t0 + n], in_=xt[:, :, 0:n]), reads=["xt"], writes=["XS"])
        p.pop()

    def phase_gmlp(l, i, src, dst):
        p.push(f"o1{l}_")
        kinds = [0] * (HL // 128) + [1]
        Acol = p.sb([128, 2, 8], F32, "Acol")
        for s in range(2):
            p.op("dve", lambda e, s=s: e.scalar_tensor_tensor(out=Acol[:, s, :], in0=mcol(l, 1, s), scalar=1.0,
                                                              in1=n1[:, l * 8:(l + 1) * 8], op0=ALU.add, op1=ALU.mult),
                 reads=["MC", "n1"], writes=["modc"])
        stg = [p.sb([128, 2048], F32, f"stg{k}") for k in range(2)]
        win = p.sb([128, 8, 4096], BF16, "win")
        wout = p.sb([128, 16, 1024], BF16, "wout")
        load_w_bf16(p, win, v.gm_win[i], 8, 4096, stg)
        load_w_bf16(p, wout, v.gm_wout[i], 16, 1024, stg)
        ws_sb = p.sb([128, 8, 128], F32, "ws_sb")
        p.load(ws_sb[:], v.gm_wsT[i], "ws_sb")
        lng = p.sb([128, 2048], F32, "lng")
        lnb = p.sb([128, 2048], F32, "lnb")
        p.load(lng[:], v.gm_reps[i, 0], "lng")
        p.load(lnb[:], v.gm_reps[i, 1], "lnb")
        bs_sb = p.sb([128, 8, 128], F32, "bs_sb")
        p.load(bs_sb[:], v.gm_bsr[i], "bs_sb")
        xts = [p.sb([128, 8, 128], F32, f"xt{k}") for k in range(2)]
        sq = p.sb([128, 8, 128], F32, "sq")
        rstd = p.sb([128, 128], F32, "rstd")
        hT = p.sb([128, 8, 128], BF16, "hT")
        uT = p.sb([128, 16, 128], F32, "uT")
        vt = p.sb([128, 2048], F32, "vt")
        junk = p.sb([128, 2048], F32, "junk")
        st = p.sb([128, 16], F32, "st")
        gated = p.sb([128, 16, 128], BF16, "gated")
        tmp = p.sb([128, 512], F32, "tmp")
        pbs = PS
        xv = src.rearrange("(k p) t -> p k t", p=128)
        xov = dst.rearrange("(k p) t -> p k t", p=128)
        for ci, s in enumerate(kinds):
            t0 = ci * 128
            xt = xts[ci % 2]
            p.load(xt[:], xv[:, :, t0:t0 + 128], xt.name, skey="XS")
            fm_norm_mod(p, xt, 128, onesT, sq, pbs[0], rstd, Acol[:, s, :], mcol(l, 0, s), hT)
            for jb in range(4):
                pb = pbs[1 + jb % 3]
                for jj in range(4):
                    j = jb * 4 + jj
                    for k in range(8):
                        p.op("pe", lambda e, pb=pb, jj=jj, j=j, k=k: e.matmul(
                            pb[:, jj * 128:(jj + 1) * 128], lhsT=win[:, k, j * 128:(j + 1) * 128], rhs=hT[:, k, :],
                            start=(k == 0), stop=(k == 7)), reads=["win", "hT"], writes=[pb.name])
                p.op("act", lambda e, pb=pb, jb=jb: e.activation(out=uT[:, jb * 4:(jb + 1) * 4, :],
                                                                in_=pb[:].rearrange("p (j t) -> p j t", j=4), func=AF.Gelu),
                     reads=[pb.name], writes=["uT"])
            for cbk in range(4):
                pb = pbs[4 + cbk % 2]
                for k in range(8):
                    p.op("pe", lambda e, pb=pb, cbk=cbk, k=k: e.matmul(
                        pb[:], lhsT=hT[:, k, :], rhs=win[:, k, 2048 + cbk * 512:2048 + (cbk + 1) * 512],
                        start=(k == 0), stop=(k == 7)), reads=["win", "hT"], writes=[pb.name])
                p.op("act", lambda e, pb=pb, cbk=cbk: e.activation(out=vt[:, cbk * 512:(cbk + 1) * 512], in_=pb[:], func=AF.Gelu),
                     reads=[pb.name], writes=["vt"])
            p.op("dve", lambda e: e.tensor_reduce(out=st[:, 0:1], in_=vt[:], axis=AX.X, op=ALU.add), reads=["vt"], writes=["st"])
            p.op("act", lambda e: e.activation(out=junk[:], in_=vt[:], func=AF.Square), reads=["vt"], writes=["junk"])
            p.op("dve", lambda e: e.tensor_reduce(out=st[:, 1:2], in_=junk[:], axis=AX.X, op=ALU.add), reads=["junk"], writes=["st"])
            p.op("dve", lambda e: e.tensor_scalar(out=st[:, 2:4], in0=st[:, 0:2], scalar1=1.0 / 2048, scalar2=None, op0=ALU.mult),
                 reads=[], writes=["st"])
            p.op("dve", lambda e: e.tensor_tensor(out=st[:, 4:5], in0=st[:, 2:3], in1=st[:, 2:3], op=ALU.mult), reads=[], writes=["st"])
            p.op("dve", lambda e: e.tensor_tensor(out=st[:, 5:6], in0=st[:, 3:4], in1=st[:, 4:5], op=ALU.subtract), reads=[], writes=["st"])
            p.op("act", lambda e: e.activation(out=st[:, 6:7], in_=st[:, 5:6], func=AF.Sqrt, bias=EPS), reads=[], writes=["st"])
            p.op("dve", lambda e: e.reciprocal(out=st[:, 7:8], in_=st[:, 6:7]), reads=[], writes=["st"])
            p.op("dve", lambda e: e.tensor_scalar(out=vt[:], in0=vt[:], scalar1=st[:, 2:3], scalar2=st[:, 7:8], op0=ALU.subtract,
                                                  op1=ALU.mult), reads=["st"], writes=["vt"])
            p.op("dve", lambda e: e.tensor_tensor(out=vt[:], in0=vt[:], in1=lng[:], op=ALU.mult), reads=["lng"], writes=["vt"])
            p.op("dve", lambda e: e.tensor_tensor(out=vt[:], in0=vt[:], in1=lnb[:], op=ALU.add), reads=["lnb"], writes=["vt"])
            for jb in range(4):
                pb = pbs[6 + jb % 2]
                for jj in range(4):
                    j = jb * 4 + jj
                    p.op("pe", lambda e, pb=pb, jj=jj, j=j: e.matmul(pb[:, jj * 128:(jj + 1) * 128], lhsT=vt[:, j * 128:(j + 1) * 128],
                                                                     rhs=ws_sb[:, j // 2, :], start=True, stop=True),
                         reads=["vt", "ws_sb"], writes=[pb.name])
                p.op("dve", lambda e, pb=pb, jb=jb: e.tensor_tensor(
                    out=tmp[:].rearrange("p (g r t) -> p g r t", g=2, r=2), in0=pb[:].rearrange("p (g r t) -> p g r t", g=2, r=2),
                    in1=bs_sb[:, jb * 2:jb * 2 + 2, :].unsqueeze(2).to_broadcast([128, 2, 2, 128]), op=ALU.add),
                    reads=[pb.name, "bs_sb"], writes=["tmp"])
                p.op("dve", lambda e, jb=jb: e.tensor_tensor(out=gated[:, jb * 4:(jb + 1) * 4, :],
                                                             in0=tmp[:].rearrange("p (j t) -> p j t", j=4),
                                                             in1=uT[:, jb * 4:(jb + 1) * 4, :], op=ALU.mult),
                     reads=["tmp", "uT"], writes=["gated"])
            for jb in range(2):
                pb = pbs[1 + jb]
                for jj in range(4):
                    j = jb * 4 + jj
                    for k in range(16):
                        p.op("pe", lambda e, pb=pb, jj=jj, j=j, k=k: e.matmul(
                            pb[:, jj * 128:(jj + 1) * 128], lhsT=wout[:, k, j * 128:(j + 1) * 128], rhs=gated[:, k, :],
                            start=(k == 0), stop=(k == 15)), reads=["wout", "gated"], writes=[pb.name])
                for jj in range(4):
                    j = jb * 4 + jj
                    p.op("dve", lambda e, pb=pb, jj=jj, j=j, s=s: e.scalar_tensor_tensor(
                        out=xt[:, j, :], in0=pb[:, jj * 128:(jj + 1) * 128], scalar=MC[:, l, 16 + j, s:s + 1], in1=xt[:, j, :],
                        op0=ALU.mult, op1=ALU.add), reads=[pb.name, "MC"], writes=[xt.name])
            p.dma(lambda e, t0=t0, xt=xt: e.dma_start(out=xov[:, :, t0:t0 + 128], in_=xt[:]), reads=[xt.name], writes=["XS"])
        p.pop()

    v.phase_attn, v.phase_outproj, v.phase_gmlp = phase_attn, phase_outproj, phase_gmlp
    return _fused_peer_and_chain(v)


def _fused_peer_and_chain(v):
    p, PS, cs, ident, onesT, MC, iota16, s_sb = v.p, v.PS, v.cs, v.ident, v.onesT, v.MC, v.iota16, v.s_sb
    HL, HC, TPC, depth, XS = v.HL, v.HC, v.TPC, v.depth, v.XS

    def phase_peer(l, src, dst, final):
        p.push(f"pp{l}_")
        ctx_live = any(j % 2 == 0 for j in range(l + 1, depth))
        NT = TPC // 128 if ctx_live else HL // 128
        kinds = [0] * (HL // 128) + [1]
        utab, vtab = v.putab[l], v.pvtab[l]
        wq_sb = p.sb([128, 8, 2048], BF16, "wq")
        k1_sb = p.sb([128, 8, 128], F32, "k1")
        k2_sb = p.sb([128, 8, 128], F32, "k2")
        A_rep = [p.sb([128, 1024], F32, f"A{s}") for s in range(2)]
        B_rep = [p.sb([128, 1024], F32, f"B{s}") for s in range(2)]
        G2_rep = [p.sb([128, 1024], F32, f"G{s}") for s in range(2)]
        NG = 6
        gbuf = [p.sb([128, 2, 2048], BF16, f"gb{k}") for k in range(NG)]
        awt = p.sb([128, 8, 256], F32, "awt")
        gring = Ring(list(range(NG)))
        W = [p.sb([128, 2048], F32, f"W{k}") for k in range(4)]
        xt = p.sb([128, 1024], F32, "xt")
        tt = p.sb([128, 1024], F32, "tt")
        acc = p.sb([128, 1024], F32, "acc")
        tT = p.sb([128, 8, 128], BF16, "tT")
        xTt = p.sb([128, 8, 128], F32, "xTt")
        small = p.sb([128, 64], F32, "small")
        v16 = p.sb([128, 16, 16], F32, "v16")
        i16u = p.sb([128, 16, 16], U32, "i16u")
        i16f = p.sb([128, 16, 16], F32, "i16f")
        ts16 = p.sb([128, 8, 16], F32, "ts16")
        j16u = p.sb([128, 8, 16], U32, "j16u")
        jhu = p.sb([128, 8, 16], U32, "jhu")
        jlu = p.sb([128, 8, 16], U32, "jlu")
        jhi = p.sb([128, 8, 16], F32, "jhi")
        jlo = p.sb([128, 8, 16], F32, "jlo")
        sel1 = p.sb([128, 8, 16], F32, "sel1")
        sel2 = p.sb([128, 8, 16], F32, "sel2")
        eidf = p.sb([128, 128], F32, "eidf")
        eidu = p.sb([128, 128], I32, "eidu")
        gate = p.sb([128, 8, 16], F32, "gate")
        aval = p.sb([128, 128], F32, "aval")
        wval = p.sb([128, 128], F32, "wval")
        psum = PS
        if final:
            gf = p.sb([128, 1024], F32, "gf")
            p.load(gf[:], v.gfd[:, :], "gf")
        p.load(k1_sb[:], v.pk1T[l], "k1")
        p.load(k2_sb[:], v.pk2T[l], "k2")
        wq_v = v.pwq[l].rearrange("(k p) n -> p k n", p=128)
        wi = 0
        for k in range(8):
            wst = W[wi % 4]
            wi += 1
            p.load(wst[:], wq_v[:, k, :], wst.name)
            p.op("dve" if k % 2 else "act", (lambda e, wst=wst, k=k: e.tensor_copy(out=wq_sb[:, k, :], in_=wst[:])) if k % 2 else
                 (lambda e, wst=wst, k=k: e.activation(out=wq_sb[:, k, :], in_=wst[:], func=AF.Copy)),
                 reads=[wst.name], writes=["wq"])
        for which, tab in enumerate((v.putab[l], v.pvtab[l])):
            tv = tab.rearrange("(c p a) n -> c p (a n)", p=128, a=2)
            bv = v.UVB3[:, which, :].rearrange("(c p a) n -> c p a n", p=128, a=2)
            for cpi in range(64):
                wst = W[wi % 4]
                gi = gring.get()
                wi += 1
                cb16 = gbuf[gi][:, 0, :]
                p.load(wst[:], tv[cpi], wst.name)
                if cpi % 2:
                    p.op("dve", lambda e, wst=wst, cb16=cb16: e.tensor_copy(out=cb16, in_=wst[:]), reads=[wst.name], writes=[f"gb{gi}"])
                else:
                    p.op("act", lambda e, wst=wst, cb16=cb16: e.activation(out=cb16, in_=wst[:], func=AF.Copy),
                         reads=[wst.name], writes=[f"gb{gi}"])
                p.dma(lambda e, cpi=cpi, bv=bv, cb16=cb16: e.dma_start(out=bv[cpi], in_=cb16.rearrange("p (a n) -> p a n", a=2)),
                      reads=[f"gb{gi}"], writes=["UVB"])
        S_rep = [W[0][:, 0:1024].rearrange("p (k m) -> p k m", k=8), W[1][:, 0:1024].rearrange("p (k m) -> p k m", k=8)]
        n2 = W[2][:, 0:1024]
        for s in range(2):
            p.op("dve", lambda e, s=s: e.tensor_copy(out=S_rep[s], in_=s_sb[:, :, s:s + 1].to_broadcast([128, 8, 128])),
                 reads=["s_sb"], writes=[f"W{s}"])
        p.load(n2, v.n2g_rep[l], "W2")
        br = W[3][:, 0:256]
        for m in (3, 4, 5):
            for qq in range(4):
                c0 = m * 1024 + qq * 256
                p.load(awt[:], v.ada_w[l].rearrange("(k p) n -> p k n", p=128)[:, :, c0:c0 + 256], "awt")
                p.load(br, v.ada_br[l][:, c0 - 3072:c0 - 3072 + 256], "W3")
                for s in range(2):
                    pb = psum[s]
                    for kc in range(8):
                        p.op("pe", lambda e, pb=pb, s=s, kc=kc: e.matmul(pb[:, 0:256], lhsT=S_rep[s][:, kc, :], rhs=awt[:, kc, :],
                                                                        start=(kc == 0), stop=(kc == 7)),
                             reads=[f"W{s}", "awt"], writes=[pb.name])
                    hs = slice(qq * 256, (qq + 1) * 256)
                    dstt = {3: B_rep, 4: A_rep, 5: G2_rep}[m][s]
                    p.op("dve", lambda e, pb=pb, dstt=dstt, hs=hs: e.tensor_tensor(out=dstt[:, hs], in0=pb[:, 0:256], in1=br, op=ALU.add),
                         reads=[pb.name, "W3"], writes=[dstt.name])
                    if m == 4:
                        p.op("dve", lambda e, dstt=dstt, hs=hs: e.scalar_tensor_tensor(out=dstt[:, hs], in0=dstt[:, hs], scalar=1.0,
                                                                                      in1=n2[:, hs], op0=ALU.add, op1=ALU.mult),
                             reads=["W2"], writes=[dstt.name])
        srcv = src.rearrange("(k p) t -> p k t", p=128)
        dstv = dst.rearrange("(k p) t -> p k t", p=128) if dst is not None else None
        for ti in range(NT):
            s = kinds[ti]
            r0 = ti * 128
            p.load(xTt[:], srcv[:, :, r0:r0 + 128], "xTt", skey="XS")
            for half in range(2):
                pb = psum[half]
                for c in range(4):
                    k = half * 4 + c
                    p.op("pe", lambda e, pb=pb, c=c, k=k: e.transpose(out=pb[:, c * 128:(c + 1) * 128], in_=xTt[:, k, :], identity=ident),
                         reads=["xTt", "cs"], writes=[pb.name])
                p.op("act", lambda e, pb=pb, half=half: e.activation(out=xt[:, half * 512:(half + 1) * 512], in_=pb[:], func=AF.Copy),
                     reads=[pb.name], writes=["xt"])
            p.op("act", lambda e: e.activation(out=tt[:], in_=xt[:], func=AF.Square, accum_out=small[:, 0:1]),
                 reads=["xt"], writes=["tt", "small"])
            p.op("dve", lambda e: e.tensor_scalar(out=small[:, 1:2], in0=small[:, 0:1], scalar1=1.0 / 1024, scalar2=EPS,
                                                  op0=ALU.mult, op1=ALU.add), reads=["small"], writes=["small"])
            p.op("act", lambda e: e.activation(out=small[:, 3:4], in_=small[:, 1:2], func=AF.Sqrt), reads=["small"], writes=["small"])
            p.op("dve", lambda e: e.reciprocal(out=small[:, 2:3], in_=small[:, 3:4]), reads=["small"], writes=["small"])
            p.op("dve", lambda e, s=s: e.scalar_tensor_tensor(out=tt[:], in0=xt[:], scalar=small[:, 2:3], in1=A_rep[s][:],
                                                              op0=ALU.mult, op1=ALU.mult),
                 reads=["xt", "small", f"A{s}"], writes=["tt"])
            p.op("dve", lambda e, s=s: e.tensor_tensor(out=tt[:], in0=tt[:], in1=B_rep[s][:], op=ALU.add),
                 reads=[f"B{s}"], writes=["tt"])
            for half in range(2):
                pb = psum[half]
                for c in range(4):
                    k = half * 4 + c
                    p.op("pe", lambda e, pb=pb, c=c, k=k: e.transpose(out=pb[:, c * 128:(c + 1) * 128],
                                                                      in_=tt[:, k * 128:(k + 1) * 128], identity=ident),
                         reads=["tt", "cs"], writes=[pb.name])
                p.op("act", lambda e, pb=pb, half=half: e.activation(
                    out=tT[:, half * 4:(half + 1) * 4, :], in_=pb[:].rearrange("p (c t) -> p c t", c=4), func=AF.Copy),
                    reads=[pb.name], writes=["tT"])
            qT = W[0]
            for jb in range(4):
                pb = psum[2 + jb]
                for jj in range(4):
                    j = jb * 4 + jj
                    for k in range(8):
                        p.op("pe", lambda e, pb=pb, jj=jj, j=j, k=k: e.matmul(
                            pb[:, jj * 128:(jj + 1) * 128], lhsT=wq_sb[:, k, j * 128:(j + 1) * 128], rhs=tT[:, k, :],
                            start=(k == 0), stop=(k == 7)), reads=["wq", "tT"], writes=[pb.name])
                p.op("act", lambda e, pb=pb, jb=jb: e.activation(out=qT[:, jb * 512:(jb + 1) * 512], in_=pb[:], func=AF.Copy),
                     reads=[pb.name], writes=["W0"])
            S = W[1]
            for gb in range(4):
                pb = psum[(6 + gb) % 8]
                for gg in range(4):
                    g = gb * 4 + gg
                    h, half = g // 2, g % 2
                    ksb = k1_sb if half == 0 else k2_sb
                    p.op("pe", lambda e, pb=pb, gg=gg, g=g, h=h, ksb=ksb: e.matmul(
                        pb[:, gg * 128:(gg + 1) * 128], lhsT=qT[:, g * 128:(g + 1) * 128], rhs=ksb[:, h, :],
                        start=True, stop=True), reads=["W0", "k1", "k2"], writes=[pb.name])
                p.op("act", lambda e, pb=pb, gb=gb: e.activation(out=S[:, gb * 512:(gb + 1) * 512], in_=pb[:], func=AF.Copy),
                     reads=[pb.name], writes=["W1"])
            S2 = W[2]
            for g in range(16):
                sl = slice(g * 128, (g + 1) * 128)
                p.op("dve", lambda e, g=g, sl=sl: e.max(out=v16[:, g, 0:8], in_=S[:, sl]), reads=["W1"], writes=["v16"])
                p.op("dve", lambda e, g=g, sl=sl: e.max_index(out=i16u[:, g, 0:8], in_max=v16[:, g, 0:8], in_values=S[:, sl]),
                     reads=["W1", "v16"], writes=["i16u"])
                p.op("dve", lambda e, g=g, sl=sl: e.match_replace(out=S2[:, sl], in_to_replace=v16[:, g, 0:8],
                                                                  in_values=S[:, sl], imm_value=-1e30),
                     reads=["W1", "v16"], writes=["W2"])
                p.op("dve", lambda e, g=g, sl=sl: e.max(out=v16[:, g, 8:16], in_=S2[:, sl]), reads=["W2"], writes=["v16"])
                p.op("dve", lambda e, g=g, sl=sl: e.max_index(out=i16u[:, g, 8:16], in_max=v16[:, g, 8:16], in_values=S2[:, sl]),
                     reads=["W2", "v16"], writes=["i16u"])
            p.op("dve", lambda e: e.tensor_copy(out=i16f[:], in_=i16u[:]), reads=["i16u"], writes=["i16f"])
            v4 = v16[:].rearrange("p (h two) k -> p h two k", two=2)
            csum = W[3][:].rearrange("p (h i j) -> p h i j", h=8, i=16)
            p.op("dve", lambda e: e.tensor_tensor(out=csum, in0=v4[:, :, 0, :].unsqueeze(3).to_broadcast([128, 8, 16, 16]),
                                                  in1=v4[:, :, 1, :].unsqueeze(2).to_broadcast([128, 8, 16, 16]), op=ALU.add),
                 reads=["v16"], writes=["W3"])
            cs2 = W[0]
            for h in range(8):
                sl = slice(h * 256, (h + 1) * 256)
                p.op("dve", lambda e, h=h, sl=sl: e.max(out=ts16[:, h, 0:8], in_=W[3][:, sl]), reads=["W3"], writes=["ts16"])
                p.op("dve", lambda e, h=h, sl=sl: e.max_index(out=j16u[:, h, 0:8], in_max=ts16[:, h, 0:8], in_values=W[3][:, sl]),
                     reads=["W3", "ts16"], writes=["j16u"])
                p.op("dve", lambda e, h=h, sl=sl: e.match_replace(out=cs2[:, sl], in_to_replace=ts16[:, h, 0:8],
                                                                  in_values=W[3][:, sl], imm_value=-1e30),
                     reads=["W3", "ts16"], writes=["W0"])
                p.op("dve", lambda e, h=h, sl=sl: e.max(out=ts16[:, h, 8:16], in_=cs2[:, sl]), reads=["W0"], writes=["ts16"])
                p.op("dve", lambda e, h=h, sl=sl: e.max_index(out=j16u[:, h, 8:16], in_max=ts16[:, h, 8:16], in_values=cs2[:, sl]),
                     reads=["W0", "ts16"], writes=["j16u"])
            p.op("dve", lambda e: e.tensor_scalar(out=jhu[:], in0=j16u[:], scalar1=4, scalar2=None, op0=ALU.logical_shift_right),
                 reads=["j16u"], writes=["jhu"])
            p.op("dve", lambda e: e.tensor_scalar(out=jlu[:], in0=j16u[:], scalar1=15, scalar2=None, op0=ALU.bitwise_and),
                 reads=["j16u"], writes=["jlu"])
            p.op("dve", lambda e: e.tensor_copy(out=jhi[:], in_=jhu[:]), reads=["jhu"], writes=["jhi"])
            p.op("dve", lambda e: e.tensor_copy(out=jlo[:], in_=jlu[:]), reads=["jlu"], writes=["jlo"])
            i4 = i16f[:].rearrange("p (h two) k -> p h two k", two=2)
            eq = W[1][:].rearrange("p (h r k) -> p h r k", h=8, r=16)
            for which, (jsel, dsel) in enumerate(((jhi, sel1), (jlo, sel2))):
                for h in range(8):
                    p.op("dve", lambda e, h=h, jsel=jsel: e.tensor_tensor(
                        out=eq[:, h], in0=jsel[:, h, :].unsqueeze(2).to_broadcast([128, 16, 16]),
                        in1=iota16[:].unsqueeze(1).to_broadcast([128, 16, 16]), op=ALU.is_equal),
                        reads=[jsel.name, "iota16"], writes=["W1"])
                    p.op("dve", lambda e, h=h, which=which: e.tensor_tensor(
                        out=eq[:, h], in0=eq[:, h], in1=i4[:, h, which, :].unsqueeze(1).to_broadcast([128, 16, 16]),
                        op=ALU.mult), reads=["i16f"], writes=["W1"])
                p.op("dve", lambda e, dsel=dsel: e.tensor_reduce(out=dsel[:], in_=eq, axis=AX.X, op=ALU.add),
                     reads=["W1"], writes=[dsel.name])
            p.op("dve", lambda e: e.scalar_tensor_tensor(out=eidf[:], in0=sel1[:].rearrange("p h r -> p (h r)"), scalar=128.0,
                                                         in1=sel2[:].rearrange("p h r -> p (h r)"), op0=ALU.mult, op1=ALU.add),
                 reads=["sel1", "sel2"], writes=["eidf"])
            p.op("dve", lambda e: e.tensor_copy(out=eidu[:], in_=eidf[:]), reads=["eidf"], writes=["eidu"])
            p.op("dve", lambda e: e.tensor_tensor(out=gate[:], in0=ts16[:], in1=ts16[:, :, 0:1].to_broadcast([128, 8, 16]),
                                                  op=ALU.subtract), reads=["ts16"], writes=["gate"])
            p.op("act", lambda e: e.activation(out=gate[:], in_=gate[:], func=AF.Exp), reads=[], writes=["gate"])
            p.op("dve", lambda e: e.tensor_reduce(out=small[:, 8:16], in_=gate[:], axis=AX.X, op=ALU.add),
                 reads=["gate"], writes=["small"])
            p.op("dve", lambda e: e.reciprocal(out=small[:, 16:24], in_=small[:, 8:16]), reads=[], writes=["small"])
            p.op("dve", lambda e: e.tensor_tensor(out=gate[:], in0=gate[:],
                                                  in1=small[:, 16:24].unsqueeze(2).to_broadcast([128, 8, 16]), op=ALU.mult),
                 reads=["small"], writes=["gate"])
            gflat = gate[:].rearrange("p h r -> p (h r)")
            NCK = 64

            def tail_ops(c, gi):
                cs_ = slice(2 * c, 2 * c + 2)
                p.op("dve", lambda e: e.tensor_tensor(out=wval[:, cs_], in0=aval[:, cs_], in1=gflat[:, cs_], op=ALU.mult),
                     reads=[("gl", c % 4), "gate"], writes=[("wv", c % 4)])
                for ee in range(2):
                    col = 2 * c + ee
                    if col == 0:
                        p.op("dve", lambda e, ee=ee, col=col: e.tensor_scalar(
                            out=acc[:], in0=gbuf[gi][:, ee, 1024:2048], scalar1=wval[:, col:col + 1], scalar2=None, op0=ALU.mult),
                            reads=[f"gb{gi}", ("wv", c % 4)], writes=["acc"])
                    else:
                        p.op("dve", lambda e, ee=ee, col=col: e.scalar_tensor_tensor(
                            out=acc[:], in0=gbuf[gi][:, ee, 1024:2048], scalar=wval[:, col:col + 1], in1=acc[:],
                            op0=ALU.mult, op1=ALU.add), reads=[f"gb{gi}", ("wv", c % 4)], writes=["acc"])

            prev = None
            for c in range(NCK):
                gi = gring.get()
                cs_ = slice(2 * c, 2 * c + 2)
                for ee in range(2):
                    col = 2 * c + ee
                    p.dma(lambda e, gi=gi, ee=ee, col=col: e.indirect_dma_start(
                        out=gbuf[gi][:, ee, :], out_offset=None, in_=v.UVB,
                        in_offset=bass.IndirectOffsetOnAxis(ap=eidu[:, col:col + 1], axis=0)),
                        reads=["eidu", "UVB"], writes=[f"gb{gi}"], q="pool")
                p.op("dve", lambda e, gi=gi: e.tensor_tensor(out=gbuf[gi][:, :, 0:1024], in0=gbuf[gi][:, :, 0:1024],
                                                             in1=tt[:].unsqueeze(1).to_broadcast([128, 2, 1024]), op=ALU.mult),
                     reads=["tt"], writes=[f"gb{gi}"])
                p.op("dve", lambda e, gi=gi, cs_=cs_: e.tensor_reduce(out=aval[:, cs_], in_=gbuf[gi][:, :, 0:1024], axis=AX.X, op=ALU.add),
                     reads=[f"gb{gi}"], writes=[("av", c % 4)])
                p.op("act", lambda e, cs_=cs_: e.activation(out=aval[:, cs_], in_=aval[:, cs_], func=AF.Gelu),
                     reads=[("av", c % 4)], writes=[("gl", c % 4)])
                if prev is not None:
                    tail_ops(*prev)
                prev = (c, gi)
            tail_ops(*prev)
            p.op("dve", lambda e, s=s: e.tensor_tensor(out=acc[:], in0=acc[:], in1=G2_rep[s][:], op=ALU.mult),
                 reads=[f"G{s}"], writes=["acc"])
            p.op("dve", lambda e: e.tensor_tensor(out=acc[:], in0=acc[:], in1=xt[:], op=ALU.add), reads=["xt"], writes=["acc"])
            if final:
                p.op("act", lambda e: e.activation(out=tt[:], in_=acc[:], func=AF.Square, accum_out=small[:, 32:33]),
                     reads=["acc"], writes=["tt", "small"])
                p.op("dve", lambda e: e.tensor_scalar(out=small[:, 33:34], in0=small[:, 32:33], scalar1=1.0 / 1024, scalar2=EPS,
                                                      op0=ALU.mult, op1=ALU.add), reads=[], writes=["small"])
                p.op("act", lambda e: e.activation(out=small[:, 34:35], in_=small[:, 33:34], func=AF.Sqrt), reads=[], writes=["small"])
                p.op("dve", lambda e: e.reciprocal(out=small[:, 35:36], in_=small[:, 34:35]), reads=[], writes=["small"])
                p.op("dve", lambda e: e.scalar_tensor_tensor(out=acc[:], in0=acc[:], scalar=small[:, 35:36], in1=gf[:],
                                                             op0=ALU.mult, op1=ALU.mult), reads=["small", "gf"], writes=["acc"])
                p.store(v.yout[r0:r0 + 128, :], acc[:], "acc")
            else:
                for half in range(2):
                    pb = psum[half]
                    for c in range(4):
                        k = half * 4 + c
                        p.op("pe", lambda e, pb=pb, c=c, k=k: e.transpose(out=pb[:, c * 128:(c + 1) * 128],
                                                                          in_=acc[:, k * 128:(k + 1) * 128], identity=ident),
                             reads=["acc", "cs"], writes=[pb.name])
                    p.op("act", lambda e, pb=pb, half=half: e.activation(
                        out=xTt[:, half * 4:(half + 1) * 4, :], in_=pb[:].rearrange("p (c t) -> p c t", c=4), func=AF.Copy),
                        reads=[pb.name], writes=["xTt"])
                p.dma(lambda e, r0=r0: e.dma_start(out=dstv[:, :, r0:r0 + 128], in_=xTt[:]), reads=["xTt"], writes=["XS"])
        p.pop()

    cur = v.xT0
    for l in range(depth):
        i = l // 2
        if l % 2 == 0:
            v.phase_inproj(l, i, cur)
            v.phase_ssd(i)
            v.phase_attn(i)
            v.phase_outproj(l, i, cur, XS[0])
        else:
            v.phase_gmlp(l, i, cur, XS[0])
        final = l == depth - 1
        phase_peer(l, XS[0], None if final else XS[1], final)
        cur = XS[1]
    return p.finish()


def kernel(x, c, ctx, c_ctx, ada_w, ada_b, norm1_g, norm2_g, hyb_w_in, ssd_conv_w, ssd_conv_b,
           ssd_a_log, ssd_dt_bias, ssd_d, ssd_norm_g, mla_q_norm_g, mla_w_qb, mla_kv_norm_g, mla_w_kvb,
           hyb_w_out, gm_w_in, gm_ln_g, gm_ln_b, gm_ws, gm_bs, gm_w_out, peer_wq, peer_k1, peer_k2,
           peer_u, peer_v, final_norm_g):
    f32 = lambda a: np.ascontiguousarray(np.asarray(a, dtype=np.float32))
    x, c, ctx, c_ctx = f32(x), f32(c), f32(ctx), f32(c_ctx)
    B, L, D = x.shape
    LC = ctx.shape[1]
    depth = ada_w.shape[0]
    ne, no = (depth + 1) // 2, depth // 2
    HL, HC = L // 2, LC // 2
    HLB = HL // 512
    TPC = HL + HC
    ada_w, ada_b = f32(ada_w), f32(ada_b)
    shared = {}
    shared["ada_w"] = ada_w
    shared["ada_bc"] = np.ascontiguousarray(ada_b.reshape(depth, 48, 128).transpose(2, 0, 1).reshape(128, depth * 48))
    shared["ada_br"] = np.ascontiguousarray(np.broadcast_to(ada_b[:, None, 3072:6144], (depth, 128, 3072)))
    shared["n1g"] = np.ascontiguousarray(f32(norm1_g).reshape(depth, 8, 128).transpose(2, 0, 1).reshape(128, depth * 8))
    shared["n2g_rep"] = np.ascontiguousarray(np.broadcast_to(f32(norm2_g)[:, None, :], (depth, 128, D)))
    shared["cst"] = _ssd_consts()
    shared["iotad"] = np.ascontiguousarray(np.broadcast_to(np.arange(16, dtype=np.float32)[None], (128, 16)))
    perm = np.concatenate([np.arange(0, 1024), np.arange(2576, 2960),
                           np.arange(1024, 1536), np.arange(2048, 2176), np.arange(2304, 2432),
                           np.arange(1536, 2048), np.arange(2176, 2304), np.arange(2432, 2560),
                           np.arange(2960, 3216), np.arange(3216, 3280), np.arange(2560, 2576)])
    w_in = np.zeros((ne, D, 3328), np.float32)
    w_in[:, :, :perm.size] = f32(hyb_w_in)[:, :, perm]
    shared["w_in"] = w_in
    wqb_, wkvb_ = f32(mla_w_qb), f32(mla_w_kvb)
    cat = lambda w, lo, hi, st: np.ascontiguousarray(np.concatenate([w[:, :, h * st + lo:h * st + hi] for h in range(8)], 2))
    shared["wqn"], shared["wq1"], shared["wq2"] = cat(wqb_, 0, 128, 192), cat(wqb_, 128, 160, 192), cat(wqb_, 160, 192, 192)
    shared["wkn"], shared["wvd"] = cat(wkvb_, 0, 128, 256), cat(wkvb_, 128, 256, 256)
    shared["qg"] = np.stack([_colv(mla_q_norm_g[i], 3) for i in range(ne)])
    shared["kvg"] = np.stack([_colv(mla_kv_norm_g[i], 2) for i in range(ne)])
    cosT, sinT = _rope_tables(L, LC)
    shared["cosK"], shared["sinK"] = cosT, sinT
    shared["w_out"] = f32(hyb_w_out)
    shared["ssd_ng"] = np.stack([_colv(ssd_norm_g[i]) for i in range(ne)])
    shared["gm_win"], shared["gm_wout"] = f32(gm_w_in), f32(gm_w_out)
    shared["gm_wsT"] = np.ascontiguousarray(f32(gm_ws).transpose(0, 3, 1, 2))
    shared["gm_reps"] = np.ascontiguousarray(np.stack([np.broadcast_to(f32(gm_ln_g)[:, None, :], (no, 128, 2048)),
                                                       np.broadcast_to(f32(gm_ln_b)[:, None, :], (no, 128, 2048))], 1))
    shared["gm_bsr"] = np.ascontiguousarray(np.broadcast_to(f32(gm_bs)[:, None], (no, 128, 8, 128)))
    shared["pwq"] = f32(peer_wq)
    shared["pk1T"] = np.ascontiguousarray(f32(peer_k1).transpose(0, 3, 1, 2))
    shared["pk2T"] = np.ascontiguousarray(f32(peer_k2).transpose(0, 3, 1, 2))
    for l_ in range(depth):
        shared[f"putab{l_}"] = f32(peer_u[l_])
        shared[f"pvtab{l_}"] = f32(peer_v[l_])
    shared["gfd"] = _rows(f32(final_norm_g))
    cw, cb = f32(ssd_conv_w), f32(ssd_conv_b)
    ims = []
    for ci in range(NCORES):
        b, r = ci // 2, ci % 2
        im = dict(shared)
        im["xT0"] = np.ascontiguousarray(np.concatenate([x[b, r * HL:(r + 1) * HL], ctx[b, r * HC:(r + 1) * HC]], 0).T)
        cv2 = np.stack([c[b], c_ctx])
        im["c2T"] = np.ascontiguousarray(cv2.T.reshape(8, 128, 2).transpose(1, 0, 2))
        m = np.zeros((128, 2), np.float32)
        m[:, r] = 1.0
        im["m01"] = m
        chsel = np.concatenate([r * 512 + np.arange(512), 1024 + r * 128 + np.arange(128), 1280 + r * 128 + np.arange(128)])
        hs = slice(r * 8, (r + 1) * 8)
        im["convw"] = np.ascontiguousarray(cw[:, chsel])
        im["convb"] = np.ascontiguousarray(cb[:, chsel].reshape(ne, 6, 128).transpose(0, 2, 1))
        im["rep8"] = np.ascontiguousarray(np.broadcast_to(
            np.stack([f32(ssd_dt_bias)[:, 0, hs], f32(ssd_dt_bias)[:, 1, hs], f32(ssd_a_log)[:, 0, hs], f32(ssd_a_log)[:, 1, hs],
                      f32(ssd_d)[:, hs]], 1)[:, None], (ne, 128, 5, 8)))
        im["cosQ"] = np.ascontiguousarray(np.concatenate([cosT[:, LC + r * HL:LC + (r + 1) * HL], cosT[:, r * HC:(r + 1) * HC]], 1))
        im["sinQ"] = np.ascontiguousarray(np.concatenate([sinT[:, LC + r * HL:LC + (r + 1) * HL], sinT[:, r * HC:(r + 1) * HC]], 1))
        ims.append(im)
    res = _run(build_fused(HLB, depth), ims)
    out = np.empty((B, L, D), np.float32)
    for ci in range(NCORES):
        out[ci // 2, (ci % 2) * HL:(ci % 2 + 1) * HL] = res[ci]["y"]
    return out
```

```python
import math
from contextlib import ExitStack
import numpy as np
import concourse.bass as bass
import concourse.mybir as mybir
from concourse.bass_utils import run_bass_kernel_spmd

F32 = mybir.dt.float32
BF16 = mybir.dt.bfloat16
U32 = mybir.dt.uint32
I32 = mybir.dt.int32
AF = mybir.ActivationFunctionType
ALU = mybir.AluOpType
AX = mybir.AxisListType

EPS = 1e-6
NCORES = 8


class Prog:
    def __init__(self, name):
        self.nc = bass.Bass("TRN2", target_bir_lowering=False)
        self.es = ExitStack()
        nc = self.nc
        self.E = {"pe": nc.tensor, "dve": nc.vector, "act": nc.scalar, "pool": nc.gpsimd, "sp": nc.sync}
        self.sem = {k: self.es.enter_context(nc.semaphore("s_" + k)) for k in self.E}
        self.cnt = {k: 0 for k in self.E}
        self.seen = {k: {} for k in self.E}
        nds = 24
        self.dsem = [self.es.enter_context(nc.semaphore(f"d{i}")) for i in range(nds)]
        self.dcnt = [0] * nds
        self.dnext = 0
        self.lastw = {}
        self.readers = {}
        self.uid = 0
        self.out_toks = []
        self.ccsem = self.es.enter_context(nc.semaphore("ccsem"))
        self.cccnt = 0
        self.scopes = []
        self.prefix = ""

    def push(self, prefix):
        self.scopes.append(ExitStack())
        self.prefix = prefix

    def pop(self):
        self.barrier()
        self.scopes.pop().close()
        self.prefix = ""

    def barrier(self):
        for e in self.E:
            for f in self.E:
                if f != e and self.cnt[f] > 0:
                    self._wait(e, ("e", f, self.cnt[f]))
            for j in range(len(self.dsem)):
                if self.dcnt[j] > 0:
                    self._wait(e, ("d", j, 16 * self.dcnt[j]))
            if self.cccnt > 0:
                self._wait(e, ("c", 0, self.cccnt))

    def cc(self, fn, reads=(), writes=()):
        self._deps("pool", reads, writes)
        ins = fn(self.E["pool"])
        self.cccnt += 1
        ins.then_inc(self.ccsem)
        self._record(("c", 0, self.cccnt), reads, writes)

    def sb(self, shape, dtype=F32, name=None):
        self.uid += 1
        es = self.scopes[-1] if self.scopes else self.es
        return es.enter_context(self.nc.sbuf_tensor(self.prefix + (name or f"t_{self.uid}"), list(shape), dtype))

    def ps(self, shape=(128, 512), dtype=F32, name=None):
        self.uid += 1
        return self.es.enter_context(self.nc.psum_tensor(name or f"p_{self.uid}", list(shape), dtype))

    def dram(self, name, shape, dtype=F32, kind="ExternalInput"):
        return self.nc.dram_tensor(name, list(shape), dtype, kind=kind).ap()

    def _wait(self, eng, tok):
        if tok[0] == "e":
            _, e2, c = tok
            if e2 == eng and eng == "pe":
                return
            key = ("e", e2)
            sem = self.sem[e2]
        elif tok[0] == "c":
            _, _, c = tok
            key = ("c", 0)
            sem = self.ccsem
        else:
            _, j, c = tok
            key = ("d", j)
            sem = self.dsem[j]
        if self.seen[eng].get(key, 0) >= c:
            return
        self.E[eng].wait_ge(sem, c)
        self.seen[eng][key] = c

    def _norm(self, keys):
        pf = self.prefix
        if not pf:
            return list(keys)
        return [k[len(pf):] if isinstance(k, str) and k.startswith(pf) else k for k in keys]

    def _deps(self, eng, reads, writes):
        reads, writes = self._norm(reads), self._norm(writes)
        for k in reads:
            t = self.lastw.get(k)
            if t is not None:
                self._wait(eng, t)
        for k in writes:
            t = self.lastw.get(k)
            if t is not None:
                self._wait(eng, t)
            for t in self.readers.get(k, {}).values():
                self._wait(eng, t)

    def _record(self, tok, reads, writes):
        reads, writes = self._norm(reads), self._norm(writes)
        for k in writes:
            self.lastw[k] = tok
            self.readers[k] = {}
        for k in reads:
            if k in writes:
                continue
            self.readers.setdefault(k, {})[tok[:2]] = tok

    def op(self, eng, fn, reads=(), writes=()):
        self._deps(eng, reads, writes)
        ins = fn(self.E[eng])
        self.cnt[eng] += 1
        ins.then_inc(self.sem[eng], 1)
        self._record(("e", eng, self.cnt[eng]), reads, writes)

    def dma(self, fn, reads=(), writes=(), q="sp", is_out=False):
        j = self.dnext
        self.dnext = (self.dnext + 1) % len(self.dsem)
        if self.dcnt[j] > 0:
            self._wait(q, ("d", j, 16 * self.dcnt[j]))
        self._deps(q, reads, writes)
        ins = fn(self.E[q])
        self.dcnt[j] += 1
        ins.then_inc(self.dsem[j], 16)
        tok = ("d", j, 16 * self.dcnt[j])
        self._record(tok, reads, writes)
        if is_out:
            self.out_toks.append(tok)
        return tok

    def finish(self):
        for j in range(len(self.dsem)):
            if self.dcnt[j] > 0:
                self._wait("sp", ("d", j, 16 * self.dcnt[j]))
        if self.cccnt > 0:
            self._wait("sp", ("c", 0, self.cccnt))
        self.es.close()
        return self.nc

    def load(self, dst_ap, src_ap, dkey, skey=None, q="sp"):
        self.dma(lambda e: e.dma_start(out=dst_ap, in_=src_ap), reads=[skey] if skey else [], writes=[dkey], q=q)

    def store(self, dst_ap, src_ap, skey, dkey=None, q="sp"):
        self.dma(lambda e: e.dma_start(out=dst_ap, in_=src_ap), reads=[skey], writes=[dkey] if dkey else [],
                 q=q, is_out=True)


class Ring:
    def __init__(self, items):
        self.items = items
        self.i = 0

    def get(self):
        it = self.items[self.i]
        self.i = (self.i + 1) % len(self.items)
        return it


def _run(nc, in_maps, ncores=NCORES):
    res = run_bass_kernel_spmd(nc, in_maps, core_ids=list(range(ncores)))
    return res.results


def build_peer(NT, kinds, final=False):
    p = Prog("peer")
    T = NT * 128
    x = p.dram("x", [T, 1024])
    rep = p.dram("rep", [2, 4, 128, 1024])
    wq = p.dram("wqd", [1024, 2048])
    k1T = p.dram("k1T", [128, 8, 128])
    k2T = p.dram("k2T", [128, 8, 128])
    utab = p.dram("utab", [16384, 1024])
    vtab = p.dram("vtab", [16384, 1024])
    ident_d = p.dram("identd", [128, 128])
    iota_d = p.dram("iotad", [128, 16])
    y = p.dram("y", [T, 1024], kind="ExternalOutput")
    if final:
        gfd = p.dram("gfd", [128, 1024])
        gf = p.sb([128, 1024], F32, "gf")
        p.load(gf[:], gfd[:, :], "gf")

    ident = p.sb([128, 128], F32, "ident")
    iota16 = p.sb([128, 16], F32, "iota16")
    wq_sb = p.sb([128, 8, 2048], BF16, "wq")
    k1_sb = p.sb([128, 8, 128], F32, "k1")
    k2_sb = p.sb([128, 8, 128], F32, "k2")
    A_rep = [p.sb([128, 1024], F32, f"A{s}") for s in range(2)]
    B_rep = [p.sb([128, 1024], F32, f"B{s}") for s in range(2)]
    G2_rep = [p.sb([128, 1024], F32, f"G{s}") for s in range(2)]
    NG = 3
    gbuf = [p.sb([128, 4, 1024], F32, f"gb{i}") for i in range(NG)]
    gring = Ring(list(range(NG)))
    W = [p.sb([128, 2048], F32, f"W{i}") for i in range(4)]
    xt = p.sb([128, 1024], F32, "xt")
    tt = p.sb([128, 1024], F32, "tt")
    acc = p.sb([128, 1024], F32, "acc")
    tT = p.sb([128, 8, 128], BF16, "tT")
    small = p.sb([128, 64], F32, "small")
    v16 = p.sb([128, 16, 16], F32, "v16")
    i16u = p.sb([128, 16, 16], U32, "i16u")
    i16f = p.sb([128, 16, 16], F32, "i16f")
    ts16 = p.sb([128, 8, 16], F32, "ts16")
    j16u = p.sb([128, 8, 16], U32, "j16u")
    jhu = p.sb([128, 8, 16], U32, "jhu")
    jlu = p.sb([128, 8, 16], U32, "jlu")
    jhi = p.sb([128, 8, 16], F32, "jhi")
    jlo = p.sb([128, 8, 16], F32, "jlo")
    sel1 = p.sb([128, 8, 16], F32, "sel1")
    sel2 = p.sb([128, 8, 16], F32, "sel2")
    eidf = p.sb([128, 128], F32, "eidf")
    eidu = p.sb([128, 128], I32, "eidu")
    gate = p.sb([128, 8, 16], F32, "gate")
    aval = p.sb([128, 128], F32, "aval")
    wval = p.sb([128, 128], F32, "wval")
    psum = [p.ps([128, 512], F32, f"ps{i}") for i in range(8)]

    p.load(ident[:], ident_d[:, :], "ident")
    p.load(iota16[:], iota_d[:, :], "iota16")
    p.load(k1_sb[:], k1T[:, :, :], "k1")
    p.load(k2_sb[:], k2T[:, :, :], "k2")
    wq_v = wq.rearrange("(k p) n -> p k n", p=128)
    for k in range(8):
        for hlf in range(2):
            gi = gring.get()
            p.load(gbuf[gi][:, 0:1, :], wq_v[:, k:k + 1, hlf * 1024:(hlf + 1) * 1024], f"gb{gi}")
            p.op("dve", lambda e, gi=gi, k=k, hlf=hlf: e.tensor_copy(
                out=wq_sb[:, k, hlf * 1024:(hlf + 1) * 1024], in_=gbuf[gi][:, 0, :]),
                reads=[f"gb{gi}"], writes=["wq"])
    for s in range(2):
        gi = gring.get()
        p.load(gbuf[gi][:, 0:4, :], rep[s].rearrange("f p n -> p f n"), f"gb{gi}")
        p.op("dve", lambda e, gi=gi, s=s: e.scalar_tensor_tensor(
            out=A_rep[s][:], in0=gbuf[gi][:, 1, :], scalar=1.0, in1=gbuf[gi][:, 0, :], op0=ALU.add, op1=ALU.mult),
            reads=[f"gb{gi}"], writes=[f"A{s}"])
        p.op("dve", lambda e, gi=gi, s=s: e.tensor_copy(out=B_rep[s][:], in_=gbuf[gi][:, 2, :]),
             reads=[f"gb{gi}"], writes=[f"B{s}"])
        p.op("dve", lambda e, gi=gi, s=s: e.tensor_copy(out=G2_rep[s][:], in_=gbuf[gi][:, 3, :]),
             reads=[f"gb{gi}"], writes=[f"G{s}"])

    for ti in range(NT):
        s = kinds[ti]
        r0 = ti * 128
        p.load(xt[:], x[r0:r0 + 128, :], "xt")
        p.op("act", lambda e: e.activation(out=tt[:], in_=xt[:], func=AF.Square, accum_out=small[:, 0:1]),
             reads=["xt"], writes=["tt", "small"])
        p.op("dve", lambda e: e.tensor_scalar(out=small[:, 1:2], in0=small[:, 0:1], scalar1=1.0 / 1024, scalar2=EPS,
                                              op0=ALU.mult, op1=ALU.add), reads=["small"], writes=["small"])
        p.op("act", lambda e: e.activation(out=small[:, 3:4], in_=small[:, 1:2], func=AF.Sqrt), reads=["small"], writes=["small"])
        p.op("dve", lambda e: e.reciprocal(out=small[:, 2:3], in_=small[:, 3:4]), reads=["small"], writes=["small"])
        p.op("dve", lambda e, s=s: e.scalar_tensor_tensor(out=tt[:], in0=xt[:], scalar=small[:, 2:3], in1=A_rep[s][:],
                                                          op0=ALU.mult, op1=ALU.mult),
             reads=["xt", "small", f"A{s}"], writes=["tt"])
        p.op("dve", lambda e, s=s: e.tensor_tensor(out=tt[:], in0=tt[:], in1=B_rep[s][:], op=ALU.add),
             reads=[f"B{s}"], writes=["tt"])
        for half in range(2):
            pb = psum[half]
            for c in range(4):
                k = half * 4 + c
                p.op("pe", lambda e, pb=pb, c=c, k=k: e.transpose(out=pb[:, c * 128:(c + 1) * 128],
                                                                  in_=tt[:, k * 128:(k + 1) * 128], identity=ident[:]),
                     reads=["tt", "ident"], writes=[pb.name])
            p.op("act", lambda e, pb=pb, half=half: e.activation(
                out=tT[:, half * 4:(half + 1) * 4, :], in_=pb[:].rearrange("p (c t) -> p c t", c=4), func=AF.Copy),
                reads=[pb.name], writes=["tT"])
        qT = W[0]
        for jb in range(4):
            pb = psum[2 + jb]
            for jj in range(4):
                j = jb * 4 + jj
                for k in range(8):
                    p.op("pe", lambda e, pb=pb, jj=jj, j=j, k=k: e.matmul(
                        pb[:, jj * 128:(jj + 1) * 128], lhsT=wq_sb[:, k, j * 128:(j + 1) * 128], rhs=tT[:, k, :],
                        start=(k == 0), stop=(k == 7)), reads=["wq", "tT"], writes=[pb.name])
            p.op("act", lambda e, pb=pb, jb=jb: e.activation(out=qT[:, jb * 512:(jb + 1) * 512], in_=pb[:], func=AF.Copy),
                 reads=[pb.name], writes=["W0"])
        S = W[1]
        for gb in range(4):
            pb = psum[(6 + gb) % 8]
            for gg in range(4):
                g = gb * 4 + gg
                h, half = g // 2, g % 2
                ksb = k1_sb if half == 0 else k2_sb
                p.op("pe", lambda e, pb=pb, gg=gg, g=g, h=h, ksb=ksb: e.matmul(
                    pb[:, gg * 128:(gg + 1) * 128], lhsT=qT[:, g * 128:(g + 1) * 128], rhs=ksb[:, h, :],
                    start=True, stop=True), reads=["W0", "k1", "k2"], writes=[pb.name])
            p.op("act", lambda e, pb=pb, gb=gb: e.activation(out=S[:, gb * 512:(gb + 1) * 512], in_=pb[:], func=AF.Copy),
                 reads=[pb.name], writes=["W1"])
        S2 = W[2]
        for g in range(16):
            sl = slice(g * 128, (g + 1) * 128)
            p.op("dve", lambda e, g=g, sl=sl: e.max(out=v16[:, g, 0:8], in_=S[:, sl]), reads=["W1"], writes=["v16"])
            p.op("dve", lambda e, g=g, sl=sl: e.max_index(out=i16u[:, g, 0:8], in_max=v16[:, g, 0:8], in_values=S[:, sl]),
                 reads=["W1", "v16"], writes=["i16u"])
            p.op("dve", lambda e, g=g, sl=sl: e.match_replace(out=S2[:, sl], in_to_replace=v16[:, g, 0:8],
                                                              in_values=S[:, sl], imm_value=-1e30),
                 reads=["W1", "v16"], writes=["W2"])
            p.op("dve", lambda e, g=g, sl=sl: e.max(out=v16[:, g, 8:16], in_=S2[:, sl]), reads=["W2"], writes=["v16"])
            p.op("dve", lambda e, g=g, sl=sl: e.max_index(out=i16u[:, g, 8:16], in_max=v16[:, g, 8:16], in_values=S2[:, sl]),
                 reads=["W2", "v16"], writes=["i16u"])
        p.op("dve", lambda e: e.tensor_copy(out=i16f[:], in_=i16u[:]), reads=["i16u"], writes=["i16f"])
        v4 = v16[:].rearrange("p (h two) k -> p h two k", two=2)
        cs = W[3][:].rearrange("p (h i j) -> p h i j", h=8, i=16)
        p.op("dve", lambda e: e.tensor_tensor(out=cs, in0=v4[:, :, 0, :].unsqueeze(3).to_broadcast([128, 8, 16, 16]),
                                              in1=v4[:, :, 1, :].unsqueeze(2).to_broadcast([128, 8, 16, 16]), op=ALU.add),
             reads=["v16"], writes=["W3"])
        cs2 = W[0]
        for h in range(8):
            sl = slice(h * 256, (h + 1) * 256)
            p.op("dve", lambda e, h=h, sl=sl: e.max(out=ts16[:, h, 0:8], in_=W[3][:, sl]), reads=["W3"], writes=["ts16"])
            p.op("dve", lambda e, h=h, sl=sl: e.max_index(out=j16u[:, h, 0:8], in_max=ts16[:, h, 0:8], in_values=W[3][:, sl]),
                 reads=["W3", "ts16"], writes=["j16u"])
            p.op("dve", lambda e, h=h, sl=sl: e.match_replace(out=cs2[:, sl], in_to_replace=ts16[:, h, 0:8],
                                                              in_values=W[3][:, sl], imm_value=-1e30),
                 reads=["W3", "ts16"], writes=["W0"])
            p.op("dve", lambda e, h=h, sl=sl: e.max(out=ts16[:, h, 8:16], in_=cs2[:, sl]), reads=["W0"], writes=["ts16"])
            p.op("dve", lambda e, h=h, sl=sl: e.max_index(out=j16u[:, h, 8:16], in_max=ts16[:, h, 8:16], in_values=cs2[:, sl]),
                 reads=["W0", "ts16"], writes=["j16u"])
        p.op("dve", lambda e: e.tensor_scalar(out=jhu[:], in0=j16u[:], scalar1=4, scalar2=None, op0=ALU.logical_shift_right),
             reads=["j16u"], writes=["jhu"])
        p.op("dve", lambda e: e.tensor_scalar(out=jlu[:], in0=j16u[:], scalar1=15, scalar2=None, op0=ALU.bitwise_and),
             reads=["j16u"], writes=["jlu"])
        p.op("dve", lambda e: e.tensor_copy(out=jhi[:], in_=jhu[:]), reads=["jhu"], writes=["jhi"])
        p.op("dve", lambda e: e.tensor_copy(out=jlo[:], in_=jlu[:]), reads=["jlu"], writes=["jlo"])
        i4 = i16f[:].rearrange("p (h two) k -> p h two k", two=2)
        eq = W[1][:].rearrange("p (h r k) -> p h r k", h=8, r=16)
        for which, (jsel, dst) in enumerate(((jhi, sel1), (jlo, sel2))):
            for h in range(8):
                p.op("dve", lambda e, h=h, jsel=jsel: e.tensor_tensor(
                    out=eq[:, h], in0=jsel[:, h, :].unsqueeze(2).to_broadcast([128, 16, 16]),
                    in1=iota16[:].unsqueeze(1).to_broadcast([128, 16, 16]), op=ALU.is_equal),
                    reads=[jsel.name, "iota16"], writes=["W1"])
                p.op("dve", lambda e, h=h, which=which: e.tensor_tensor(
                    out=eq[:, h], in0=eq[:, h], in1=i4[:, h, which, :].unsqueeze(1).to_broadcast([128, 16, 16]),
                    op=ALU.mult), reads=["i16f"], writes=["W1"])
            p.op("dve", lambda e, dst=dst: e.tensor_reduce(out=dst[:], in_=eq, axis=AX.X, op=ALU.add),
                 reads=["W1"], writes=[dst.name])
        p.op("dve", lambda e: e.scalar_tensor_tensor(out=eidf[:], in0=sel1[:].rearrange("p h r -> p (h r)"), scalar=128.0,
                                                     in1=sel2[:].rearrange("p h r -> p (h r)"), op0=ALU.mult, op1=ALU.add),
             reads=["sel1", "sel2"], writes=["eidf"])
        p.op("dve", lambda e: e.tensor_copy(out=eidu[:], in_=eidf[:]), reads=["eidf"], writes=["eidu"])
        p.op("dve", lambda e: e.tensor_tensor(out=gate[:], in0=ts16[:], in1=ts16[:, :, 0:1].to_broadcast([128, 8, 16]),
                                              op=ALU.subtract), reads=["ts16"], writes=["gate"])
        p.op("act", lambda e: e.activation(out=gate[:], in_=gate[:], func=AF.Exp), reads=[], writes=["gate"])
        p.op("dve", lambda e: e.tensor_reduce(out=small[:, 8:16], in_=gate[:], axis=AX.X, op=ALU.add),
             reads=["gate"], writes=["small"])
        p.op("dve", lambda e: e.reciprocal(out=small[:, 16:24], in_=small[:, 8:16]), reads=[], writes=["small"])
        p.op("dve", lambda e: e.tensor_tensor(out=gate[:], in0=gate[:],
                                              in1=small[:, 16:24].unsqueeze(2).to_broadcast([128, 8, 16]), op=ALU.mult),
             reads=["small"], writes=["gate"])
        for c in range(32):
            gi = gring.get()
            for ee in range(4):
                col = c * 4 + ee
                p.dma(lambda e, gi=gi, ee=ee, col=col: e.indirect_dma_start(
                    out=gbuf[gi][:, ee, :], out_offset=None, in_=utab[:, :],
                    in_offset=bass.IndirectOffsetOnAxis(ap=eidu[:, col:col + 1], axis=0)),
                    reads=["eidu"], writes=[f"gb{gi}"], q="pool")
            p.op("dve", lambda e, gi=gi: e.tensor_tensor(out=gbuf[gi][:], in0=gbuf[gi][:],
                                                         in1=tt[:].unsqueeze(1).to_broadcast([128, 4, 1024]), op=ALU.mult),
                 reads=["tt"], writes=[f"gb{gi}"])
            p.op("dve", lambda e, gi=gi, c=c: e.tensor_reduce(out=aval[:, c * 4:(c + 1) * 4], in_=gbuf[gi][:], axis=AX.X,
                                                              op=ALU.add), reads=[f"gb{gi}"], writes=["aval"])
        p.op("act", lambda e: e.activation(out=wval[:], in_=aval[:], func=AF.Gelu), reads=["aval"], writes=["wval"])
        p.op("dve", lambda e: e.tensor_tensor(out=wval[:], in0=wval[:], in1=gate[:].rearrange("p h r -> p (h r)"),
                                              op=ALU.mult), reads=["gate"], writes=["wval"])
        for c in range(32):
            gi = gring.get()
            for ee in range(4):
                col = c * 4 + ee
                p.dma(lambda e, gi=gi, ee=ee, col=col: e.indirect_dma_start(
                    out=gbuf[gi][:, ee, :], out_offset=None, in_=vtab[:, :],
                    in_offset=bass.IndirectOffsetOnAxis(ap=eidu[:, col:col + 1], axis=0)),
                    reads=["eidu"], writes=[f"gb{gi}"], q="pool")
            for ee in range(4):
                col = c * 4 + ee
                if col == 0:
                    p.op("dve", lambda e, gi=gi, ee=ee, col=col: e.tensor_scalar(
                        out=acc[:], in0=gbuf[gi][:, ee, :], scalar1=wval[:, col:col + 1], scalar2=None, op0=ALU.mult),
                        reads=[f"gb{gi}", "wval"], writes=["acc"])
                else:
                    p.op("dve", lambda e, gi=gi, ee=ee, col=col: e.scalar_tensor_tensor(
                        out=acc[:], in0=gbuf[gi][:, ee, :], scalar=wval[:, col:col + 1], in1=acc[:],
                        op0=ALU.mult, op1=ALU.add), reads=[f"gb{gi}", "wval"], writes=["acc"])
        p.op("dve", lambda e, s=s: e.tensor_tensor(out=acc[:], in0=acc[:], in1=G2_rep[s][:], op=ALU.mult),
             reads=[f"G{s}"], writes=["acc"])
        p.op("dve", lambda e: e.tensor_tensor(out=acc[:], in0=acc[:], in1=xt[:], op=ALU.add), reads=["xt"], writes=["acc"])
        if final:
            p.op("act", lambda e: e.activation(out=tt[:], in_=acc[:], func=AF.Square, accum_out=small[:, 32:33]),
                 reads=["acc"], writes=["tt", "small"])
            p.op("dve", lambda e: e.tensor_scalar(out=small[:, 33:34], in0=small[:, 32:33], scalar1=1.0 / 1024, scalar2=EPS,
                                                  op0=ALU.mult, op1=ALU.add), reads=[], writes=["small"])
            p.op("act", lambda e: e.activation(out=small[:, 34:35], in_=small[:, 33:34], func=AF.Sqrt), reads=[], writes=["small"])
            p.op("dve", lambda e: e.reciprocal(out=small[:, 35:36], in_=small[:, 34:35]), reads=[], writes=["small"])
            p.op("dve", lambda e: e.scalar_tensor_tensor(out=acc[:], in0=acc[:], scalar=small[:, 35:36], in1=gf[:],
                                                         op0=ALU.mult, op1=ALU.mult), reads=["small", "gf"], writes=["acc"])
        p.store(y[r0:r0 + 128, :], acc[:], "acc")
    return p.finish()


def load_w_bf16(p, dst, src, kc, n, stg, piece=2048):
    v = src.rearrange("(k p) n -> p k n", p=128)
    i = 0
    for k in range(kc):
        for c0 in range(0, n, piece):
            c1 = min(n, c0 + piece)
            st = stg[i % len(stg)]
            i += 1
            p.load(st[:, 0:c1 - c0], v[:, k, c0:c1], st.name)
            eng = "dve" if i % 2 else "act"
            if eng == "dve":
                p.op("dve", lambda e, st=st, k=k, c0=c0, c1=c1: e.tensor_copy(out=dst[:, k, c0:c1], in_=st[:, 0:c1 - c0]),
                     reads=[st.name], writes=[dst.name])
            else:
                p.op("act", lambda e, st=st, k=k, c0=c0, c1=c1: e.activation(out=dst[:, k, c0:c1], in_=st[:, 0:c1 - c0],
                                                                               func=AF.Copy),
                     reads=[st.name], writes=[dst.name])


def fm_rstd(p, src, kcs, n, ones, sq, pb, rstd, inv_n):
    k0, k1 = kcs
    p.op("act", lambda e: e.activation(out=sq[:, k0:k1, 0:n], in_=src[:, k0:k1, 0:n], func=AF.Square),
         reads=[src.name], writes=[sq.name])
    for k in range(k0, k1):
        p.op("pe", lambda e, k=k: e.matmul(pb[:, 0:n], lhsT=ones[:], rhs=sq[:, k, 0:n], start=(k == k0), stop=(k == k1 - 1)),
             reads=[sq.name, ones.name], writes=[pb.name])
    p.op("act", lambda e: e.activation(out=rstd[:, 0:n], in_=pb[:, 0:n], func=AF.Sqrt, scale=inv_n, bias=EPS),
         reads=[pb.name], writes=[rstd.name])
    p.op("dve", lambda e: e.reciprocal(out=rstd[:, 0:n], in_=rstd[:, 0:n]), reads=[], writes=[rstd.name])


def fm_norm_mod(p, xt, n, ones, sq, pb, rstd, A, B, hT):
    fm_rstd(p, xt, (0, 8), n, ones, sq, pb, rstd, 1.0 / 1024)
    p.op("dve", lambda e: e.tensor_tensor(out=sq[:, :, 0:n], in0=xt[:, :, 0:n],
                                          in1=rstd[:, 0:n].unsqueeze(1).to_broadcast([128, 8, n]), op=ALU.mult),
         reads=[xt.name, rstd.name], writes=[sq.name])
    for k in range(8):
        if k % 2 == 0:
            p.op("act", lambda e, k=k: e.activation(out=hT[:, k, 0:n], in_=sq[:, k, 0:n], func=AF.Identity,
                                                    scale=A[:, k:k + 1], bias=B[:, k:k + 1]),
                 reads=[sq.name, "modc"], writes=[hT.name])
        else:
            p.op("dve", lambda e, k=k: e.tensor_scalar(out=hT[:, k, 0:n], in0=sq[:, k, 0:n], scalar1=A[:, k:k + 1],
                                                       scalar2=B[:, k:k + 1], op0=ALU.mult, op1=ALU.add),
                 reads=[sq.name, "modc"], writes=[hT.name])


def load_modc(p, modd, nsets, nv):
    modc = p.sb([128, nsets, nv, 8], F32, "modc")
    p.load(modc[:], modd.rearrange("s v p c -> p s v c"), "modc")
    return modc


def build_ada(NL=4, NCOL=768):
    p = Prog("ada")
    cT = p.dram("cT", [128, 8, 5])
    w = p.dram("w", [NL, 1024, NCOL])
    b = p.dram("b", [NL, 5, NCOL])
    out = p.dram("out", [NL, 5, NCOL], kind="ExternalOutput")
    c_sb = p.sb([128, 8, 5], F32, "c_sb")
    s_sb = p.sb([128, 8, 5], F32, "s_sb")
    w_sb = [p.sb([128, 8, NCOL], F32, f"w_sb{i}") for i in range(2)]
    b_sb = p.sb([5, NL, NCOL], F32, "b_sb")
    o_sb = p.sb([5, NL, NCOL], F32, "o_sb")
    pbs = [p.ps([128, 512], F32, f"pa{i}") for i in range(4)]
    p.load(c_sb[:], cT[:, :, :], "c_sb")
    p.load(b_sb[:], b.rearrange("l r n -> r l n"), "b_sb")
    p.op("act", lambda e: e.activation(out=s_sb[:], in_=c_sb[:], func=AF.Silu), reads=["c_sb"], writes=["s_sb"])
    half = NCOL // 2
    for l in range(NL):
        ws = w_sb[l % 2]
        p.load(ws[:], w[l].rearrange("(k p) n -> p k n", p=128), ws.name)
        for hh in range(2):
            pb = pbs[(l * 2 + hh) % 4]
            for k in range(8):
                p.op("pe", lambda e, pb=pb, k=k, hh=hh, ws=ws: e.matmul(
                    pb[0:5, 0:half], lhsT=s_sb[:, k, :], rhs=ws[:, k, hh * half:(hh + 1) * half],
                    start=(k == 0), stop=(k == 7)), reads=["s_sb", ws.name], writes=[pb.name])
            p.op("dve", lambda e, pb=pb, l=l, hh=hh: e.tensor_tensor(
                out=o_sb[:, l, hh * half:(hh + 1) * half], in0=pb[0:5, 0:half], in1=b_sb[:, l, hh * half:(hh + 1) * half],
                op=ALU.add), reads=[pb.name, "b_sb"], writes=["o_sb"])
    p.store(out.rearrange("l r n -> r l n"), o_sb[:], "o_sb")
    return p.finish()


def build_inproj(blocks, NOB):
    p = Prog("inproj")
    T = max(t0 + n for t0, n, _ in blocks)
    xT = p.dram("xT", [1024, T])
    modd = p.dram("modd", [2, 2, 128, 8])
    gcol = p.dram("gcol", [128, 8])
    wd = p.dram("wd", [1024, NOB * 128])
    onesd = p.dram("onesd", [128, 128])
    proj = p.dram("proj", [NOB * 128, T], kind="ExternalOutput")
    ones = p.sb([128, 128], F32, "ones")
    p.load(ones[:], onesd[:, :], "ones")
    modc = load_modc(p, modd, 2, 2)
    g_sb = p.sb([128, 8], F32, "g_sb")
    p.load(g_sb[:], gcol[:, :], "g_sb")
    Acol = p.sb([128, 2, 8], F32, "Acol")
    for s in range(2):
        p.op("dve", lambda e, s=s: e.scalar_tensor_tensor(out=Acol[:, s, :], in0=modc[:, s, 0, :], scalar=1.0, in1=g_sb[:],
                                                          op0=ALU.add, op1=ALU.mult),
             reads=["modc", "g_sb"], writes=["modc"])
    stg = [p.sb([128, 2048], F32, f"stg{i}") for i in range(2)]
    w_sb = p.sb([128, 8, NOB * 128], BF16, "w_sb")
    load_w_bf16(p, w_sb, wd, 8, NOB * 128, stg)
    xts = [p.sb([128, 8, 512], F32, f"xt{i}") for i in range(2)]
    sq = p.sb([128, 8, 512], F32, "sq")
    rstd = p.sb([128, 512], F32, "rstd")
    hT = p.sb([128, 8, 512], BF16, "hT")
    outs = [p.sb([128, 512], F32, f"ot{i}") for i in range(4)]
    pbs = [p.ps([128, 512], F32, f"pp{i}") for i in range(8)]
    xv = xT.rearrange("(k p) t -> p k t", p=128)
    oi = 0
    for bi, (t0, n, s) in enumerate(blocks):
        xt = xts[bi % 2]
        p.load(xt[:, :, 0:n], xv[:, :, t0:t0 + n], xt.name)
        fm_norm_mod(p, xt, n, ones, sq, pbs[0], rstd, Acol[:, s, :], modc[:, s, 1, :], hT)
        for j in range(NOB):
            pb = pbs[1 + j % 7]
            for k in range(8):
                p.op("pe", lambda e, pb=pb, j=j, k=k: e.matmul(pb[:, 0:n], lhsT=w_sb[:, k, j * 128:(j + 1) * 128],
                                                               rhs=hT[:, k, 0:n], start=(k == 0), stop=(k == 7)),
                     reads=["w_sb", "hT"], writes=[pb.name])
            ot = outs[oi % 4]
            oi += 1
            if oi % 2:
                p.op("act", lambda e, pb=pb, ot=ot: e.activation(out=ot[:, 0:n], in_=pb[:, 0:n], func=AF.Copy),
                     reads=[pb.name], writes=[ot.name])
            else:
                p.op("dve", lambda e, pb=pb, ot=ot: e.tensor_copy(out=ot[:, 0:n], in_=pb[:, 0:n]),
                     reads=[pb.name], writes=[ot.name])
            p.store(proj[j * 128:(j + 1) * 128, t0:t0 + n], ot[:, 0:n], ot.name)
    return p.finish()


def build_ssd(NCH_CTX=2, NCH_LAT=64):
    p = Prog("ssd")
    NCH = NCH_CTX + NCH_LAT
    NTOK = NCH * 128
    xbcT = p.dram("xbcT", [768, NTOK])
    dtr = p.dram("dtr", [NTOK, 8])
    convw = p.dram("convw", [768, 5])
    convb = p.dram("convb", [128, 6])
    rep8 = p.dram("rep8", [128, 5, 8])
    cst = p.dram("cst", [6, 128, 128])
    yf = p.dram("yf", [NTOK, 512], kind="ExternalOutput")
    yb = p.dram("yb", [NTOK, 512], kind="ExternalOutput")

    cs = p.sb([128, 6, 128], F32, "cs")
    p.load(cs[:], cst.rearrange("c p n -> p c n"), "cs")
    ident, ones = cs[:, 0, :], cs[:, 5, :]
    mask01 = p.sb([128, 2, 128], F32, "mask01")
    p.op("dve", lambda e: e.tensor_copy(out=mask01[:], in_=cs[:, 1:3, :]), reads=["cs"], writes=["mask01"])
    dt_all = p.sb([128, NCH, 8], F32, "dt_all")
    p.load(dt_all[:], dtr.rearrange("(c p) h -> p c h", p=128), "dt_all")
    cw = p.sb([128, 6, 5], F32, "cw")
    p.load(cw[:], convw.rearrange("(k p) j -> p k j", p=128), "cw")
    cb = p.sb([128, 6], F32, "cb")
    p.load(cb[:], convb[:, :], "cb")
    r8 = p.sb([128, 5, 8], F32, "r8")
    p.load(r8[:], rep8[:, :, :], "r8")
    aneg = p.sb([128, 2, 8], F32, "aneg")
    p.op("act", lambda e: e.activation(out=aneg[:], in_=r8[:, 2:4, :], func=AF.Exp), reads=["r8"], writes=["aneg"])
    p.op("dve", lambda e: e.tensor_scalar(out=aneg[:], in0=aneg[:], scalar1=-1.0, scalar2=None, op0=ALU.mult),
         reads=[], writes=["aneg"])

    wins = [p.sb([128, 6, 132], F32, f"win{i}") for i in range(2)]
    xc = p.sb([128, 6, 128], F32, "xc")
    acc = p.sb([128, 6, 128], F32, "cacc")
    xs_tm = p.sb([128, 512], F32, "xs_tm")
    btm = p.sb([128, 128], F32, "btm")
    sm = p.sb([128, 96], F32, "sm")
    da_b = p.sb([128, 8, 128], F32, "da_b")
    xdw = p.sb([128, 512], F32, "xdw")
    xd = p.sb([128, 512], F32, "xd")
    cbm = p.sb([128, 128], F32, "cbm")
    arg = p.sb([128, 8, 128], F32, "arg")
    dec = p.sb([128, 8, 128], F32, "dec")
    ys = [p.sb([128, 512], F32, f"y{i}") for i in range(2)]
    ytmp = p.sb([128, 512], F32, "ytmp")
    H = p.sb([128, 512], F32, "H")
    P = [p.ps([128, 512], F32, f"pq{i}") for i in range(8)]
    xv = xbcT.rearrange("(k p) t -> p k t", p=128)

    it = 0
    for d in range(2):
        tri = cs[:, 1 + d, :]
        neg = cs[:, 3 + d, :]
        yout = yf if d == 0 else yb
        order = list(range(NCH)) if d == 0 else list(range(NCH_CTX - 1, -1, -1)) + list(range(NCH - 1, NCH_CTX - 1, -1))
        p.op("dve", lambda e: e.memset(H[:], 0.0), reads=[], writes=["H"])
        for c in order:
            s0, s1 = (0, NCH_CTX) if c < NCH_CTX else (NCH_CTX, NCH)
            win = wins[it % 2]
            yt = ys[it % 2]
            it += 1
            lo = 0 if c > s0 else 2
            hi = 132 if c < s1 - 1 else 130
            if lo or hi < 132:
                p.op("dve", lambda e, win=win: e.memset(win[:], 0.0), reads=[], writes=[win.name])
            p.load(win[:, :, lo:hi], xv[:, :, c * 128 - 2 + lo:c * 128 - 2 + hi], win.name)
            for k in range(6):
                for j in range(5):
                    if j == 0:
                        p.op("dve", lambda e, k=k, j=j, win=win: e.tensor_scalar(
                            out=acc[:, k, :], in0=win[:, k, j:j + 128], scalar1=cw[:, k, j:j + 1], scalar2=None, op0=ALU.mult),
                            reads=[win.name, "cw"], writes=["cacc"])
                    else:
                        p.op("dve", lambda e, k=k, j=j, win=win: e.scalar_tensor_tensor(
                            out=acc[:, k, :], in0=win[:, k, j:j + 128], scalar=cw[:, k, j:j + 1], in1=acc[:, k, :],
                            op0=ALU.mult, op1=ALU.add), reads=[win.name, "cw"], writes=["cacc"])
            for k in range(6):
                p.op("act", lambda e, k=k: e.activation(out=xc[:, k, :], in_=acc[:, k, :], func=AF.Silu, bias=cb[:, k:k + 1]),
                     reads=["cacc", "cb"], writes=["xc"])
            for k in range(4):
                p.op("pe", lambda e, k=k: e.transpose(out=P[0][:, k * 128:(k + 1) * 128], in_=xc[:, k, :], identity=ident),
                     reads=["xc", "cs"], writes=["pq0"])
            p.op("act", lambda e: e.activation(out=xs_tm[:], in_=P[0][:], func=AF.Copy), reads=["pq0"], writes=["xs_tm"])
            p.op("pe", lambda e: e.transpose(out=P[1][:, 0:128], in_=xc[:, 4, :], identity=ident),
                 reads=["xc", "cs"], writes=["pq1"])
            p.op("dve", lambda e: e.tensor_copy(out=btm[:], in_=P[1][:, 0:128]), reads=["pq1"], writes=["btm"])
            p.op("dve", lambda e, c=c, d=d: e.tensor_tensor(out=sm[:, 16:24], in0=dt_all[:, c, :], in1=r8[:, d, :], op=ALU.add),
                 reads=["dt_all", "r8"], writes=["sm"])
            p.op("act", lambda e: e.activation(out=sm[:, 16:24], in_=sm[:, 16:24], func=AF.Exp), reads=[], writes=["sm"])
            p.op("act", lambda e: e.activation(out=sm[:, 0:8], in_=sm[:, 16:24], func=AF.Ln, bias=1.0), reads=[], writes=["sm"])
            p.op("dve", lambda e, d=d: e.tensor_tensor(out=sm[:, 8:16], in0=sm[:, 0:8], in1=aneg[:, d, :], op=ALU.mult),
                 reads=["aneg"], writes=["sm"])
            p.op("pe", lambda e, tri=tri: e.matmul(P[1][:, 128:136], lhsT=tri, rhs=sm[:, 8:16], start=True, stop=True),
                 reads=["sm", "cs"], writes=["pq1"])
            p.op("pe", lambda e: e.matmul(P[1][:, 136:144], lhsT=ones, rhs=sm[:, 8:16], start=True, stop=True),
                 reads=["sm", "cs"], writes=["pq1"])
            p.op("dve", lambda e: e.tensor_copy(out=sm[:, 24:40], in_=P[1][:, 128:144]), reads=["pq1"], writes=["sm"])
            p.op("dve", lambda e: e.tensor_tensor(out=sm[:, 40:48], in0=sm[:, 32:40], in1=sm[:, 24:32], op=ALU.subtract),
                 reads=[], writes=["sm"])
            p.op("act", lambda e: e.activation(out=sm[:, 48:72], in_=sm[:, 24:48], func=AF.Exp), reads=[], writes=["sm"])
            p.op("dve", lambda e: e.tensor_scalar(out=sm[:, 72:80], in0=sm[:, 24:32], scalar1=-1.0, scalar2=None, op0=ALU.mult),
                 reads=[], writes=["sm"])
            p.op("dve", lambda e: e.tensor_tensor(out=sm[:, 80:88], in0=sm[:, 0:8], in1=sm[:, 64:72], op=ALU.mult),
                 reads=[], writes=["sm"])
            ecum, cdec = sm[:, 48:56], sm[:, 56:64]
            xs3 = xs_tm[:].rearrange("p (h q) -> p h q", h=8)
            p.op("dve", lambda e: e.tensor_tensor(out=xdw[:].rearrange("p (h q) -> p h q", h=8), in0=xs3,
                                                  in1=sm[:, 80:88].unsqueeze(2).to_broadcast([128, 8, 64]), op=ALU.mult),
                 reads=["xs_tm", "sm"], writes=["xdw"])
            p.op("dve", lambda e: e.tensor_tensor(out=xd[:].rearrange("p (h q) -> p h q", h=8), in0=xs3,
                                                  in1=sm[:, 0:8].unsqueeze(2).to_broadcast([128, 8, 64]), op=ALU.mult),
                 reads=["xs_tm", "sm"], writes=["xd"])
            p.op("dve", lambda e: e.tensor_copy(out=da_b[:], in_=sm[:, 8:16].unsqueeze(2).to_broadcast([128, 8, 128])),
                 reads=["sm"], writes=["da_b"])
            p.op("pe", lambda e: e.matmul(P[2][:], lhsT=btm[:], rhs=xdw[:], start=True, stop=True),
                 reads=["btm", "xdw"], writes=["pq2"])
            p.op("pe", lambda e: e.matmul(P[3][:], lhsT=xc[:, 5, :], rhs=H[:], start=True, stop=True),
                 reads=["xc", "H"], writes=["pq3"])
            p.op("pe", lambda e: e.matmul(P[1][:, 256:384], lhsT=xc[:, 4, :], rhs=xc[:, 5, :], start=True, stop=True),
                 reads=["xc"], writes=["pq1"])
            p.op("dve", lambda e, d=d: e.tensor_tensor(out=cbm[:], in0=P[1][:, 256:384], in1=mask01[:, d, :], op=ALU.mult),
                 reads=["pq1", "mask01"], writes=["cbm"])
            for hb in range(2):
                pb = P[4 + hb]
                for hh in range(4):
                    h = hb * 4 + hh
                    p.op("pe", lambda e, pb=pb, hh=hh, h=h, tri=tri: e.matmul(
                        pb[:, hh * 128:(hh + 1) * 128], lhsT=da_b[:, h, :], rhs=tri, start=True, stop=True),
                        reads=["da_b", "cs"], writes=[pb.name])
                p.op("dve", lambda e, pb=pb, hb=hb, neg=neg: e.tensor_tensor(
                    out=arg[:, hb * 4:(hb + 1) * 4, :], in0=pb[:].rearrange("p (h t) -> p h t", h=4),
                    in1=neg.unsqueeze(1).to_broadcast([128, 4, 128]), op=ALU.add),
                    reads=[pb.name, "cs"], writes=["arg"])
            for h in range(8):
                p.op("act", lambda e, h=h: e.activation(out=dec[:, h, :], in_=arg[:, h, :], func=AF.Exp, bias=sm[:, 72 + h:73 + h]),
                     reads=["arg", "sm"], writes=["dec"])
            p.op("dve", lambda e: e.tensor_tensor(out=dec[:], in0=dec[:], in1=cbm[:].unsqueeze(1).to_broadcast([128, 8, 128]),
                                                  op=ALU.mult), reads=["cbm"], writes=["dec"])
            for h in range(8):
                p.op("pe", lambda e, h=h: e.matmul(P[6][:, h * 64:(h + 1) * 64], lhsT=dec[:, h, :], rhs=xd[:, h * 64:(h + 1) * 64],
                                                   start=True, stop=True), reads=["dec", "xd"], writes=["pq6"])
            p.op("dve", lambda e: e.tensor_tensor(out=ytmp[:].rearrange("p (h q) -> p h q", h=8),
                                                  in0=P[3][:].rearrange("p (h q) -> p h q", h=8),
                                                  in1=ecum.unsqueeze(2).to_broadcast([128, 8, 64]), op=ALU.mult),
                 reads=["pq3", "sm"], writes=["ytmp"])
            p.op("dve", lambda e, yt=yt: e.tensor_tensor(out=yt[:], in0=ytmp[:], in1=P[6][:], op=ALU.add),
                 reads=["ytmp", "pq6"], writes=[yt.name])
            if d == 0:
                p.op("dve", lambda e: e.tensor_tensor(out=ytmp[:].rearrange("p (h q) -> p h q", h=8), in0=xs3,
                                                      in1=r8[:, 4, :].unsqueeze(2).to_broadcast([128, 8, 64]), op=ALU.mult),
                     reads=["xs_tm", "r8"], writes=["ytmp"])
                p.op("dve", lambda e, yt=yt: e.tensor_tensor(out=yt[:], in0=yt[:], in1=ytmp[:], op=ALU.add),
                     reads=["ytmp"], writes=[yt.name])
            p.store(yout[c * 128:(c + 1) * 128, :], yt[:], yt.name)
            p.op("dve", lambda e: e.tensor_tensor(out=H[:].rearrange("p (h q) -> p h q", h=8),
                                                  in0=H[:].rearrange("p (h q) -> p h q", h=8),
                                                  in1=cdec.unsqueeze(2).to_broadcast([128, 8, 64]), op=ALU.mult),
                 reads=["sm"], writes=["H"])
            p.op("dve", lambda e: e.tensor_tensor(out=H[:], in0=H[:], in1=P[2][:], op=ALU.add), reads=["pq2"], writes=["H"])
    return p.finish()


def build_attn(NCTX=256, NLAT=8192, NH=4, stop=99):
    p = Prog("attn")
    NTOK = NCTX + NLAT
    scale = (128 + 64) ** -0.5
    cqT = p.dram("cqT", [384, NTOK])
    ckvT = p.dram("ckvT", [256, NTOK])
    kr2 = p.dram("kr2", [2, 32, NTOK])
    cosd = p.dram("cosd", [32, NTOK])
    sind = p.dram("sind", [32, NTOK])
    wqn_d = p.dram("wqn", [384, NH * 128])
    wq1_d = p.dram("wq1", [384, NH * 32])
    wq2_d = p.dram("wq2", [384, NH * 32])
    wkn_d = p.dram("wkn", [256, NH * 128])
    wv_d = p.dram("wvd", [256, NH * 128])
    qg_d = p.dram("qg", [128, 3])
    kvg_d = p.dram("kvg", [128, 2])
    ones_d = p.dram("onesd", [128, 128])
    att = p.dram("att", [NH * 128, NTOK], kind="ExternalOutput")

    ones = p.sb([128, 128], F32, "ones")
    p.load(ones[:], ones_d[:, :], "ones")
    onesb = p.sb([128, 128], BF16, "onesb")
    p.op("dve", lambda e: e.tensor_copy(out=onesb[:], in_=ones[:]), reads=["ones"], writes=["onesb"])
    qg = p.sb([128, 3], F32, "qgs")
    p.load(qg[:], qg_d[:, :], "qgs")
    kvg = p.sb([128, 2], F32, "kvgs")
    p.load(kvg[:], kvg_d[:, :], "kvgs")
    stg = [p.sb([128, 2048], F32, f"stg{i}") for i in range(2)]
    wqn = p.sb([128, 3, NH * 128], BF16, "wqn_s")
    wq1 = p.sb([128, 3, NH * 32], BF16, "wq1_s")
    wq2 = p.sb([128, 3, NH * 32], BF16, "wq2_s")
    wkn = p.sb([128, 2, NH * 128], BF16, "wkn_s")
    wv = p.sb([128, 2, NH * 128], BF16, "wv_s")
    load_w_bf16(p, wqn, wqn_d, 3, NH * 128, stg)
    load_w_bf16(p, wq1, wq1_d, 3, NH * 32, stg)
    load_w_bf16(p, wq2, wq2_d, 3, NH * 32, stg)
    load_w_bf16(p, wkn, wkn_d, 2, NH * 128, stg)
    load_w_bf16(p, wv, wv_d, 2, NH * 128, stg)

    ckvn = p.sb([128, 2, NTOK], BF16, "ckvn")
    K1 = p.sb([32, NTOK], BF16, "K1")
    K2 = p.sb([32, NTOK], BF16, "K2")
    KnT = p.sb([128, NTOK], BF16, "KnT")
    Vh = p.sb([128, NTOK // 128, 128], BF16, "Vh")
    cqn = p.sb([128, 3, 512], BF16, "cqn")
    xin = p.sb([128, 3, 512], F32, "xin")
    sq = p.sb([128, 3, 512], F32, "sq")
    rstd = p.sb([128, 512], F32, "rstd")
    XA = p.sb([32, 512], F32, "XA")
    XB = p.sb([32, 512], F32, "XB")
    CS = p.sb([32, 512], F32, "CS")
    SN = p.sb([32, 512], F32, "SN")
    r1 = p.sb([32, 512], F32, "r1")
    r2 = p.sb([32, 512], F32, "r2")
    t1 = p.sb([32, 512], F32, "t1")
    t2 = p.sb([32, 512], F32, "t2")
    QnT = p.sb([128, 512], BF16, "QnT")
    Q1 = p.sb([32, 512], BF16, "Q1")
    Q2 = p.sb([32, 512], BF16, "Q2")
    NM = p.sb([128, 512], BF16, "NM")
    Pts = [p.sb([128, 512], BF16, f"Pt{i}") for i in range(3)]
    osb = p.sb([128, 512], F32, "osb")
    mx = p.sb([128, 8], F32, "mx")
    P = [p.ps([128, 512], F32, f"pw{i}") for i in range(8)]
    p.op("dve", lambda e: e.memset(mx[:], 0.0), reads=[], writes=["mx"])

    blocks = [(t0, min(512, NTOK - t0)) for t0 in range(0, NTOK, 512)]

    def rope32(n, o1, o2, pb):
        p.op("dve", lambda e: e.tensor_tensor(out=t1[:, 0:n], in0=XA[:, 0:n], in1=CS[:, 0:n], op=ALU.mult), reads=["XA", "CS"], writes=["t1"])
        p.op("dve", lambda e: e.tensor_tensor(out=t2[:, 0:n], in0=XB[:, 0:n], in1=SN[:, 0:n], op=ALU.mult), reads=["XB", "SN"], writes=["t2"])
        p.op("dve", lambda e: e.tensor_tensor(out=r1[:, 0:n], in0=t1[:, 0:n], in1=t2[:, 0:n], op=ALU.subtract), reads=["t1", "t2"], writes=["r1"])
        p.op("dve", lambda e: e.tensor_tensor(out=t1[:, 0:n], in0=XB[:, 0:n], in1=CS[:, 0:n], op=ALU.mult), reads=["XB", "CS"], writes=["t1"])
        p.op("dve", lambda e: e.tensor_tensor(out=t2[:, 0:n], in0=XA[:, 0:n], in1=SN[:, 0:n], op=ALU.mult), reads=["XA", "SN"], writes=["t2"])
        p.op("dve", lambda e: e.tensor_tensor(out=r2[:, 0:n], in0=t1[:, 0:n], in1=t2[:, 0:n], op=ALU.add), reads=["t1", "t2"], writes=["r2"])
        p.op("act", lambda e: e.activation(out=o1, in_=r1[:, 0:n], func=AF.Copy), reads=["r1"], writes=[o1.name])
        p.op("act", lambda e: e.activation(out=o2, in_=r2[:, 0:n], func=AF.Copy), reads=["r2"], writes=[o2.name])
        p.op("act", lambda e: e.activation(out=t1[:, 0:n], in_=r1[:, 0:n], func=AF.Square), reads=["r1"], writes=["t1"])
        p.op("act", lambda e: e.activation(out=t2[:, 0:n], in_=r2[:, 0:n], func=AF.Square), reads=["r2"], writes=["t2"])
        p.op("pe", lambda e: e.matmul(pb[:, 0:n], lhsT=ones[0:32, :], rhs=t1[:, 0:n], start=False, stop=False),
             reads=["t1", "ones"], writes=[pb.name])
        p.op("pe", lambda e: e.matmul(pb[:, 0:n], lhsT=ones[0:32, :], rhs=t2[:, 0:n], start=False, stop=True),
             reads=["t2", "ones"], writes=[pb.name])

    def load_rope_tables(t0, n):
        p.load(CS[:, 0:n], cosd[:, t0:t0 + n], "CS")
        p.load(SN[:, 0:n], sind[:, t0:t0 + n], "SN")

    def running_max(pb, n, col):
        p.op("dve", lambda e: e.tensor_reduce(out=mx[:, 2:3], in_=pb[:, 0:n], axis=AX.X, op=ALU.max),
             reads=[pb.name], writes=["mx"])
        p.op("dve", lambda e: e.tensor_tensor(out=mx[:, col:col + 1], in0=mx[:, col:col + 1], in1=mx[:, 2:3], op=ALU.max),
             reads=[], writes=["mx"])

    def norm_block(src, kc, gcol, dst_fn, inv, t0, n):
        p.load(xin[:, 0:kc, 0:n], src[:, :, t0:t0 + n], "xin")
        fm_rstd(p, xin, (0, kc), n, ones, sq, P[5], rstd, inv)
        p.op("dve", lambda e: e.tensor_tensor(out=sq[:, 0:kc, 0:n], in0=xin[:, 0:kc, 0:n],
                                              in1=rstd[:, 0:n].unsqueeze(1).to_broadcast([128, kc, n]), op=ALU.mult),
             reads=["xin", "rstd"], writes=["sq"])
        for k in range(kc):
            dst = dst_fn(k)
            p.op("act", lambda e, k=k, dst=dst: e.activation(out=dst, in_=sq[:, k, 0:n], func=AF.Identity, scale=gcol[:, k:k + 1]),
                 reads=["sq", gcol.name], writes=[dst.name])

    cqv = cqT.rearrange("(k p) t -> p k t", p=128)
    ckvv = ckvT.rearrange("(k p) t -> p k t", p=128)
    zero_ap = ones
    for (t0, n) in blocks:
        norm_block(ckvv, 2, kvg, lambda k: ckvn[:, k, t0:t0 + n], 1.0 / 256, t0, n)
        load_rope_tables(t0, n)
        p.load(XA[:, 0:n], kr2[0, :, t0:t0 + n], "XA")
        p.load(XB[:, 0:n], kr2[1, :, t0:t0 + n], "XB")
        p.op("act", lambda e: e.activation(out=t1[:, 0:n], in_=XA[:, 0:n], func=AF.Copy, scale=0.0), reads=["XA"], writes=["t1"])
        p.op("pe", lambda e: e.matmul(P[6][:, 0:n], lhsT=ones[0:32, :], rhs=t1[:, 0:n], start=True, stop=False),
             reads=["t1", "ones"], writes=["pw6"])
        rope32(n, K1[:, t0:t0 + n], K2[:, t0:t0 + n], P[6])
        running_max(P[6], n, 0)
    if stop == 0:
        return p.finish()

    qblocks = [(0, NCTX, NCTX // 128)] + [(NCTX + i * 512, 512, NTOK // 128) for i in range(NLAT // 512)]
    pi = 0
    for h in range(NH):
        p.op("dve", lambda e: e.memset(mx[:, 1:2], 0.0), reads=[], writes=["mx"])
        for (t0, n) in blocks:
            pb = P[5]
            for k in range(2):
                p.op("pe", lambda e, k=k: e.matmul(pb[:, 0:n], lhsT=wkn[:, k, h * 128:(h + 1) * 128], rhs=ckvn[:, k, t0:t0 + n],
                                                   start=(k == 0), stop=(k == 1)), reads=["wkn_s", "ckvn"], writes=[pb.name])
            p.op("act", lambda e: e.activation(out=KnT[:, t0:t0 + n], in_=pb[:, 0:n], func=AF.Copy), reads=[pb.name], writes=["KnT"])
            p.op("act", lambda e: e.activation(out=sq[:, 0, 0:n], in_=pb[:, 0:n], func=AF.Square), reads=[pb.name], writes=["sq"])
            p.op("pe", lambda e: e.matmul(P[6][:, 0:n], lhsT=ones[:], rhs=sq[:, 0, 0:n], start=True, stop=True),
                 reads=["sq", "ones"], writes=["pw6"])
            running_max(P[6], n, 1)
            pv = P[7]
            nt = n // 128
            for j in range(nt):
                for k in range(2):
                    p.op("pe", lambda e, j=j, k=k: e.matmul(
                        pv[:, j * 128:(j + 1) * 128], lhsT=ckvn[:, k, t0 + j * 128:t0 + (j + 1) * 128],
                        rhs=wv[:, k, h * 128:(h + 1) * 128], start=(k == 0), stop=(k == 1)),
                        reads=["wv_s", "ckvn"], writes=["pw7"])
            p.op("dve", lambda e, nt=nt: e.tensor_copy(out=Vh[:, t0 // 128:t0 // 128 + nt, :],
                                                      in_=pv[:, 0:nt * 128].rearrange("p (j d) -> p j d", j=nt)),
                 reads=["pw7"], writes=["Vh"])
        p.op("dve", lambda e: e.tensor_tensor(out=mx[:, 4:5], in0=mx[:, 0:1], in1=mx[:, 1:2], op=ALU.add), reads=[], writes=["mx"])
        p.op("act", lambda e: e.activation(out=mx[:, 4:5], in_=mx[:, 4:5], func=AF.Sqrt), reads=[], writes=["mx"])
        p.op("dve", lambda e: e.tensor_scalar(out=mx[:, 3:4], in0=mx[:, 4:5], scalar1=-1.0, scalar2=None, op0=ALU.mult),
             reads=[], writes=["mx"])
        if stop == 1:
            return p.finish()
        for (q0, n, nkt) in qblocks:
            norm_block(cqv, 3, qg, lambda k: cqn[:, k, 0:n], 1.0 / 384, q0, n)
            pb = P[5]
            for k in range(3):
                p.op("pe", lambda e, k=k: e.matmul(pb[:, 0:n], lhsT=wqn[:, k, h * 128:(h + 1) * 128], rhs=cqn[:, k, 0:n],
                                                   start=(k == 0), stop=(k == 2)), reads=["wqn_s", "cqn"], writes=[pb.name])
            p.op("act", lambda e: e.activation(out=QnT[:, 0:n], in_=pb[:, 0:n], func=AF.Copy), reads=[pb.name], writes=["QnT"])
            p.op("act", lambda e: e.activation(out=sq[:, 0, 0:n], in_=pb[:, 0:n], func=AF.Square), reads=[pb.name], writes=["sq"])
            for (wsb, pq, dst) in ((wq1, P[6], XA), (wq2, P[7], XB)):
                for k in range(3):
                    p.op("pe", lambda e, k=k, wsb=wsb, pq=pq: e.matmul(
                        pq[0:32, 0:n], lhsT=wsb[:, k, h * 32:(h + 1) * 32], rhs=cqn[:, k, 0:n],
                        start=(k == 0), stop=(k == 2)), reads=[wsb.name, "cqn"], writes=[pq.name])
                p.op("dve", lambda e, pq=pq, dst=dst: e.tensor_copy(out=dst[:, 0:n], in_=pq[0:32, 0:n]), reads=[pq.name], writes=[dst.name])
            load_rope_tables(q0, n)
            p.op("pe", lambda e: e.matmul(P[5][:, 0:n], lhsT=ones[:], rhs=sq[:, 0, 0:n], start=True, stop=False),
                 reads=["sq", "ones"], writes=["pw5"])
            rope32(n, Q1[:, 0:n], Q2[:, 0:n], P[5])
            p.op("act", lambda e: e.activation(out=rstd[:, 0:n], in_=P[5][:, 0:n], func=AF.Sqrt), reads=["pw5"], writes=["rstd"])
            p.op("dve", lambda e: e.tensor_scalar(out=NM[:, 0:n], in0=rstd[:, 0:n], scalar1=mx[:, 3:4], scalar2=None,
                                                  op0=ALU.mult), reads=["rstd", "mx"], writes=["NM"])
            if stop == 2:
                return p.finish()
            po, pd = P[3], P[4]
            for kt in range(nkt):
                ps = P[pi % 3]
                pt = Pts[pi % 3]
                pi += 1
                ks = slice(kt * 128, (kt + 1) * 128)
                p.op("pe", lambda e, ps=ps, ks=ks: e.matmul(ps[:, 0:n], lhsT=KnT[:, ks], rhs=QnT[:, 0:n], start=True, stop=False),
                     reads=["KnT", "QnT"], writes=[ps.name])
                p.op("pe", lambda e, ps=ps, ks=ks: e.matmul(ps[:, 0:n], lhsT=K1[:, ks], rhs=Q1[:, 0:n], start=False, stop=False),
                     reads=["K1", "Q1"], writes=[ps.name])
                p.op("pe", lambda e, ps=ps, ks=ks: e.matmul(ps[:, 0:n], lhsT=K2[:, ks], rhs=Q2[:, 0:n], start=False, stop=False),
                     reads=["K2", "Q2"], writes=[ps.name])
                p.op("pe", lambda e, ps=ps: e.matmul(ps[:, 0:n], lhsT=onesb[0:1, :], rhs=NM[0:1, 0:n], start=False, stop=True),
                     reads=["onesb", "NM"], writes=[ps.name])
                p.op("act", lambda e, ps=ps, pt=pt: e.activation(out=pt[:, 0:n], in_=ps[:, 0:n], func=AF.Exp, scale=scale),
                     reads=[ps.name], writes=[pt.name])
                p.op("pe", lambda e, pt=pt, kt=kt: e.matmul(po[:, 0:n], lhsT=Vh[:, kt, :], rhs=pt[:, 0:n],
                                                            start=(kt == 0), stop=(kt == nkt - 1)),
                     reads=["Vh", pt.name], writes=[po.name])
                p.op("pe", lambda e, pt=pt, kt=kt: e.matmul(pd[:, 0:n], lhsT=onesb[:], rhs=pt[:, 0:n],
                                                            start=(kt == 0), stop=(kt == nkt - 1)),
                     reads=["onesb", pt.name], writes=[pd.name])
            p.op("dve", lambda e: e.reciprocal(out=rstd[:, 0:n], in_=pd[:, 0:n]), reads=[pd.name], writes=["rstd"])
            p.op("dve", lambda e: e.tensor_tensor(out=osb[:, 0:n], in0=po[:, 0:n], in1=rstd[:, 0:n], op=ALU.mult),
                 reads=[po.name, "rstd"], writes=["osb"])
            p.store(att[h * 128:(h + 1) * 128, q0:q0 + n], osb[:, 0:n], "osb")
    return p.finish()


def build_outproj(blocks):
    p = Prog("outproj")
    T = max(t0 + n for t0, n, _ in blocks)
    yfT = p.dram("yfT", [1024, T])
    ybT = p.dram("ybT", [1024, T])
    zT = p.dram("zT", [1024, T])
    attT = p.dram("attT", [1024, T])
    xT = p.dram("xT", [1024, T])
    wd = p.dram("wd", [2048, 1024])
    ngd = p.dram("ngd", [128, 8])
    modd = p.dram("modd", [2, 1, 128, 8])
    onesd = p.dram("onesd", [128, 128])
    xo = p.dram("xo", [1024, T], kind="ExternalOutput")
    ones = p.sb([128, 128], F32, "ones")
    p.load(ones[:], onesd[:, :], "ones")
    modc = load_modc(p, modd, 2, 1)
    ng = p.sb([128, 8], F32, "ng")
    p.load(ng[:], ngd[:, :], "ng")
    stg = [p.sb([128, 2048], F32, f"stg{i}") for i in range(2)]
    w_sb = p.sb([128, 16, 1024], BF16, "w_sb")
    load_w_bf16(p, w_sb, wd, 16, 1024, stg)
    yf = p.sb([128, 8, 512], F32, "yf")
    yb = p.sb([128, 8, 512], F32, "yb")
    zt = p.sb([128, 8, 512], F32, "zt")
    at = p.sb([128, 8, 512], F32, "at")
    xt = p.sb([128, 8, 512], F32, "xt")
    sq = p.sb([128, 8, 512], F32, "sq")
    rs = [p.sb([128, 512], F32, f"rs{i}") for i in range(2)]
    cat = p.sb([128, 16, 512], BF16, "cat")
    pbs = [p.ps([128, 512], F32, f"pp{i}") for i in range(8)]
    v = lambda a: a.rearrange("(k p) t -> p k t", p=128)
    for (t0, n, s) in blocks:
        for (tile, src) in ((yf, yfT), (yb, ybT), (zt, zT), (at, attT), (xt, xT)):
            p.load(tile[:, :, 0:n], v(src)[:, :, t0:t0 + n], tile.name)
        p.op("dve", lambda e: e.tensor_tensor(out=yf[:, :, 0:n], in0=yf[:, :, 0:n], in1=yb[:, :, 0:n], op=ALU.add),
             reads=["yb"], writes=["yf"])
        p.op("act", lambda e: e.activation(out=zt[:, :, 0:n], in_=zt[:, :, 0:n], func=AF.Silu), reads=[], writes=["zt"])
        p.op("dve", lambda e: e.tensor_tensor(out=yf[:, :, 0:n], in0=yf[:, :, 0:n], in1=zt[:, :, 0:n], op=ALU.mult),
             reads=["zt"], writes=["yf"])
        for g in range(2):
            fm_rstd(p, yf, (g * 4, g * 4 + 4), n, ones, sq, pbs[g], rs[g], 1.0 / 512)
        for g in range(2):
            p.op("dve", lambda e, g=g: e.tensor_tensor(out=sq[:, g * 4:g * 4 + 4, 0:n], in0=yf[:, g * 4:g * 4 + 4, 0:n],
                                                       in1=rs[g][:, 0:n].unsqueeze(1).to_broadcast([128, 4, n]), op=ALU.mult),
                 reads=["yf", rs[g].name], writes=["sq"])
        for k in range(8):
            p.op("act", lambda e, k=k: e.activation(out=cat[:, k, 0:n], in_=sq[:, k, 0:n], func=AF.Identity, scale=ng[:, k:k + 1]),
                 reads=["sq", "ng"], writes=["cat"])
        p.op("dve", lambda e: e.tensor_copy(out=cat[:, 8:16, 0:n], in_=at[:, :, 0:n]), reads=["at"], writes=["cat"])
        for j in range(8):
            pb = pbs[2 + j % 6]
            for k in range(16):
                p.op("pe", lambda e, pb=pb, j=j, k=k: e.matmul(pb[:, 0:n], lhsT=w_sb[:, k, j * 128:(j + 1) * 128],
                                                               rhs=cat[:, k, 0:n], start=(k == 0), stop=(k == 15)),
                     reads=["w_sb", "cat"], writes=[pb.name])
            p.op("dve", lambda e, pb=pb, j=j, s=s: e.scalar_tensor_tensor(
                out=xt[:, j, 0:n], in0=pb[:, 0:n], scalar=modc[:, s, 0, j:j + 1], in1=xt[:, j, 0:n], op0=ALU.mult, op1=ALU.add),
                reads=[pb.name, "modc"], writes=["xt"])
        p.store(v(xo)[:, :, t0:t0 + n], xt[:, :, 0:n], "xt")
    return p.finish()


def build_gmlp(kinds):
    p = Prog("gmlp")
    NCHK = len(kinds)
    T = NCHK * 128
    xT = p.dram("xT", [1024, T])
    modd = p.dram("modd", [2, 3, 128, 8])
    gcol = p.dram("gcol", [128, 8])
    wind = p.dram("wind", [1024, 4096])
    woutd = p.dram("woutd", [2048, 1024])
    wsT = p.dram("wsT", [128, 8, 128])
    reps = p.dram("reps", [2, 128, 2048])
    bsr = p.dram("bsr", [128, 8, 128])
    onesd = p.dram("onesd", [128, 128])
    xo = p.dram("xo", [1024, T], kind="ExternalOutput")
    ones = p.sb([128, 128], F32, "ones")
    p.load(ones[:], onesd[:, :], "ones")
    modc = load_modc(p, modd, 2, 3)
    g_sb = p.sb([128, 8], F32, "g_sb")
    p.load(g_sb[:], gcol[:, :], "g_sb")
    Acol = p.sb([128, 2, 8], F32, "Acol")
    for s in range(2):
        p.op("dve", lambda e, s=s: e.scalar_tensor_tensor(out=Acol[:, s, :], in0=modc[:, s, 0, :], scalar=1.0, in1=g_sb[:],
                                                          op0=ALU.add, op1=ALU.mult),
             reads=["modc", "g_sb"], writes=["modc"])
    stg = [p.sb([128, 2048], F32, f"stg{i}") for i in range(2)]
    win = p.sb([128, 8, 4096], BF16, "win")
    wout = p.sb([128, 16, 1024], BF16, "wout")
    load_w_bf16(p, win, wind, 8, 4096, stg)
    load_w_bf16(p, wout, woutd, 16, 1024, stg)
    ws_sb = p.sb([128, 8, 128], F32, "ws_sb")
    p.load(ws_sb[:], wsT[:, :, :], "ws_sb")
    lng = p.sb([128, 2048], F32, "lng")
    lnb = p.sb([128, 2048], F32, "lnb")
    p.load(lng[:], reps[0], "lng")
    p.load(lnb[:], reps[1], "lnb")
    bs_sb = p.sb([128, 8, 128], F32, "bs_sb")
    p.load(bs_sb[:], bsr[:, :, :], "bs_sb")
    xts = [p.sb([128, 8, 128], F32, f"xt{i}") for i in range(2)]
    sq = p.sb([128, 8, 128], F32, "sq")
    rstd = p.sb([128, 128], F32, "rstd")
    hT = p.sb([128, 8, 128], BF16, "hT")
    uT = p.sb([128, 16, 128], F32, "uT")
    vt = p.sb([128, 2048], F32, "vt")
    junk = p.sb([128, 2048], F32, "junk")
    st = p.sb([128, 16], F32, "st")
    gated = p.sb([128, 16, 128], BF16, "gated")
    tmp = p.sb([128, 512], F32, "tmp")
    pbs = [p.ps([128, 512], F32, f"pp{i}") for i in range(8)]
    xv = xT.rearrange("(k p) t -> p k t", p=128)
    xov = xo.rearrange("(k p) t -> p k t", p=128)
    for ci, s in enumerate(kinds):
        t0 = ci * 128
        xt = xts[ci % 2]
        p.load(xt[:], xv[:, :, t0:t0 + 128], xt.name)
        fm_norm_mod(p, xt, 128, ones, sq, pbs[0], rstd, Acol[:, s, :], modc[:, s, 1, :], hT)
        for jb in range(4):
            pb = pbs[1 + jb % 3]
            for jj in range(4):
                j = jb * 4 + jj
                for k in range(8):
                    p.op("pe", lambda e, pb=pb, jj=jj, j=j, k=k: e.matmul(
                        pb[:, jj * 128:(jj + 1) * 128], lhsT=win[:, k, j * 128:(j + 1) * 128], rhs=hT[:, k, :],
                        start=(k == 0), stop=(k == 7)), reads=["win", "hT"], writes=[pb.name])
            p.op("act", lambda e, pb=pb, jb=jb: e.activation(out=uT[:, jb * 4:(jb + 1) * 4, :],
                                                            in_=pb[:].rearrange("p (j t) -> p j t", j=4), func=AF.Gelu),
                 reads=[pb.name], writes=["uT"])
        for cb in range(4):
            pb = pbs[4 + cb % 2]
            for k in range(8):
                p.op("pe", lambda e, pb=pb, cb=cb, k=k: e.matmul(
                    pb[:], lhsT=hT[:, k, :], rhs=win[:, k, 2048 + cb * 512:2048 + (cb + 1) * 512],
                    start=(k == 0), stop=(k == 7)), reads=["win", "hT"], writes=[pb.name])
            p.op("act", lambda e, pb=pb, cb=cb: e.activation(out=vt[:, cb * 512:(cb + 1) * 512], in_=pb[:], func=AF.Gelu),
                 reads=[pb.name], writes=["vt"])
        p.op("dve", lambda e: e.tensor_reduce(out=st[:, 0:1], in_=vt[:], axis=AX.X, op=ALU.add), reads=["vt"], writes=["st"])
        p.op("act", lambda e: e.activation(out=junk[:], in_=vt[:], func=AF.Square), reads=["vt"], writes=["junk"])
        p.op("dve", lambda e: e.tensor_reduce(out=st[:, 1:2], in_=junk[:], axis=AX.X, op=ALU.add), reads=["junk"], writes=["st"])
        p.op("dve", lambda e: e.tensor_scalar(out=st[:, 2:4], in0=st[:, 0:2], scalar1=1.0 / 2048, scalar2=None, op0=ALU.mult),
             reads=[], writes=["st"])
        p.op("dve", lambda e: e.tensor_tensor(out=st[:, 4:5], in0=st[:, 2:3], in1=st[:, 2:3], op=ALU.mult), reads=[], writes=["st"])
        p.op("dve", lambda e: e.tensor_tensor(out=st[:, 5:6], in0=st[:, 3:4], in1=st[:, 4:5], op=ALU.subtract), reads=[], writes=["st"])
        p.op("act", lambda e: e.activation(out=st[:, 6:7], in_=st[:, 5:6], func=AF.Sqrt, bias=EPS), reads=[], writes=["st"])
        p.op("dve", lambda e: e.reciprocal(out=st[:, 7:8], in_=st[:, 6:7]), reads=[], writes=["st"])
        p.op("dve", lambda e: e.tensor_scalar(out=vt[:], in0=vt[:], scalar1=st[:, 2:3], scalar2=st[:, 7:8], op0=ALU.subtract,
                                              op1=ALU.mult), reads=["st"], writes=["vt"])
        p.op("dve", lambda e: e.tensor_tensor(out=vt[:], in0=vt[:], in1=lng[:], op=ALU.mult), reads=["lng"], writes=["vt"])
        p.op("dve", lambda e: e.tensor_tensor(out=vt[:], in0=vt[:], in1=lnb[:], op=ALU.add), reads=["lnb"], writes=["vt"])
        for jb in range(4):
            pb = pbs[6 + jb % 2]
            for jj in range(4):
                j = jb * 4 + jj
                p.op("pe", lambda e, pb=pb, jj=jj, j=j: e.matmul(pb[:, jj * 128:(jj + 1) * 128], lhsT=vt[:, j * 128:(j + 1) * 128],
                                                                 rhs=ws_sb[:, j // 2, :], start=True, stop=True),
                     reads=["vt", "ws_sb"], writes=[pb.name])
            p.op("dve", lambda e, pb=pb, jb=jb: e.tensor_tensor(
                out=tmp[:].rearrange("p (g r t) -> p g r t", g=2, r=2), in0=pb[:].rearrange("p (g r t) -> p g r t", g=2, r=2),
                in1=bs_sb[:, jb * 2:jb * 2 + 2, :].unsqueeze(2).to_broadcast([128, 2, 2, 128]), op=ALU.add),
                reads=[pb.name, "bs_sb"], writes=["tmp"])
            p.op("dve", lambda e, jb=jb: e.tensor_tensor(out=gated[:, jb * 4:(jb + 1) * 4, :],
                                                         in0=tmp[:].rearrange("p (j t) -> p j t", j=4),
                                                         in1=uT[:, jb * 4:(jb + 1) * 4, :], op=ALU.mult),
                 reads=["tmp", "uT"], writes=["gated"])
        for jb in range(2):
            pb = pbs[1 + jb]
            for jj in range(4):
                j = jb * 4 + jj
                for k in range(16):
                    p.op("pe", lambda e, pb=pb, jj=jj, j=j, k=k: e.matmul(
                        pb[:, jj * 128:(jj + 1) * 128], lhsT=wout[:, k, j * 128:(j + 1) * 128], rhs=gated[:, k, :],
                        start=(k == 0), stop=(k == 15)), reads=["wout", "gated"], writes=[pb.name])
            for jj in range(4):
                j = jb * 4 + jj
                p.op("dve", lambda e, pb=pb, jj=jj, j=j, s=s: e.scalar_tensor_tensor(
                    out=xt[:, j, :], in0=pb[:, jj * 128:(jj + 1) * 128], scalar=modc[:, s, 2, j:j + 1], in1=xt[:, j, :],
                    op0=ALU.mult, op1=ALU.add), reads=[pb.name, "modc"], writes=[xt.name])
        p.store(xov[:, :, t0:t0 + 128], xt[:], xt.name)
    return p.finish()


def _colv(v, k=8):
    return np.ascontiguousarray(np.asarray(v, np.float32).reshape(k, 128).T)


def _rows(v, n=128):
    return np.ascontiguousarray(np.broadcast_to(np.asarray(v, np.float32)[None, :], (n, v.shape[0])))


def _rope_tables(n_lat, n_ctx):
    grid_w, n_freq = 64, 16
    rows = n_lat // grid_w
    row = np.broadcast_to(np.arange(rows, dtype=np.float32)[:, None], (rows, grid_w)).reshape(-1)
    col = np.broadcast_to(np.arange(grid_w, dtype=np.float32)[None, :], (rows, grid_w)).reshape(-1)
    inv = (np.float32(10000.0) ** (-np.arange(n_freq, dtype=np.float32) / np.float32(n_freq))).astype(np.float32)
    ang = np.concatenate([row[:, None] * inv, col[:, None] * inv], axis=-1).astype(np.float32)
    cos = np.concatenate([np.ones((n_ctx, 32), np.float32), np.cos(ang).astype(np.float32)], 0)
    sin = np.concatenate([np.zeros((n_ctx, 32), np.float32), np.sin(ang).astype(np.float32)], 0)
    return np.ascontiguousarray(cos.T), np.ascontiguousarray(sin.T)


def _ssd_consts():
    s = np.arange(128)[:, None]
    t = np.arange(128)[None, :]
    trif = (s <= t).astype(np.float32)
    trib = (s >= t).astype(np.float32)
    return np.stack([np.eye(128, dtype=np.float32), trif, trib, (1 - trif) * np.float32(-30000.0),
                     (1 - trib) * np.float32(-30000.0), np.ones((128, 128), np.float32)]).astype(np.float32)


def kernel_unfused(x, c, ctx, c_ctx, ada_w, ada_b, norm1_g, norm2_g, hyb_w_in, ssd_conv_w, ssd_conv_b,
           ssd_a_log, ssd_dt_bias, ssd_d, ssd_norm_g, mla_q_norm_g, mla_w_qb, mla_kv_norm_g, mla_w_kvb,
           hyb_w_out, gm_w_in, gm_ln_g, gm_ln_b, gm_ws, gm_bs, gm_w_out, peer_wq, peer_k1, peer_k2,
           peer_u, peer_v, final_norm_g):
    f32 = lambda a: np.ascontiguousarray(np.asarray(a, dtype=np.float32))
    x, c, ctx, c_ctx = f32(x), f32(c), f32(ctx), f32(c_ctx)
    B, L, D = x.shape
    LC = ctx.shape[1]
    depth = ada_w.shape[0]
    HL, HC = L // 2, LC // 2
    NTOK = LC + L
    ones128 = np.ones((128, 128), np.float32)
    ident = np.eye(128, dtype=np.float32)

    cv = np.concatenate([c, c_ctx[None]], 0)
    cT = np.ascontiguousarray(cv.T.reshape(8, 128, B + 1).transpose(1, 0, 2))
    ncol = 6 * D // NCORES
    ada_w, ada_b = f32(ada_w), f32(ada_b)
    ims = [dict(cT=cT, w=np.ascontiguousarray(ada_w[:, :, ci * ncol:(ci + 1) * ncol]),
                b=np.ascontiguousarray(np.broadcast_to(ada_b[:, None, ci * ncol:(ci + 1) * ncol], (depth, B + 1, ncol))))
           for ci in range(NCORES)]
    res = _run(build_ada(depth, ncol), ims)
    mod = np.concatenate([r["out"] for r in res], axis=2)
    mvec = lambda l, r, m: mod[l, r, m * D:(m + 1) * D]

    X = [np.concatenate([x[ci // 2, (ci % 2) * HL:(ci % 2 + 1) * HL], ctx[ci // 2, (ci % 2) * HC:(ci % 2 + 1) * HC]], 0)
         for ci in range(NCORES)]
    TPC = HL + HC
    blocks = [(t0, 512, 0) for t0 in range(0, HL, 512)] + [(HL, HC, 1)]
    kinds = [0] * (HL // 128) + [1] * (HC // 128)

    def to_batch(per_core):
        out = []
        for b in range(B):
            fb = np.empty((per_core[0].shape[0], NTOK), np.float32)
            for hf in range(2):
                fc = per_core[2 * b + hf]
                fb[:, LC + hf * HL:LC + (hf + 1) * HL] = fc[:, :HL]
                fb[:, hf * HC:(hf + 1) * HC] = fc[:, HL:]
            out.append(fb)
        return out

    def to_core(fb, hf):
        return np.ascontiguousarray(np.concatenate([fb[:, LC + hf * HL:LC + (hf + 1) * HL], fb[:, hf * HC:(hf + 1) * HC]], 1))

    nc_inproj = nc_ssd = nc_attn = nc_outproj = nc_gmlp = None
    cosT, sinT = _rope_tables(L, LC)
    for layer in range(depth):
        i = layer // 2
        XT = [np.ascontiguousarray(xc_.T) for xc_ in X]
        if layer % 2 == 0:
            if nc_inproj is None:
                nc_inproj = build_inproj(blocks, 26)
            W = np.zeros((D, 26 * 128), np.float32)
            W[:, :hyb_w_in.shape[2]] = hyb_w_in[i]
            ims = []
            for ci in range(NCORES):
                b = ci // 2
                modd = np.stack([np.stack([_colv(mvec(layer, r, 1)), _colv(mvec(layer, r, 0))]) for r in (b, B)])
                ims.append(dict(xT=XT[ci], modd=modd, gcol=_colv(norm1_g[layer]), wd=W, onesd=ones128))
            proj = [r["proj"] for r in _run(nc_inproj, ims)]
            Fb = to_batch(proj)
            if nc_ssd is None:
                nc_ssd = build_ssd(LC // 128, L // 128)
            cst = _ssd_consts()
            cw, cb = f32(ssd_conv_w[i]), f32(ssd_conv_b[i])
            ims = []
            for ci in range(NCORES):
                b, g = ci // 2, ci % 2
                rowsel = np.concatenate([1024 + g * 512 + np.arange(512), 2048 + g * 128 + np.arange(128),
                                         2304 + g * 128 + np.arange(128)])
                chsel = rowsel - 1024
                hs = slice(g * 8, (g + 1) * 8)
                rep8 = np.ascontiguousarray(np.broadcast_to(
                    np.stack([ssd_dt_bias[i][0, hs], ssd_dt_bias[i][1, hs], ssd_a_log[i][0, hs], ssd_a_log[i][1, hs],
                              ssd_d[i][hs]]).astype(np.float32)[None], (128, 5, 8)))
                ims.append(dict(xbcT=np.ascontiguousarray(Fb[b][rowsel]),
                                dtr=np.ascontiguousarray(Fb[b][2560 + g * 8:2560 + (g + 1) * 8].T),
                                convw=np.ascontiguousarray(cw[chsel]), convb=np.ascontiguousarray(cb[chsel].reshape(6, 128).T),
                                rep8=rep8, cst=cst))
            rs = _run(nc_ssd, ims)
            yfb = [np.ascontiguousarray(np.concatenate([rs[2 * b]["yf"], rs[2 * b + 1]["yf"]], 1).T) for b in range(B)]
            ybb = [np.ascontiguousarray(np.concatenate([rs[2 * b]["yb"], rs[2 * b + 1]["yb"]], 1).T) for b in range(B)]
            if nc_attn is None:
                nc_attn = build_attn(LC, L, 4)
            wqb_, wkvb_ = f32(mla_w_qb[i]), f32(mla_w_kvb[i])
            ims = []
            for ci in range(NCORES):
                b, hh = ci // 2, ci % 2
                heads = range(hh * 4, hh * 4 + 4)
                cat = lambda w, lo, hi, st: np.ascontiguousarray(np.concatenate([w[:, h * st + lo:h * st + hi] for h in heads], 1))
                ims.append(dict(cqT=np.ascontiguousarray(Fb[b][2576:2960]), ckvT=np.ascontiguousarray(Fb[b][2960:3216]),
                                kr2=np.ascontiguousarray(Fb[b][3216:3280].reshape(2, 32, NTOK)), cosd=cosT, sind=sinT,
                                wqn=cat(wqb_, 0, 128, 192), wq1=cat(wqb_, 128, 160, 192), wq2=cat(wqb_, 160, 192, 192),
                                wkn=cat(wkvb_, 0, 128, 256), wvd=cat(wkvb_, 128, 256, 256),
                                qg=_colv(mla_q_norm_g[i], 3), kvg=_colv(mla_kv_norm_g[i], 2), onesd=ones128))
            rs = _run(nc_attn, ims)
            attb = [np.concatenate([rs[2 * b]["att"], rs[2 * b + 1]["att"]], 0) for b in range(B)]
            del Fb
            if nc_outproj is None:
                nc_outproj = build_outproj(blocks)
            ims = []
            for ci in range(NCORES):
                b, hf = ci // 2, ci % 2
                modd = np.stack([_colv(mvec(layer, r, 2))[None] for r in (b, B)])
                ims.append(dict(yfT=to_core(yfb[b], hf), ybT=to_core(ybb[b], hf), zT=np.ascontiguousarray(proj[ci][0:1024]),
                                attT=to_core(attb[b], hf), xT=XT[ci], wd=f32(hyb_w_out[i]), ngd=_colv(ssd_norm_g[i]),
                                modd=modd, onesd=ones128))
            xo = [r["xo"] for r in _run(nc_outproj, ims)]
            del proj, yfb, ybb, attb
        else:
            if nc_gmlp is None:
                nc_gmlp = build_gmlp(kinds)
            reps = np.stack([_rows(gm_ln_g[i]), _rows(gm_ln_b[i])])
            bsr = np.ascontiguousarray(np.broadcast_to(f32(gm_bs[i])[None], (128, 8, 128)))
            wsT = np.ascontiguousarray(f32(gm_ws[i]).transpose(2, 0, 1))
            ims = []
            for ci in range(NCORES):
                b = ci // 2
                modd = np.stack([np.stack([_colv(mvec(layer, r, 1)), _colv(mvec(layer, r, 0)), _colv(mvec(layer, r, 2))])
                                 for r in (b, B)])
                ims.append(dict(xT=XT[ci], modd=modd, gcol=_colv(norm1_g[layer]), wind=f32(gm_w_in[i]), woutd=f32(gm_w_out[i]),
                                wsT=wsT, reps=reps, bsr=bsr, onesd=ones128))
            xo = [r["xo"] for r in _run(nc_gmlp, ims)]
        del XT
        final = layer == depth - 1
        nc_peer = build_peer(TPC // 128, kinds, final=final)
        k1T = np.ascontiguousarray(f32(peer_k1[layer]).transpose(2, 0, 1))
        k2T = np.ascontiguousarray(f32(peer_k2[layer]).transpose(2, 0, 1))
        iota16 = np.ascontiguousarray(np.broadcast_to(np.arange(16, dtype=np.float32)[None], (128, 16)))
        ut, vt_, wq_ = f32(peer_u[layer]), f32(peer_v[layer]), f32(peer_wq[layer])
        ims = []
        for ci in range(NCORES):
            b = ci // 2
            rep = np.stack([np.stack([_rows(norm2_g[layer]), _rows(mvec(layer, r, 4)), _rows(mvec(layer, r, 3)),
                                      _rows(mvec(layer, r, 5))]) for r in (b, B)])
            im = dict(x=np.ascontiguousarray(xo[ci].T), rep=rep, wqd=wq_, k1T=k1T, k2T=k2T, utab=ut, vtab=vt_,
                      identd=ident, iotad=iota16)
            if final:
                im["gfd"] = _rows(final_norm_g)
            ims.append(im)
        del xo
        X = [r["y"] for r in _run(nc_peer, ims)]
    out = np.empty((B, L, D), np.float32)
    for ci in range(NCORES):
        out[ci // 2, (ci % 2) * HL:(ci % 2 + 1) * HL] = X[ci][:HL]
    return out


def build_fused(HLB=8, depth=4):
    HL, HC = HLB * 512, 128
    TPC = HL + HC
    NTOK = 2 * TPC
    NKT = NTOK // 128
    ne, no = (depth + 1) // 2, depth // 2
    blocks = [(j * 512, 512, 0) for j in range(HLB)] + [(HL, HC, 1)]
    NBLK = HLB + 1
    p = Prog("fused")
    nc = p.nc
    D = p.dram
    xT0 = D("xT0", [1024, TPC])
    c2T = D("c2T", [128, 8, 2])
    ada_w = D("ada_w", [depth, 1024, 6144])
    ada_bc = D("ada_bc", [128, depth * 48])
    ada_br = D("ada_br", [depth, 128, 3072])
    n1g = D("n1g", [128, depth * 8])
    n2g_rep = D("n2g_rep", [depth, 128, 1024])
    m01d = D("m01", [128, 2])
    cstd = D("cst", [6, 128, 128])
    iotad = D("iotad", [128, 16])
    w_in = D("w_in", [ne, 1024, 3328])
    convw = D("convw", [ne, 768, 5])
    convb = D("convb", [ne, 128, 6])
    rep8 = D("rep8", [ne, 128, 5, 8])
    wqn_d = D("wqn", [ne, 384, 1024])
    wq1_d = D("wq1", [ne, 384, 256])
    wq2_d = D("wq2", [ne, 384, 256])
    wkn_d = D("wkn", [ne, 256, 1024])
    wv_d = D("wvd", [ne, 256, 1024])
    qg_d = D("qg", [ne, 128, 3])
    kvg_d = D("kvg", [ne, 128, 2])
    cosK = D("cosK", [32, NTOK])
    sinK = D("sinK", [32, NTOK])
    cosQ = D("cosQ", [32, TPC])
    sinQ = D("sinQ", [32, TPC])
    w_out = D("w_out", [ne, 2048, 1024])
    ssd_ng = D("ssd_ng", [ne, 128, 8])
    gm_win = D("gm_win", [no, 1024, 4096])
    gm_wout = D("gm_wout", [no, 2048, 1024])
    gm_wsT = D("gm_wsT", [no, 128, 8, 128])
    gm_reps = D("gm_reps", [no, 2, 128, 2048])
    gm_bsr = D("gm_bsr", [no, 128, 8, 128])
    pwq = D("pwq", [depth, 1024, 2048])
    pk1T = D("pk1T", [depth, 128, 8, 128])
    pk2T = D("pk2T", [depth, 128, 8, 128])
    putab = [D(f"putab{l_}", [16384, 1024]) for l_ in range(depth)]
    pvtab = [D(f"pvtab{l_}", [16384, 1024]) for l_ in range(depth)]
    gfd = D("gfd", [128, 1024])
    yout = D("y", [HL, 1024], kind="ExternalOutput")

    def scratch(name, shape):
        return nc.dram_tensor(name, list(shape), F32).ap()

    XS = [scratch("XSa", [1024, TPC]), scratch("XSb", [1024, TPC])]
    ZT = scratch("ZT", [1024, TPC])
    CQT = scratch("CQT", [384, TPC])
    ATT = scratch("ATT", [1024, TPC])
    BA = [scratch(f"BA{j}", [896, n]) for j, (_, n, _) in enumerate(blocks)]
    BB = [scratch(f"BB{j}", [1024, n]) for j, (_, n, _) in enumerate(blocks)]
    GA = [scratch(f"GA{j}", [2 * 896, n]) for j, (_, n, _) in enumerate(blocks)]
    GB = [scratch(f"GB{j}", [2 * 1024, n]) for j, (_, n, _) in enumerate(blocks)]
    NQ = 2 * HLB + 1
    qn = [512] * (2 * HLB) + [256]
    YB = [scratch(f"YB{q}", [512, qn[q]]) for q in range(NQ)]
    GY = [scratch(f"GY{q}", [1024, qn[q]]) for q in range(NQ)]
    UVB3 = nc.dram_tensor("UVB16", [16384, 2, 1024], BF16).ap()
    UVB = UVB3.rearrange("e w n -> e (w n)")
    rg = [[2 * b, 2 * b + 1] for b in range(NCORES // 2)]

    def allgather(src, dst, skey, dkey):
        p.cc(lambda e: e.collective_compute("AllGather", ALU.bypass, replica_groups=rg, ins=[src.opt()], outs=[dst.opt()]),
             reads=[skey], writes=[dkey])

    cs = p.sb([128, 6, 128], F32, "cs")
    p.load(cs[:], cstd.rearrange("c p n -> p c n"), "cs")
    ident, ones = cs[:, 0, :], cs[:, 5, :]
    onesT = p.sb([128, 128], F32, "ones")
    p.load(onesT[:], cstd[5], "ones")
    m01 = p.sb([128, 2], F32, "m01s")
    p.load(m01[:], m01d[:, :], "m01s")
    iota16 = p.sb([128, 16], F32, "iota16")
    p.load(iota16[:], iotad[:, :], "iota16")
    n1 = p.sb([128, depth * 8], F32, "n1")
    p.load(n1[:], n1g[:, :], "n1")
    s_sb = p.sb([128, 8, 2], F32, "s_sb")
    MC = p.sb([128, depth, 48, 2], F32, "MC")
    PS = [p.ps([128, 512], F32, f"ps{i}") for i in range(8)]

    p.push("adaph_")
    c_sb = p.sb([128, 8, 2], F32, "c_sb")
    p.load(c_sb[:], c2T[:, :, :], "c_sb")
    p.op("act", lambda e: e.activation(out=s_sb[:], in_=c_sb[:], func=AF.Silu), reads=["c_sb"], writes=["s_sb"])
    bc = p.sb([128, depth * 48], F32, "bc")
    p.load(bc[:], ada_bc[:, :], "bc")
    awt = [p.sb([128, 8, 512], F32, f"awt{i}") for i in range(2)]
    ai = 0
    for l in range(depth):
        pb = PS[l % 2]
        for cbk in range(12):
            wt = awt[ai % 2]
            ai += 1
            p.load(wt[:], ada_w[l].rearrange("(k p) n -> p k n", p=128)[:, :, cbk * 512:(cbk + 1) * 512], wt.name)
            for q in range(4):
                ck = cbk * 4 + q
                for kc in range(8):
                    p.op("pe", lambda e, pb=pb, wt=wt, q=q, ck=ck, kc=kc: e.matmul(
                        pb[:, ck * 2:ck * 2 + 2], lhsT=wt[:, kc, q * 128:(q + 1) * 128], rhs=s_sb[:, kc, :],
                        start=(kc == 0), stop=(kc == 7)), reads=[wt.name, "s_sb"], writes=[pb.name])
        p.op("dve", lambda e, pb=pb, l=l: e.tensor_tensor(
            out=MC[:, l], in0=pb[:, 0:96].rearrange("p (c s) -> p c s", s=2),
            in1=bc[:, l * 48:(l + 1) * 48].unsqueeze(2).to_broadcast([128, 48, 2]), op=ALU.add),
            reads=[pb.name, "bc"], writes=["MC"])
    p.pop()
    mcol = lambda l, m, s: MC[:, l, m * 8:(m + 1) * 8, s]

    def exch_rows(e, r, j):
        if e < 7:
            return GA[j][r * 896 + e * 128:r * 896 + (e + 1) * 128, :]
        return GB[j][r * 1024 + (e - 7) * 128:r * 1024 + (e - 6) * 128, :]

    def phase_inproj(l, i, src):
        p.push(f"e1{l}_")
        stg = [p.sb([128, 2048], F32, f"stg{k}") for k in range(2)]
        w_sb = p.sb([128, 8, 3328], BF16, "w_sb")
        load_w_bf16(p, w_sb, w_in[i], 8, 3328, stg)
        Acol = p.sb([128, 2, 8], F32, "Acol")
        for s in range(2):
            p.op("dve", lambda e, s=s: e.scalar_tensor_tensor(out=Acol[:, s, :], in0=mcol(l, 1, s), scalar=1.0,
                                                              in1=n1[:, l * 8:(l + 1) * 8], op0=ALU.add, op1=ALU.mult),
                 reads=["MC", "n1"], writes=["modc"])
        xts = [p.sb([128, 8, 512], F32, f"xt{k}") for k in range(2)]
        sq = p.sb([128, 8, 512], F32, "sq")
        rstd = p.sb([128, 512], F32, "rstd")
        hT = p.sb([128, 8, 512], BF16, "hT")
        outs = [p.sb([128, 512], F32, f"ot{k}") for k in range(4)]
        xv = src.rearrange("(k p) t -> p k t", p=128)
        oi = 0
        for bi, (t0, n, s) in enumerate(blocks):
            xt = xts[bi % 2]
            p.load(xt[:, :, 0:n], xv[:, :, t0:t0 + n], xt.name, skey="XS")
            fm_norm_mod(p, xt, n, onesT, sq, PS[0], rstd, Acol[:, s, :], mcol(l, 0, s), hT)
            for j in range(26):
                pb = PS[1 + j % 7]
                for k in range(8):
                    p.op("pe", lambda e, pb=pb, j=j, k=k: e.matmul(pb[:, 0:n], lhsT=w_sb[:, k, j * 128:(j + 1) * 128],
                                                                   rhs=hT[:, k, 0:n], start=(k == 0), stop=(k == 7)),
                         reads=["w_sb", "hT"], writes=[pb.name])
                ot = outs[oi % 4]
                oi += 1
                if oi % 2:
                    p.op("act", lambda e, pb=pb, ot=ot: e.activation(out=ot[:, 0:n], in_=pb[:, 0:n], func=AF.Copy),
                         reads=[pb.name], writes=[ot.name])
                else:
                    p.op("dve", lambda e, pb=pb, ot=ot: e.tensor_copy(out=ot[:, 0:n], in_=pb[:, 0:n]),
                         reads=[pb.name], writes=[ot.name])
                if j < 8:
                    dst, dk = ZT[j * 128:(j + 1) * 128, t0:t0 + n], "ZT"
                elif j < 11:
                    dst, dk = CQT[(j - 8) * 128:(j - 7) * 128, t0:t0 + n], "CQT"
                elif j < 18:
                    dst, dk = BA[bi][(j - 11) * 128:(j - 10) * 128, :], f"BA{bi}"
                else:
                    dst, dk = BB[bi][(j - 18) * 128:(j - 17) * 128, :], f"BB{bi}"
                p.dma(lambda e, dst=dst, ot=ot: e.dma_start(out=dst, in_=ot[:, 0:n]), reads=[ot.name], writes=[dk])
            allgather(BA[bi], GA[bi], f"BA{bi}", f"GA{bi}")
            allgather(BB[bi], GB[bi], f"BB{bi}", f"GB{bi}")
        p.pop()

    NCH = NTOK // 128
    LCH = NCH - 2

    def seq_segments(c, lo, hi):
        segs = []
        if c < 2:
            a, b = c * 128 - 2 + lo, c * 128 - 2 + hi
            t = a
            while t < b:
                r = t // 128
                e = min(b, (r + 1) * 128)
                segs.append((r, HLB, t - r * 128, e - r * 128, t - (c * 128 - 2)))
                t = e
        else:
            a, b = (c - 2) * 128 - 2 + lo, (c - 2) * 128 - 2 + hi
            t = a
            while t < b:
                r = t // HL
                j = (t - r * HL) // 512
                base = r * HL + j * 512
                e = min(b, base + 512)
                segs.append((r, j, t - base, e - base, t - ((c - 2) * 128 - 2)))
                t = e
        return segs

    def chunk_home(c):
        if c < 2:
            return (c, HLB, 0), (2 * HLB, c * 128)
        lc = c - 2
        t = lc * 128
        r = t // HL
        j = (t - r * HL) // 512
        return (r, j, t - r * HL - j * 512), (t // 512, t % 512)

    def phase_ssd(i):
        p.push(f"e2{i}_")
        mask01 = p.sb([128, 2, 128], F32, "mask01")
        p.op("dve", lambda e: e.tensor_copy(out=mask01[:], in_=cs[:, 1:3, :]), reads=["cs"], writes=["mask01"])
        cw = p.sb([128, 6, 5], F32, "cw")
        p.load(cw[:], convw[i].rearrange("(k p) j -> p k j", p=128), "cw")
        cb = p.sb([128, 6], F32, "cb")
        p.load(cb[:], convb[i], "cb")
        r8 = p.sb([128, 5, 8], F32, "r8")
        p.load(r8[:], rep8[i], "r8")
        aneg = p.sb([128, 2, 8], F32, "aneg")
        p.op("act", lambda e: e.activation(out=aneg[:], in_=r8[:, 2:4, :], func=AF.Exp), reads=["r8"], writes=["aneg"])
        p.op("dve", lambda e: e.tensor_scalar(out=aneg[:], in0=aneg[:], scalar1=-1.0, scalar2=None, op0=ALU.mult),
             reads=[], writes=["aneg"])
        wins = [[p.sb([128, 6, 132], F32, f"win{g}{k}") for k in range(2)] for g in range(2)]
        def two(shape, nm):
            return [p.sb(shape, F32, f"{nm}{q}") for q in range(2)]
        win_, dtT_, xc_, acc_ = two([128, 6, 132], "win"), two([16, 128], "dtT"), two([128, 6, 128], "xc"), two([128, 6, 128], "cacc")
        xs_tm_, btm_, sm_, da_b_ = two([128, 512], "xs_tm"), two([128, 128], "btm"), two([128, 128], "sm"), two([128, 8, 128], "da_b")
        xdw_, xd_, cbm_, arg_ = two([128, 512], "xdw"), two([128, 512], "xd"), two([128, 128], "cbm"), two([128, 8, 128], "arg")
        dec_, yt_, ytmp_ = two([128, 8, 128], "dec"), two([128, 512], "yt"), two([128, 512], "ytmp")
        yT_, yold_ = two([128, 4, 128], "yT"), two([128, 4, 128], "yold")
        H = p.sb([128, 512], F32, "H")
        P = PS
        it = 0
        for d in range(2):
            tri = cs[:, 1 + d, :]
            neg = cs[:, 3 + d, :]
            order = list(range(NCH)) if d == 0 else [1, 0] + list(range(NCH - 1, 1, -1))
            p.op("dve", lambda e: e.memset(H[:], 0.0), reads=[], writes=["H"])
            for c in order:
                par = it % 2
                KK = lambda nm: f"{nm}{par}"
                win, dtT, xc, acc = win_[par], dtT_[par], xc_[par], acc_[par]
                xs_tm, btm, sm, da_b = xs_tm_[par], btm_[par], sm_[par], da_b_[par]
                xdw, xd, cbm, arg = xdw_[par], xd_[par], cbm_[par], arg_[par]
                dec, yt, ytmp, yT, yold = dec_[par], yt_[par], ytmp_[par], yT_[par], yold_[par]
                s0, s1 = (0, 2) if c < 2 else (2, NCH)
                lo = 0 if c > s0 else 2
                hi = 132 if c < s1 - 1 else 130
                wg = [wins[g][it % 2] for g in range(2)]
                it += 1
                for g in range(2):
                    if lo or hi < 132:
                        p.op("dve", lambda e, w=wg[g]: e.memset(w[:], 0.0), reads=[], writes=[wg[g].name])
                    for (r, j, c0, c1, d0) in seq_segments(c, lo, hi):
                        if g == 0:
                            srcs = [(GA[j][r * 896:r * 896 + 768, c0:c1], 0, 6, f"GA{j}")]
                        else:
                            srcs = [(GA[j][r * 896 + 768:r * 896 + 896, c0:c1], 0, 1, f"GA{j}"),
                                    (GB[j][r * 1024:r * 1024 + 640, c0:c1], 1, 6, f"GB{j}")]
                        for (sap, k0, k1, sk) in srcs:
                            p.load(wg[g][:, k0:k1, d0:d0 + (c1 - c0)], sap.rearrange("(k p) t -> p k t", p=128), wg[g].name, skey=sk)
                p.op("dve", lambda e, w0=wg[0]: e.tensor_scalar(out=win[:], in0=w0[:], scalar1=m01[:, 0:1], scalar2=None, op0=ALU.mult),
                     reads=[wg[0].name, "m01s"], writes=[KK("win")])
                p.op("dve", lambda e, w1=wg[1]: e.scalar_tensor_tensor(out=win[:], in0=w1[:], scalar=m01[:, 1:2], in1=win[:],
                                                                      op0=ALU.mult, op1=ALU.add),
                     reads=[wg[1].name, "m01s"], writes=[KK("win")])
                (hr, hj, hc0), (yq, yc0) = chunk_home(c)
                p.load(dtT[:], GB[hj][hr * 1024 + 896 + 64:hr * 1024 + 896 + 80, hc0:hc0 + 128], KK("dtT"), skey=f"GB{hj}")
                for k in range(6):
                    for j in range(5):
                        if j == 0:
                            p.op("dve", lambda e, k=k, j=j: e.tensor_scalar(
                                out=acc[:, k, :], in0=win[:, k, j:j + 128], scalar1=cw[:, k, j:j + 1], scalar2=None, op0=ALU.mult),
                                reads=[KK("win"), "cw"], writes=[KK("cacc")])
                        else:
                            p.op("dve", lambda e, k=k, j=j: e.scalar_tensor_tensor(
                                out=acc[:, k, :], in0=win[:, k, j:j + 128], scalar=cw[:, k, j:j + 1], in1=acc[:, k, :],
                                op0=ALU.mult, op1=ALU.add), reads=[KK("win"), "cw"], writes=[KK("cacc")])
                for k in range(6):
                    p.op("act", lambda e, k=k: e.activation(out=xc[:, k, :], in_=acc[:, k, :], func=AF.Silu, bias=cb[:, k:k + 1]),
                         reads=[KK("cacc"), "cb"], writes=[KK("xc")])
                for k in range(4):
                    p.op("pe", lambda e, k=k: e.transpose(out=P[0][:, k * 128:(k + 1) * 128], in_=xc[:, k, :], identity=ident),
                         reads=[KK("xc"), "cs"], writes=["ps0"])
                p.op("act", lambda e: e.activation(out=xs_tm[:], in_=P[0][:], func=AF.Copy), reads=["ps0"], writes=[KK("xs_tm")])
                p.op("pe", lambda e: e.transpose(out=P[1][:, 0:128], in_=xc[:, 4, :], identity=ident),
                     reads=[KK("xc"), "cs"], writes=["ps1"])
                p.op("dve", lambda e: e.tensor_copy(out=btm[:], in_=P[1][:, 0:128]), reads=["ps1"], writes=[KK("btm")])
                p.op("pe", lambda e: e.transpose(out=P[1][:, 384:400], in_=dtT[:], identity=ident[0:16, 0:16]),
                     reads=[KK("dtT"), "cs"], writes=["ps1"])
                p.op("dve", lambda e: e.tensor_scalar(out=sm[:, 88:96], in0=P[1][:, 384:392], scalar1=m01[:, 0:1], scalar2=None,
                                                      op0=ALU.mult), reads=["ps1", "m01s"], writes=[KK("sm")])
                p.op("dve", lambda e: e.scalar_tensor_tensor(out=sm[:, 88:96], in0=P[1][:, 392:400], scalar=m01[:, 1:2],
                                                             in1=sm[:, 88:96], op0=ALU.mult, op1=ALU.add),
                     reads=["ps1", "m01s"], writes=[KK("sm")])
                p.op("dve", lambda e, d=d: e.tensor_tensor(out=sm[:, 16:24], in0=sm[:, 88:96], in1=r8[:, d, :], op=ALU.add),
                     reads=["r8"], writes=[KK("sm")])
                p.op("act", lambda e: e.activation(out=sm[:, 16:24], in_=sm[:, 16:24], func=AF.Exp), reads=[], writes=[KK("sm")])
                p.op("act", lambda e: e.activation(out=sm[:, 0:8], in_=sm[:, 16:24], func=AF.Ln, bias=1.0), reads=[], writes=[KK("sm")])
                p.op("dve", lambda e, d=d: e.tensor_tensor(out=sm[:, 8:16], in0=sm[:, 0:8], in1=aneg[:, d, :], op=ALU.mult),
                     reads=["aneg"], writes=[KK("sm")])
                p.op("pe", lambda e, tri=tri: e.matmul(P[1][:, 128:136], lhsT=tri, rhs=sm[:, 8:16], start=True, stop=True),
                     reads=[KK("sm"), "cs"], writes=["ps1"])
                p.op("pe", lambda e: e.matmul(P[1][:, 136:144], lhsT=ones, rhs=sm[:, 8:16], start=True, stop=True),
                     reads=[KK("sm"), "cs"], writes=["ps1"])
                p.op("dve", lambda e: e.tensor_copy(out=sm[:, 24:40], in_=P[1][:, 128:144]), reads=["ps1"], writes=[KK("sm")])
                p.op("dve", lambda e: e.tensor_tensor(out=sm[:, 40:48], in0=sm[:, 32:40], in1=sm[:, 24:32], op=ALU.subtract),
                     reads=[], writes=[KK("sm")])
                p.op("act", lambda e: e.activation(out=sm[:, 48:72], in_=sm[:, 24:48], func=AF.Exp), reads=[], writes=[KK("sm")])
                p.op("dve", lambda e: e.tensor_scalar(out=sm[:, 72:80], in0=sm[:, 24:32], scalar1=-1.0, scalar2=None, op0=ALU.mult),
                     reads=[], writes=[KK("sm")])
                p.op("dve", lambda e: e.tensor_tensor(out=sm[:, 80:88], in0=sm[:, 0:8], in1=sm[:, 64:72], op=ALU.mult),
                     reads=[], writes=[KK("sm")])
                ecum, cdec = sm[:, 48:56], sm[:, 56:64]
                xs3 = xs_tm[:].rearrange("p (h q) -> p h q", h=8)
                p.op("dve", lambda e: e.tensor_tensor(out=xdw[:].rearrange("p (h q) -> p h q", h=8), in0=xs3,
                                                      in1=sm[:, 80:88].unsqueeze(2).to_broadcast([128, 8, 64]), op=ALU.mult),
                     reads=[KK("xs_tm"), KK("sm")], writes=[KK("xdw")])
                p.op("dve", lambda e: e.tensor_tensor(out=xd[:].rearrange("p (h q) -> p h q", h=8), in0=xs3,
                                                      in1=sm[:, 0:8].unsqueeze(2).to_broadcast([128, 8, 64]), op=ALU.mult),
                     reads=[KK("xs_tm"), KK("sm")], writes=[KK("xd")])
                p.op("dve", lambda e: e.tensor_copy(out=da_b[:], in_=sm[:, 8:16].unsqueeze(2).to_broadcast([128, 8, 128])),
                     reads=[KK("sm")], writes=[KK("da_b")])
                p.op("pe", lambda e: e.matmul(P[2][:], lhsT=btm[:], rhs=xdw[:], start=True, stop=True),
                     reads=[KK("btm"), KK("xdw")], writes=["ps2"])
                p.op("pe", lambda e: e.matmul(P[3][:], lhsT=xc[:, 5, :], rhs=H[:], start=True, stop=True),
                     reads=[KK("xc"), "H"], writes=["ps3"])
                p.op("pe", lambda e: e.matmul(P[1][:, 256:384], lhsT=xc[:, 4, :], rhs=xc[:, 5, :], start=True, stop=True),
                     reads=[KK("xc")], writes=["ps1"])
                p.op("dve", lambda e, d=d: e.tensor_tensor(out=cbm[:], in0=P[1][:, 256:384], in1=mask01[:, d, :], op=ALU.mult),
                     reads=["ps1", "mask01"], writes=[KK("cbm")])
                for hb in range(2):
                    pb = P[4 + hb]
                    for hh in range(4):
                        h = hb * 4 + hh
                        p.op("pe", lambda e, pb=pb, hh=hh, h=h, tri=tri: e.matmul(
                            pb[:, hh * 128:(hh + 1) * 128], lhsT=da_b[:, h, :], rhs=tri, start=True, stop=True),
                            reads=[KK("da_b"), "cs"], writes=[pb.name])
                    p.op("dve", lambda e, pb=pb, hb=hb, neg=neg: e.tensor_tensor(
                        out=arg[:, hb * 4:(hb + 1) * 4, :], in0=pb[:].rearrange("p (h t) -> p h t", h=4),
                        in1=neg.unsqueeze(1).to_broadcast([128, 4, 128]), op=ALU.add),
                        reads=[pb.name, "cs"], writes=[KK("arg")])
                for h in range(8):
                    p.op("act", lambda e, h=h: e.activation(out=dec[:, h, :], in_=arg[:, h, :], func=AF.Exp, bias=sm[:, 72 + h:73 + h]),
                         reads=[KK("arg"), KK("sm")], writes=[KK("dec")])
                p.op("dve", lambda e: e.tensor_tensor(out=dec[:], in0=dec[:], in1=cbm[:].unsqueeze(1).to_broadcast([128, 8, 128]),
                                                      op=ALU.mult), reads=[KK("cbm")], writes=[KK("dec")])
                for h in range(8):
                    p.op("pe", lambda e, h=h: e.matmul(P[6][:, h * 64:(h + 1) * 64], lhsT=dec[:, h, :], rhs=xd[:, h * 64:(h + 1) * 64],
                                                       start=True, stop=True), reads=[KK("dec"), KK("xd")], writes=["ps6"])
                p.op("dve", lambda e: e.tensor_tensor(out=ytmp[:].rearrange("p (h q) -> p h q", h=8),
                                                      in0=P[3][:].rearrange("p (h q) -> p h q", h=8),
                                                      in1=ecum.unsqueeze(2).to_broadcast([128, 8, 64]), op=ALU.mult),
                     reads=["ps3", KK("sm")], writes=[KK("ytmp")])
                p.op("dve", lambda e: e.tensor_tensor(out=yt[:], in0=ytmp[:], in1=P[6][:], op=ALU.add),
                     reads=[KK("ytmp"), "ps6"], writes=[KK("yt")])
                if d == 0:
                    p.op("dve", lambda e: e.tensor_tensor(out=ytmp[:].rearrange("p (h q) -> p h q", h=8), in0=xs3,
                                                          in1=r8[:, 4, :].unsqueeze(2).to_broadcast([128, 8, 64]), op=ALU.mult),
                         reads=[KK("xs_tm"), "r8"], writes=[KK("ytmp")])
                    p.op("dve", lambda e: e.tensor_tensor(out=yt[:], in0=yt[:], in1=ytmp[:], op=ALU.add),
                         reads=[KK("ytmp")], writes=[KK("yt")])
                for k in range(4):
                    p.op("pe", lambda e, k=k: e.transpose(out=P[7][:, k * 128:(k + 1) * 128], in_=yt[:, k * 128:(k + 1) * 128],
                                                          identity=ident), reads=[KK("yt"), "cs"], writes=["ps7"])
                ydst = YB[yq].rearrange("(k p) t -> p k t", p=128)[:, :, yc0:yc0 + 128]
                if d == 0:
                    p.op("act", lambda e: e.activation(out=yT[:], in_=P[7][:].rearrange("p (k t) -> p k t", k=4), func=AF.Copy),
                         reads=["ps7"], writes=[KK("yT")])
                else:
                    p.load(yold[:], ydst, KK("yold"), skey=f"YB{yq}")
                    p.op("dve", lambda e: e.tensor_tensor(out=yT[:], in0=P[7][:].rearrange("p (k t) -> p k t", k=4), in1=yold[:],
                                                          op=ALU.add), reads=["ps7", KK("yold")], writes=[KK("yT")])
                p.dma(lambda e, ydst=ydst: e.dma_start(out=ydst, in_=yT[:]), reads=[KK("yT")], writes=[f"YB{yq}"])
                p.op("dve", lambda e: e.tensor_tensor(out=H[:].rearrange("p (h q) -> p h q", h=8),
                                                      in0=H[:].rearrange("p (h q) -> p h q", h=8),
                                                      in1=cdec.unsqueeze(2).to_broadcast([128, 8, 64]), op=ALU.mult),
                     reads=[KK("sm")], writes=["H"])
                p.op("dve", lambda e: e.tensor_tensor(out=H[:], in0=H[:], in1=P[2][:], op=ALU.add), reads=["ps2"], writes=["H"])
        for q in range(NQ):
            allgather(YB[q], GY[q], f"YB{q}", f"GY{q}")
        p.pop()
    return _fused_rest(locals())


class _NS:
    def __init__(self, d):
        self.__dict__.update(d)


def _fused_rest(L):
    v = _NS(L)
    p, nc, PS, cs, ident, ones, onesT, m01, MC, mcol, n1 = v.p, v.nc, v.PS, v.cs, v.ident, v.ones, v.onesT, v.m01, v.MC, v.mcol, v.n1
    HLB, HL, HC, TPC, NTOK, NKT, blocks, depth = v.HLB, v.HL, v.HC, v.TPC, v.NTOK, v.NKT, v.blocks, v.depth
    GA, GB, GY, ZT, CQT, ATT, XS = v.GA, v.GB, v.GY, v.ZT, v.CQT, v.ATT, v.XS
    scale = (128 + 64) ** -0.5

    def phase_attn(i):
        NH = 8
        p.push(f"e3{i}_")
        onesb = p.sb([128, 128], BF16, "onesb")
        p.op("dve", lambda e: e.tensor_copy(out=onesb[:], in_=onesT[:]), reads=["ones"], writes=["onesb"])
        qg = p.sb([128, 3], F32, "qgs")
        p.load(qg[:], v.qg_d[i], "qgs")
        kvg = p.sb([128, 2], F32, "kvgs")
        p.load(kvg[:], v.kvg_d[i], "kvgs")
        stg = [p.sb([128, 2048], F32, f"stg{k}") for k in range(2)]
        wqn = p.sb([128, 3, NH * 128], BF16, "wqn_s")
        wq1 = p.sb([128, 3, NH * 32], BF16, "wq1_s")
        wq2 = p.sb([128, 3, NH * 32], BF16, "wq2_s")
        wkn = p.sb([128, 2, NH * 128], BF16, "wkn_s")
        wv = p.sb([128, 2, NH * 128], BF16, "wv_s")
        load_w_bf16(p, wqn, v.wqn_d[i], 3, NH * 128, stg)
        load_w_bf16(p, wq1, v.wq1_d[i], 3, NH * 32, stg)
        load_w_bf16(p, wq2, v.wq2_d[i], 3, NH * 32, stg)
        load_w_bf16(p, wkn, v.wkn_d[i], 2, NH * 128, stg)
        load_w_bf16(p, wv, v.wv_d[i], 2, NH * 128, stg)
        ckvn = p.sb([128, 2, NTOK], BF16, "ckvn")
        KR = p.sb([128, NTOK], BF16, "KR")
        wq12 = p.sb([128, 3, NH, 64], BF16, "wq12")
        wq21 = p.sb([128, 3, NH, 64], BF16, "wq21")
        KnT = p.sb([128, NTOK], BF16, "KnT")
        Vh = p.sb([128, NKT, 128], BF16, "Vh")
        cqn = p.sb([128, 3, 512], BF16, "cqn")
        xin = p.sb([128, 3, 512], F32, "xin")
        sq = p.sb([128, 3, 512], F32, "sq")
        rstd = p.sb([128, 512], F32, "rstd")
        XA, XB, CS, SN, r1, t1, t2 = [p.sb([64, 512], F32, nm) for nm in ("XA", "XB", "CS", "SN", "r1", "t1", "t2")]
        QnT = p.sb([128, 512], BF16, "QnT")
        QR = p.sb([128, 512], BF16, "QR")
        Pts = [p.sb([128, 512], BF16, f"Pt{k}") for k in range(3)]
        osb = p.sb([128, 512], F32, "osb")
        mx = p.sb([128, 8], F32, "mx")
        P = PS
        p.op("dve", lambda e: e.memset(mx[:], 0.0), reads=[], writes=["mx"])
        p.op("dve", lambda e: e.memset(KR[:], 1.0), reads=[], writes=["KR"])
        w1v = wq1[:].rearrange("p k (h c) -> p k h c", h=NH)
        w2v = wq2[:].rearrange("p k (h c) -> p k h c", h=NH)
        p.op("dve", lambda e: e.tensor_copy(out=wq12[:, :, :, 0:32], in_=w1v), reads=["wq1_s"], writes=["wq12"])
        p.op("dve", lambda e: e.tensor_copy(out=wq12[:, :, :, 32:64], in_=w2v), reads=["wq2_s"], writes=["wq12"])
        p.op("dve", lambda e: e.tensor_copy(out=wq21[:, :, :, 0:32], in_=w2v), reads=["wq2_s"], writes=["wq21"])
        p.op("dve", lambda e: e.tensor_copy(out=wq21[:, :, :, 32:64], in_=w1v), reads=["wq1_s"], writes=["wq21"])

        def rope64(n, out_ap, pb):
            p.op("dve", lambda e: e.tensor_tensor(out=t1[:, 0:n], in0=XA[:, 0:n], in1=CS[:, 0:n], op=ALU.mult), reads=["XA", "CS"], writes=["t1"])
            p.op("dve", lambda e: e.tensor_tensor(out=t2[:, 0:n], in0=XB[:, 0:n], in1=SN[:, 0:n], op=ALU.mult), reads=["XB", "SN"], writes=["t2"])
            p.op("dve", lambda e: e.tensor_tensor(out=r1[:, 0:n], in0=t1[:, 0:n], in1=t2[:, 0:n], op=ALU.add), reads=["t1", "t2"], writes=["r1"])
            if out_ap is not None:
                p.op("act", lambda e: e.activation(out=out_ap, in_=r1[:, 0:n], func=AF.Copy), reads=["r1"], writes=[out_ap.name])
            p.op("act", lambda e: e.activation(out=t1[:, 0:n], in_=r1[:, 0:n], func=AF.Square), reads=["r1"], writes=["t1"])
            p.op("pe", lambda e: e.matmul(pb[:, 0:n], lhsT=onesT[0:64, :], rhs=t1[:, 0:n], start=False, stop=True),
                 reads=["t1", "ones"], writes=[pb.name])

        def load_tables(cosd, sind, t0, n):
            p.load(CS[0:32, 0:n], cosd[:, t0:t0 + n], "CS")
            p.load(CS[32:64, 0:n], cosd[:, t0:t0 + n], "CS")
            p.load(SN[0:32, 0:n], sind[:, t0:t0 + n], "SN")
            p.load(SN[32:64, 0:n], sind[:, t0:t0 + n], "SN")
            p.op("dve", lambda e: e.tensor_scalar(out=SN[0:32, 0:n], in0=SN[0:32, 0:n], scalar1=-1.0, scalar2=None, op0=ALU.mult),
                 reads=[], writes=["SN"])

        def running_max(pb, n, col):
            p.op("dve", lambda e: e.tensor_reduce(out=mx[:, 2:3], in_=pb[:, 0:n], axis=AX.X, op=ALU.max), reads=[pb.name], writes=["mx"])
            p.op("dve", lambda e: e.tensor_tensor(out=mx[:, col:col + 1], in0=mx[:, col:col + 1], in1=mx[:, 2:3], op=ALU.max),
                 reads=[], writes=["mx"])

        def norm_block(src_ap, skey, kc, gcol, dst_fn, inv, n):
            p.load(xin[:, 0:kc, 0:n], src_ap, "xin", skey=skey)
            fm_rstd(p, xin, (0, kc), n, onesT, sq, P[5], rstd, inv)
            p.op("dve", lambda e: e.tensor_tensor(out=sq[:, 0:kc, 0:n], in0=xin[:, 0:kc, 0:n],
                                                  in1=rstd[:, 0:n].unsqueeze(1).to_broadcast([128, kc, n]), op=ALU.mult),
                 reads=["xin", "rstd"], writes=["sq"])
            for k in range(kc):
                dst = dst_fn(k)
                p.op("act", lambda e, k=k, dst=dst: e.activation(out=dst, in_=sq[:, k, 0:n], func=AF.Identity, scale=gcol[:, k:k + 1]),
                     reads=["sq", gcol.name], writes=[dst.name])

        kblocks = [(0, HLB, 128), (1, HLB, 128)] + [(r, j, 512) for r in range(2) for j in range(HLB)]
        koff = 0
        for (r, j, n) in kblocks:
            t0 = koff
            koff += n
            norm_block(GB[j][r * 1024 + 640:r * 1024 + 896, :].rearrange("(k p) t -> p k t", p=128), f"GB{j}", 2, kvg,
                       lambda k, t0=t0, n=n: ckvn[:, k, t0:t0 + n], 1.0 / 256, n)
            load_tables(v.cosK, v.sinK, t0, n)
            krb = r * 1024 + 896
            p.load(XA[0:32, 0:n], GB[j][krb:krb + 32, :], "XA", skey=f"GB{j}")
            p.load(XA[32:64, 0:n], GB[j][krb + 32:krb + 64, :], "XA", skey=f"GB{j}")
            p.load(XB[0:32, 0:n], GB[j][krb + 32:krb + 64, :], "XB", skey=f"GB{j}")
            p.load(XB[32:64, 0:n], GB[j][krb:krb + 32, :], "XB", skey=f"GB{j}")
            p.op("act", lambda e: e.activation(out=t1[:, 0:n], in_=XA[:, 0:n], func=AF.Copy, scale=0.0), reads=["XA"], writes=["t1"])
            p.op("pe", lambda e: e.matmul(P[6][:, 0:n], lhsT=onesT[0:64, :], rhs=t1[:, 0:n], start=True, stop=False),
                 reads=["t1", "ones"], writes=["ps6"])
            rope64(n, KR[0:64, t0:t0 + n], P[6])
            running_max(P[6], n, 0)
        kb512 = [(t0, min(512, NTOK - t0)) for t0 in range(0, NTOK, 512)]
        qblocks = [(t0, n, NKT if s == 0 else 2) for (t0, n, s) in blocks]
        pi = 0
        cqv = CQT.rearrange("(k p) t -> p k t", p=128)
        for h in range(NH):
            p.op("dve", lambda e: e.memset(mx[:, 1:2], 0.0), reads=[], writes=["mx"])
            for (t0, n) in kb512:
                pb = P[5]
                for k in range(2):
                    p.op("pe", lambda e, k=k: e.matmul(pb[:, 0:n], lhsT=wkn[:, k, h * 128:(h + 1) * 128], rhs=ckvn[:, k, t0:t0 + n],
                                                       start=(k == 0), stop=(k == 1)), reads=["wkn_s", "ckvn"], writes=[pb.name])
                p.op("act", lambda e: e.activation(out=KnT[:, t0:t0 + n], in_=pb[:, 0:n], func=AF.Copy), reads=[pb.name], writes=["KnT"])
                p.op("act", lambda e: e.activation(out=sq[:, 0, 0:n], in_=pb[:, 0:n], func=AF.Square), reads=[pb.name], writes=["sq"])
                p.op("pe", lambda e: e.matmul(P[6][:, 0:n], lhsT=onesT[:], rhs=sq[:, 0, 0:n], start=True, stop=True),
                     reads=["sq", "ones"], writes=["ps6"])
                running_max(P[6], n, 1)
                pv = P[7]
                nt = n // 128
                for j in range(nt):
                    for k in range(2):
                        p.op("pe", lambda e, j=j, k=k: e.matmul(
                            pv[:, j * 128:(j + 1) * 128], lhsT=ckvn[:, k, t0 + j * 128:t0 + (j + 1) * 128],
                            rhs=wv[:, k, h * 128:(h + 1) * 128], start=(k == 0), stop=(k == 1)),
                            reads=["wv_s", "ckvn"], writes=["ps7"])
                p.op("dve", lambda e, nt=nt: e.tensor_copy(out=Vh[:, t0 // 128:t0 // 128 + nt, :],
                                                          in_=pv[:, 0:nt * 128].rearrange("p (j d) -> p j d", j=nt)),
                     reads=["ps7"], writes=["Vh"])
            p.op("dve", lambda e: e.tensor_tensor(out=mx[:, 4:5], in0=mx[:, 0:1], in1=mx[:, 1:2], op=ALU.add), reads=[], writes=["mx"])
            p.op("act", lambda e: e.activation(out=mx[:, 4:5], in_=mx[:, 4:5], func=AF.Sqrt), reads=[], writes=["mx"])
            p.op("dve", lambda e: e.tensor_scalar(out=mx[:, 3:4], in0=mx[:, 4:5], scalar1=-1.0, scalar2=None, op0=ALU.mult),
                 reads=[], writes=["mx"])
            for (q0, n, nkt) in qblocks:
                norm_block(cqv[:, :, q0:q0 + n], "CQT", 3, qg, lambda k, n=n: cqn[:, k, 0:n], 1.0 / 384, n)
                pb = P[5]
                for k in range(3):
                    p.op("pe", lambda e, k=k: e.matmul(pb[:, 0:n], lhsT=wqn[:, k, h * 128:(h + 1) * 128], rhs=cqn[:, k, 0:n],
                                                       start=(k == 0), stop=(k == 2)), reads=["wqn_s", "cqn"], writes=[pb.name])
                p.op("act", lambda e: e.activation(out=QnT[:, 0:n], in_=pb[:, 0:n], func=AF.Copy), reads=[pb.name], writes=["QnT"])
                p.op("act", lambda e: e.activation(out=sq[:, 0, 0:n], in_=pb[:, 0:n], func=AF.Square), reads=[pb.name], writes=["sq"])
                for (wsb, pq, dst) in ((wq12, P[6], XA), (wq21, P[7], XB)):
                    for k in range(3):
                        p.op("pe", lambda e, k=k, wsb=wsb, pq=pq: e.matmul(
                            pq[0:64, 0:n], lhsT=wsb[:, k, h, :], rhs=cqn[:, k, 0:n],
                            start=(k == 0), stop=(k == 2)), reads=[wsb.name, "cqn"], writes=[pq.name])
                    p.op("dve", lambda e, pq=pq, dst=dst: e.tensor_copy(out=dst[:, 0:n], in_=pq[0:64, 0:n]), reads=[pq.name], writes=[dst.name])
                load_tables(v.cosQ, v.sinQ, q0, n)
                p.op("pe", lambda e: e.matmul(P[5][:, 0:n], lhsT=onesT[:], rhs=sq[:, 0, 0:n], start=True, stop=False),
                     reads=["sq", "ones"], writes=["ps5"])
                rope64(n, None, P[5])
                p.op("act", lambda e: e.activation(out=rstd[:, 0:n], in_=P[5][:, 0:n], func=AF.Sqrt), reads=["ps5"], writes=["rstd"])
                p.op("dve", lambda e: e.tensor_scalar(out=QR[:, 0:n], in0=rstd[:, 0:n], scalar1=mx[:, 3:4], scalar2=None,
                                                      op0=ALU.mult), reads=["rstd", "mx"], writes=["QR"])
                p.op("act", lambda e: e.activation(out=QR[0:64, 0:n], in_=r1[:, 0:n], func=AF.Copy), reads=["r1"], writes=["QR"])
                po, pd = P[3], P[4]
                slots = {}

                def qk(kt):
                    nonlocal pi
                    ps, pt = P[pi % 3], Pts[pi % 3]
                    pi += 1
                    slots[kt] = (ps, pt)
                    ks = slice(kt * 128, (kt + 1) * 128)
                    p.op("pe", lambda e: e.matmul(ps[:, 0:n], lhsT=KnT[:, ks], rhs=QnT[:, 0:n], start=True, stop=False),
                         reads=["KnT", "QnT"], writes=[ps.name])
                    p.op("pe", lambda e: e.matmul(ps[:, 0:n], lhsT=KR[0:65, ks], rhs=QR[0:65, 0:n], start=False, stop=True),
                         reads=["KR", "QR"], writes=[ps.name])

                qk(0)
                for kt in range(nkt):
                    ps, pt = slots.pop(kt)
                    p.op("act", lambda e, ps=ps, pt=pt: e.activation(out=pt[:, 0:n], in_=ps[:, 0:n], func=AF.Exp, scale=scale),
                         reads=[ps.name], writes=[pt.name])
                    if kt + 1 < nkt:
                        qk(kt + 1)
                    p.op("pe", lambda e, pt=pt, kt=kt: e.matmul(po[:, 0:n], lhsT=Vh[:, kt, :], rhs=pt[:, 0:n],
                                                                start=(kt == 0), stop=(kt == nkt - 1)),
                         reads=["Vh", pt.name], writes=[po.name])
                    p.op("pe", lambda e, pt=pt, kt=kt: e.matmul(pd[:, 0:n], lhsT=onesb[:], rhs=pt[:, 0:n],
                                                                start=(kt == 0), stop=(kt == nkt - 1)),
                         reads=["onesb", pt.name], writes=[pd.name])
                p.op("dve", lambda e: e.reciprocal(out=rstd[:, 0:n], in_=pd[:, 0:n]), reads=[pd.name], writes=["rstd"])
                p.op("dve", lambda e: e.tensor_tensor(out=osb[:, 0:n], in0=po[:, 0:n], in1=rstd[:, 0:n], op=ALU.mult),
                     reads=[po.name, "rstd"], writes=["osb"])
                p.dma(lambda e, q0=q0, n=n: e.dma_start(out=ATT[h * 128:(h + 1) * 128, q0:q0 + n], in_=osb[:, 0:n]),
                      reads=["osb"], writes=["ATT"])
        p.pop()

    def phase_outproj(l, i, src, dst):
        p.push(f"e4{l}_")
        ng = p.sb([128, 8], F32, "ng")
        p.load(ng[:], v.ssd_ng[i], "ng")
        stg = [p.sb([128, 2048], F32, f"stg{k}") for k in range(2)]
        w_sb = p.sb([128, 16, 1024], BF16, "w_sb")
        load_w_bf16(p, w_sb, v.w_out[i], 16, 1024, stg)
        yf = p.sb([128, 8, 512], F32, "yf")
        yb = p.sb([128, 8, 512], F32, "yb")
        zt = p.sb([128, 8, 512], F32, "zt")
        at = p.sb([128, 8, 512], F32, "at")
        xt = p.sb([128, 8, 512], F32, "xt")
        sq = p.sb([128, 8, 512], F32, "sq")
        rs = [p.sb([128, 512], F32, f"rs{k}") for k in range(2)]
        cat = p.sb([128, 16, 512], BF16, "cat")
        vv = lambda a: a.rearrange("(k p) t -> p k t", p=128)
        for bi, (t0, n, s) in enumerate(blocks):
            if s == 0:
                ya, yk0 = vv(GY[bi])[:, :, 0:n], f"GY{bi}"
                ybb, yk1 = vv(GY[HLB + bi])[:, :, 0:n], f"GY{HLB + bi}"
            else:
                ya, yk0 = vv(GY[2 * HLB])[:, :, 0:128], f"GY{2 * HLB}"
                ybb, yk1 = vv(GY[2 * HLB])[:, :, 128:256], f"GY{2 * HLB}"
            p.load(yf[:, :, 0:n], ya, "yf", skey=yk0)
            p.load(yb[:, :, 0:n], ybb, "yb", skey=yk1)
            p.load(zt[:, :, 0:n], vv(ZT)[:, :, t0:t0 + n], "zt", skey="ZT")
            p.load(at[:, :, 0:n], vv(ATT)[:, :, t0:t0 + n], "at", skey="ATT")
            p.load(xt[:, :, 0:n], vv(src)[:, :, t0:t0 + n], "xt", skey="XS")
            p.op("dve", lambda e: e.tensor_scalar(out=yf[:, :, 0:n], in0=yf[:, :, 0:n], scalar1=m01[:, 0:1], scalar2=None, op0=ALU.mult),
                 reads=["m01s"], writes=["yf"])
            p.op("dve", lambda e: e.scalar_tensor_tensor(out=yf[:, :, 0:n], in0=yb[:, :, 0:n], scalar=m01[:, 1:2], in1=yf[:, :, 0:n],
                                                         op0=ALU.mult, op1=ALU.add), reads=["yb", "m01s"], writes=["yf"])
            p.op("act", lambda e: e.activation(out=zt[:, :, 0:n], in_=zt[:, :, 0:n], func=AF.Silu), reads=[], writes=["zt"])
            p.op("dve", lambda e: e.tensor_tensor(out=yf[:, :, 0:n], in0=yf[:, :, 0:n], in1=zt[:, :, 0:n], op=ALU.mult),
                 reads=["zt"], writes=["yf"])
            for g in range(2):
                fm_rstd(p, yf, (g * 4, g * 4 + 4), n, onesT, sq, PS[g], rs[g], 1.0 / 512)
            for g in range(2):
                p.op("dve", lambda e, g=g: e.tensor_tensor(out=sq[:, g * 4:g * 4 + 4, 0:n], in0=yf[:, g * 4:g * 4 + 4, 0:n],
                                                           in1=rs[g][:, 0:n].unsqueeze(1).to_broadcast([128, 4, n]), op=ALU.mult),
                     reads=["yf", rs[g].name], writes=["sq"])
            for k in range(8):
                p.op("act", lambda e, k=k: e.activation(out=cat[:, k, 0:n], in_=sq[:, k, 0:n], func=AF.Identity, scale=ng[:, k:k + 1]),
                     reads=["sq", "ng"], writes=["cat"])
            p.op("dve", lambda e: e.tensor_copy(out=cat[:, 8:16, 0:n], in_=at[:, :, 0:n]), reads=["at"], writes=["cat"])
            for j in range(8):
                pb = PS[2 + j % 6]
                for k in range(16):
                    p.op("pe", lambda e, pb=pb, j=j, k=k: e.matmul(pb[:, 0:n], lhsT=w_sb[:, k, j * 128:(j + 1) * 128],
                                                                   rhs=cat[:, k, 0:n], start=(k == 0), stop=(k == 15)),
                         reads=["w_sb", "cat"], writes=[pb.name])
                p.op("dve", lambda e, pb=pb, j=j, s=s: e.scalar_tensor_tensor(
                    out=xt[:, j, 0:n], in0=pb[:, 0:n], scalar=MC[:, l, 16 + j, s:s + 1], in1=xt[:, j, 0:n], op0=ALU.mult, op1=ALU.add),
                    reads=[pb.name, "MC"], writes=["xt"])
            p.dma(lambda e, t0=t0, n=n: e.dma_start(out=vv(dst)[:, :, t0:t0 + n], in_=xt[:, :, 0:n]), reads=["xt"], writes=["XS"])
        p.pop()

    def phase_gmlp(l, i, src, dst):
        p.push(f"o1{l}_")
        kinds = [0] * (HL // 128) + [1]
        Acol = p.sb([128, 2, 8], F32, "Acol")
        for s in range(2):
            p.op("dve", lambda e, s=s: e.scalar_tensor_tensor(out=Acol[:, s, :], in0=mcol(l, 1, s), scalar=1.0,
                                                              in1=n1[:, l * 8:(l + 1) * 8], op0=ALU.add, op1=ALU.mult),
                 reads=["MC", "n1"], writes=["modc"])
        stg = [p.sb([128, 2048], F32, f"stg{k}") for k in range(2)]
        win = p.sb([128, 8, 4096], BF16, "win")
        wout = p.sb([128, 16, 1024], BF16, "wout")
        load_w_bf16(p, win, v.gm_win[i], 8, 4096, stg)
        load_w_bf16(p, wout, v.gm_wout[i], 16, 1024, stg)
        ws_sb = p.sb([128, 8, 128], F32, "ws_sb")
        p.load(ws_sb[:], v.gm_wsT[i], "ws_sb")
        lng = p.sb([128, 2048], F32, "lng")
        lnb = p.sb([128, 2048], F32, "lnb")
        p.load(lng[:], v.gm_reps[i, 0], "lng")
        p.load(lnb[:], v.gm_reps[i, 1], "lnb")
        bs_sb = p.sb([128, 8, 128], F32, "bs_sb")
        p.load(bs_sb[:], v.gm_bsr[i], "bs_sb")
        xts = [p.sb([128, 8, 128], F32, f"xt{k}") for k in range(2)]
        sq = p.sb([128, 8, 128], F32, "sq")
        rstd = p.sb([128, 128], F32, "rstd")
        hT = p.sb([128, 8, 128], BF16, "hT")
        uT = p.sb([128, 16, 128], F32, "uT")
        vt = p.sb([128, 2048], F32, "vt")
        junk = p.sb([128, 2048], F32, "junk")
        st = p.sb([128, 16], F32, "st")
        gated = p.sb([128, 16, 128], BF16, "gated")
        tmp = p.sb([128, 512], F32, "tmp")
        pbs = PS
        xv = src.rearrange("(k p) t -> p k t", p=128)
        xov = dst.rearrange("(k p) t -> p k t", p=128)
        for ci, s in enumerate(kinds):
            t0 = ci * 128
            xt = xts[ci % 2]
            p.load(xt[:], xv[:, :, t0:t0 + 128], xt.name, skey="XS")
            fm_norm_mod(p, xt, 128, onesT, sq, pbs[0], rstd, Acol[:, s, :], mcol(l, 0, s), hT)
            for jb in range(4):
                pb = pbs[1 + jb % 3]
                for jj in range(4):
                    j = jb * 4 + jj
                    for k in range(8):
                        p.op("pe", lambda e, pb=pb, jj=jj, j=j, k=k: e.matmul(
                            pb[:, jj * 128:(jj + 1) * 128], lhsT=win[:, k, j * 128:(j + 1) * 128], rhs=hT[:, k, :],
                            start=(k == 0), stop=(k == 7)), reads=["win", "hT"], writes=[pb.name])
                p.op("act", lambda e, pb=pb, jb=jb: e.activation(out=uT[:, jb * 4:(jb + 1) * 4, :],
                                                                in_=pb[:].rearrange("p (j t) -> p j t", j=4), func=AF.Gelu),
                     reads=[pb.name], writes=["uT"])
            for cbk in range(4):
                pb = pbs[4 + cbk % 2]
                for k in range(8):
                    p.op("pe", lambda e, pb=pb, cbk=cbk, k=k: e.matmul(
                        pb[:], lhsT=hT[:, k, :], rhs=win[:, k, 2048 + cbk * 512:2048 + (cbk + 1) * 512],
                        start=(k == 0), stop=(k == 7)), reads=["win", "hT"], writes=[pb.name])
                p.op("act", lambda e, pb=pb, cbk=cbk: e.activation(out=vt[:, cbk * 512:(cbk + 1) * 512], in_=pb[:], func=AF.Gelu),
                     reads=[pb.name], writes=["vt"])
            p.op("dve", lambda e: e.tensor_reduce(out=st[:, 0:1], in_=vt[:], axis=AX.X, op=ALU.add), reads=["vt"], writes=["st"])
            p.op("act", lambda e: e.activation(out=junk[:], in_=vt[:], func=AF.Square), reads=["vt"], writes=["junk"])
            p.op("dve", lambda e: e.tensor_reduce(out=st[:, 1:2], in_=junk[:], axis=AX.X, op=ALU.add), reads=["junk"], writes=["st"])
            p.op("dve", lambda e: e.tensor_scalar(out=st[:, 2:4], in0=st[:, 0:2], scalar1=1.0 / 2048, scalar2=None, op0=ALU.mult),
                 reads=[], writes=["st"])
            p.op("dve", lambda e: e.tensor_tensor(out=st[:, 4:5], in0=st[:, 2:3], in1=st[:, 2:3], op=ALU.mult), reads=[], writes=["st"])
            p.op("dve", lambda e: e.tensor_tensor(out=st[:, 5:6], in0=st[:, 3:4], in1=st[:, 4:5], op=ALU.subtract), reads=[], writes=["st"])
            p.op("act", lambda e: e.activation(out=st[:, 6:7], in_=st[:, 5:6], func=AF.Sqrt, bias=EPS), reads=[], writes=["st"])
            p.op("dve", lambda e: e.reciprocal(out=st[:, 7:8], in_=st[:, 6:7]), reads=[], writes=["st"])
            p.op("dve", lambda e: e.tensor_scalar(out=vt[:], in0=vt[:], scalar1=st[:, 2:3], scalar2=st[:, 7:8], op0=ALU.subtract,
                                                  op1=ALU.mult), reads=["st"], writes=["vt"])
            p.op("dve", lambda e: e.tensor_tensor(out=vt[:], in0=vt[:], in1=lng[:], op=ALU.mult), reads=["lng"], writes=["vt"])
            p.op("dve", lambda e: e.tensor_tensor(out=vt[:], in0=vt[:], in1=lnb[:], op=ALU.add), reads=["lnb"], writes=["vt"])
            for jb in range(4):
                pb = pbs[6 + jb % 2]
                for jj in range(4):
                    j = jb * 4 + jj
                    p.op("pe", lambda e, pb=pb, jj=jj, j=j: e.matmul(pb[:, jj * 128:(jj + 1) * 128], lhsT=vt[:, j * 128:(j + 1) * 128],
                                                                     rhs=ws_sb[:, j // 2, :], start=True, stop=True),
                         reads=["vt", "ws_sb"], writes=[pb.name])
                p.op("dve", lambda e, pb=pb, jb=jb: e.tensor_tensor(
                    out=tmp[:].rearrange("p (g r t) -> p g r t", g=2, r=2), in0=pb[:].rearrange("p (g r t) -> p g r t", g=2, r=2),
                    in1=bs_sb[:, jb * 2:jb * 2 + 2, :].unsqueeze(2).to_broadcast([128, 2, 2, 128]), op=ALU.add),
                    reads=[pb.name, "bs_sb"], writes=["tmp"])
                p.op("dve", lambda e, jb=jb: e.tensor_tensor(out=gated[:, jb * 4:(jb + 1) * 4, :],
                                                             in0=tmp[:].rearrange("p (j t) -> p j t", j=4),
                                                             in1=uT[:, jb * 4:(jb + 1) * 4, :], op=ALU.mult),
                     reads=["tmp", "uT"], writes=["gated"])
            for jb in range(2):
                pb = pbs[1 + jb]
                for jj in range(4):
                    j = jb * 4 + jj
                    for k in range(16):
                        p.op("pe", lambda e, pb=pb, jj=jj, j=j, k=k: e.matmul(
                            pb[:, jj * 128:(jj + 1) * 128], lhsT=wout[:, k, j * 128:(j + 1) * 128], rhs=gated[:, k, :],
                            start=(k == 0), stop=(k == 15)), reads=["wout", "gated"], writes=[pb.name])
                for jj in range(4):
                    j = jb * 4 + jj
                    p.op("dve", lambda e, pb=pb, jj=jj, j=j, s=s: e.scalar_tensor_tensor(
                        out=xt[:, j, :], in0=pb[:, jj * 128:(jj + 1) * 128], scalar=MC[:, l, 16 + j, s:s + 1], in1=xt[:, j, :],
                        op0=ALU.mult, op1=ALU.add), reads=[pb.name, "MC"], writes=[xt.name])
            p.dma(lambda e, t0=t0, xt=xt: e.dma_start(out=xov[:, :, t0:t0 + 128], in_=xt[:]), reads=[xt.name], writes=["XS"])
        p.pop()

    v.phase_attn, v.phase_outproj, v.phase_gmlp = phase_attn, phase_outproj, phase_gmlp
    return _fused_peer_and_chain(v)


def _fused_peer_and_chain(v):
    p, PS, cs, ident, onesT, MC, iota16, s_sb = v.p, v.PS, v.cs, v.ident, v.onesT, v.MC, v.iota16, v.s_sb
    HL, HC, TPC, depth, XS = v.HL, v.HC, v.TPC, v.depth, v.XS

    def phase_peer(l, src, dst, final):
        p.push(f"pp{l}_")
        ctx_live = any(j % 2 == 0 for j in range(l + 1, depth))
        NT = TPC // 128 if ctx_live else HL // 128
        kinds = [0] * (HL // 128) + [1]
        utab, vtab = v.putab[l], v.pvtab[l]
        wq_sb = p.sb([128, 8, 2048], BF16, "wq")
        k1_sb = p.sb([128, 8, 128], F32, "k1")
        k2_sb = p.sb([128, 8, 128], F32, "k2")
        A_rep = [p.sb([128, 1024], F32, f"A{s}") for s in range(2)]
        B_rep = [p.sb([128, 1024], F32, f"B{s}") for s in range(2)]
        G2_rep = [p.sb([128, 1024], F32, f"G{s}") for s in range(2)]
        NG = 6
        gbuf = [p.sb([128, 2, 2048], BF16, f"gb{k}") for k in range(NG)]
        awt = p.sb([128, 8, 256], F32, "awt")
        gring = Ring(list(range(NG)))
        W = [p.sb([128, 2048], F32, f"W{k}") for k in range(4)]
        xt = p.sb([128, 1024], F32, "xt")
        tt = p.sb([128, 1024], F32, "tt")
        acc = p.sb([128, 1024], F32, "acc")
        tT = p.sb([128, 8, 128], BF16, "tT")
        xTt = p.sb([128, 8, 128], F32, "xTt")
        small = p.sb([128, 64], F32, "small")
        v16 = p.sb([128, 16, 16], F32, "v16")
        i16u = p.sb([128, 16, 16], U32, "i16u")
        i16f = p.sb([128, 16, 16], F32, "i16f")
        ts16 = p.sb([128, 8, 16], F32, "ts16")
        j16u = p.sb([128, 8, 16], U32, "j16u")
        jhu = p.sb([128, 8, 16], U32, "jhu")
        jlu = p.sb([128, 8, 16], U32, "jlu")
        jhi = p.sb([128, 8, 16], F32, "jhi")
        jlo = p.sb([128, 8, 16], F32, "jlo")
        sel1 = p.sb([128, 8, 16], F32, "sel1")
        sel2 = p.sb([128, 8, 16], F32, "sel2")
        eidf = p.sb([128, 128], F32, "eidf")
        eidu = p.sb([128, 128], I32, "eidu")
        gate = p.sb([128, 8, 16], F32, "gate")
        aval = p.sb([128, 128], F32, "aval")
        wval = p.sb([128, 128], F32, "wval")
        psum = PS
        if final:
            gf = p.sb([128, 1024], F32, "gf")
            p.load(gf[:], v.gfd[:, :], "gf")
        p.load(k1_sb[:], v.pk1T[l], "k1")
        p.load(k2_sb[:], v.pk2T[l], "k2")
        wq_v = v.pwq[l].rearrange("(k p) n -> p k n", p=128)
        wi = 0
        for k in range(8):
            wst = W[wi % 4]
            wi += 1
            p.load(wst[:], wq_v[:, k, :], wst.name)
            p.op("dve" if k % 2 else "act", (lambda e, wst=wst, k=k: e.tensor_copy(out=wq_sb[:, k, :], in_=wst[:])) if k % 2 else
                 (lambda e, wst=wst, k=k: e.activation(out=wq_sb[:, k, :], in_=wst[:], func=AF.Copy)),
                 reads=[wst.name], writes=["wq"])
        for which, tab in enumerate((v.putab[l], v.pvtab[l])):
            tv = tab.rearrange("(c p a) n -> c p (a n)", p=128, a=2)
            bv = v.UVB3[:, which, :].rearrange("(c p a) n -> c p a n", p=128, a=2)
            for cpi in range(64):
                wst = W[wi % 4]
                gi = gring.get()
                wi += 1
                cb16 = gbuf[gi][:, 0, :]
                p.load(wst[:], tv[cpi], wst.name)
                if cpi % 2:
                    p.op("dve", lambda e, wst=wst, cb16=cb16: e.tensor_copy(out=cb16, in_=wst[:]), reads=[wst.name], writes=[f"gb{gi}"])
                else:
                    p.op("act", lambda e, wst=wst, cb16=cb16: e.activation(out=cb16, in_=wst[:], func=AF.Copy),
                         reads=[wst.name], writes=[f"gb{gi}"])
                p.dma(lambda e, cpi=cpi, bv=bv, cb16=cb16: e.dma_start(out=bv[cpi], in_=cb16.rearrange("p (a n) -> p a n", a=2)),
                      reads=[f"gb{gi}"], writes=["UVB"])
        S_rep = [W[0][:, 0:1024].rearrange("p (k m) -> p k m", k=8), W[1][:, 0:1024].rearrange("p (k m) -> p k m", k=8)]
        n2 = W[2][:, 0:1024]
        for s in range(2):
            p.op("dve", lambda e, s=s: e.tensor_copy(out=S_rep[s], in_=s_sb[:, :, s:s + 1].to_broadcast([128, 8, 128])),
                 reads=["s_sb"], writes=[f"W{s}"])
        p.load(n2, v.n2g_rep[l], "W2")
        br = W[3][:, 0:256]
        for m in (3, 4, 5):
            for qq in range(4):
                c0 = m * 1024 + qq * 256
                p.load(awt[:], v.ada_w[l].rearrange("(k p) n -> p k n", p=128)[:, :, c0:c0 + 256], "awt")
                p.load(br, v.ada_br[l][:, c0 - 3072:c0 - 3072 + 256], "W3")
                for s in range(2):
                    pb = psum[s]
                    for kc in range(8):
                        p.op("pe", lambda e, pb=pb, s=s, kc=kc: e.matmul(pb[:, 0:256], lhsT=S_rep[s][:, kc, :], rhs=awt[:, kc, :],
                                                                        start=(kc == 0), stop=(kc == 7)),
                             reads=[f"W{s}", "awt"], writes=[pb.name])
                    hs = slice(qq * 256, (qq + 1) * 256)
                    dstt = {3: B_rep, 4: A_rep, 5: G2_rep}[m][s]
                    p.op("dve", lambda e, pb=pb, dstt=dstt, hs=hs: e.tensor_tensor(out=dstt[:, hs], in0=pb[:, 0:256], in1=br, op=ALU.add),
                         reads=[pb.name, "W3"], writes=[dstt.name])
                    if m == 4:
                        p.op("dve", lambda e, dstt=dstt, hs=hs: e.scalar_tensor_tensor(out=dstt[:, hs], in0=dstt[:, hs], scalar=1.0,
                                                                                      in1=n2[:, hs], op0=ALU.add, op1=ALU.mult),
                             reads=["W2"], writes=[dstt.name])
        srcv = src.rearrange("(k p) t -> p k t", p=128)
        dstv = dst.rearrange("(k p) t -> p k t", p=128) if dst is not None else None
        xt_ = [xt, p.sb([128, 1024], F32, "xt1")]
        tt_ = [tt, p.sb([128, 1024], F32, "tt1")]
        eidu_ = [eidu, p.sb([128, 128], I32, "eidu1")]
        gate_ = [gate, p.sb([128, 8, 16], F32, "gate1")]
        xTo = W[2][:, 0:1024].rearrange("p (k t) -> p k t", k=8)

        def prologue(ti):
            q_ = ti % 2
            QK = lambda nm: f"{nm}{q_}"
            xt, tt, eidu, gate = xt_[q_], tt_[q_], eidu_[q_], gate_[q_]
            s = kinds[ti]
            r0 = ti * 128
            p.load(xTt[:], srcv[:, :, r0:r0 + 128], "xTt", skey="XS")
            for half in range(2):
                pb = psum[half]
                for c in range(4):
                    k = half * 4 + c
                    p.op("pe", lambda e, pb=pb, c=c, k=k: e.transpose(out=pb[:, c * 128:(c + 1) * 128], in_=xTt[:, k, :], identity=ident),
                         reads=["xTt", "cs"], writes=[pb.name])
                p.op("act", lambda e, pb=pb, half=half: e.activation(out=xt[:, half * 512:(half + 1) * 512], in_=pb[:], func=AF.Copy),
                     reads=[pb.name], writes=[QK("xt")])
            p.op("act", lambda e: e.activation(out=tt[:], in_=xt[:], func=AF.Square, accum_out=small[:, 0:1]),
                 reads=[QK("xt")], writes=[QK("tt"), "small"])
            p.op("dve", lambda e: e.tensor_scalar(out=small[:, 1:2], in0=small[:, 0:1], scalar1=1.0 / 1024, scalar2=EPS,
                                                  op0=ALU.mult, op1=ALU.add), reads=["small"], writes=["small"])
            p.op("act", lambda e: e.activation(out=small[:, 3:4], in_=small[:, 1:2], func=AF.Sqrt), reads=["small"], writes=["small"])
            p.op("dve", lambda e: e.reciprocal(out=small[:, 2:3], in_=small[:, 3:4]), reads=["small"], writes=["small"])
            p.op("dve", lambda e, s=s: e.scalar_tensor_tensor(out=tt[:], in0=xt[:], scalar=small[:, 2:3], in1=A_rep[s][:],
                                                              op0=ALU.mult, op1=ALU.mult),
                 reads=[QK("xt"), "small", f"A{s}"], writes=[QK("tt")])
            p.op("dve", lambda e, s=s: e.tensor_tensor(out=tt[:], in0=tt[:], in1=B_rep[s][:], op=ALU.add),
                 reads=[f"B{s}"], writes=[QK("tt")])
            for half in range(2):
                pb = psum[half]
                for c in range(4):
                    k = half * 4 + c
                    p.op("pe", lambda e, pb=pb, c=c, k=k: e.transpose(out=pb[:, c * 128:(c + 1) * 128],
                                                                      in_=tt[:, k * 128:(k + 1) * 128], identity=ident),
                         reads=[QK("tt"), "cs"], writes=[pb.name])
                p.op("act", lambda e, pb=pb, half=half: e.activation(
                    out=tT[:, half * 4:(half + 1) * 4, :], in_=pb[:].rearrange("p (c t) -> p c t", c=4), func=AF.Copy),
                    reads=[pb.name], writes=["tT"])
            qT = W[0]
            for jb in range(4):
                pb = psum[2 + jb]
                for jj in range(4):
                    j = jb * 4 + jj
                    for k in range(8):
                        p.op("pe", lambda e, pb=pb, jj=jj, j=j, k=k: e.matmul(
                            pb[:, jj * 128:(jj + 1) * 128], lhsT=wq_sb[:, k, j * 128:(j + 1) * 128], rhs=tT[:, k, :],
                            start=(k == 0), stop=(k == 7)), reads=["wq", "tT"], writes=[pb.name])
                p.op("act", lambda e, pb=pb, jb=jb: e.activation(out=qT[:, jb * 512:(jb + 1) * 512], in_=pb[:], func=AF.Copy),
                     reads=[pb.name], writes=["W0"])
            S = W[1]
            for gb in range(4):
                pb = psum[(6 + gb) % 8]
                for gg in range(4):
                    g = gb * 4 + gg
                    h, half = g // 2, g % 2
                    ksb = k1_sb if half == 0 else k2_sb
                    p.op("pe", lambda e, pb=pb, gg=gg, g=g, h=h, ksb=ksb: e.matmul(
                        pb[:, gg * 128:(gg + 1) * 128], lhsT=qT[:, g * 128:(g + 1) * 128], rhs=ksb[:, h, :],
                        start=True, stop=True), reads=["W0", "k1", "k2"], writes=[pb.name])
                p.op("act", lambda e, pb=pb, gb=gb: e.activation(out=S[:, gb * 512:(gb + 1) * 512], in_=pb[:], func=AF.Copy),
                     reads=[pb.name], writes=["W1"])
            S2 = W[2]
            for g in range(16):
                sl = slice(g * 128, (g + 1) * 128)
                p.op("dve", lambda e, g=g, sl=sl: e.max(out=v16[:, g, 0:8], in_=S[:, sl]), reads=["W1"], writes=["v16"])
                p.op("dve", lambda e, g=g, sl=sl: e.max_index(out=i16u[:, g, 0:8], in_max=v16[:, g, 0:8], in_values=S[:, sl]),
                     reads=["W1", "v16"], writes=["i16u"])
                p.op("dve", lambda e, g=g, sl=sl: e.match_replace(out=S2[:, sl], in_to_replace=v16[:, g, 0:8],
                                                                  in_values=S[:, sl], imm_value=-1e30),
                     reads=["W1", "v16"], writes=["W2"])
                p.op("dve", lambda e, g=g, sl=sl: e.max(out=v16[:, g, 8:16], in_=S2[:, sl]), reads=["W2"], writes=["v16"])
                p.op("dve", lambda e, g=g, sl=sl: e.max_index(out=i16u[:, g, 8:16], in_max=v16[:, g, 8:16], in_values=S2[:, sl]),
                     reads=["W2", "v16"], writes=["i16u"])
            p.op("dve", lambda e: e.tensor_copy(out=i16f[:], in_=i16u[:]), reads=["i16u"], writes=["i16f"])
            v4 = v16[:].rearrange("p (h two) k -> p h two k", two=2)
            csum = W[3][:].rearrange("p (h i j) -> p h i j", h=8, i=16)
            p.op("dve", lambda e: e.tensor_tensor(out=csum, in0=v4[:, :, 0, :].unsqueeze(3).to_broadcast([128, 8, 16, 16]),
                                                  in1=v4[:, :, 1, :].unsqueeze(2).to_broadcast([128, 8, 16, 16]), op=ALU.add),
                 reads=["v16"], writes=["W3"])
            cs2 = W[0]
            for h in range(8):
                sl = slice(h * 256, (h + 1) * 256)
                p.op("dve", lambda e, h=h, sl=sl: e.max(out=ts16[:, h, 0:8], in_=W[3][:, sl]), reads=["W3"], writes=["ts16"])
                p.op("dve", lambda e, h=h, sl=sl: e.max_index(out=j16u[:, h, 0:8], in_max=ts16[:, h, 0:8], in_values=W[3][:, sl]),
                     reads=["W3", "ts16"], writes=["j16u"])
                p.op("dve", lambda e, h=h, sl=sl: e.match_replace(out=cs2[:, sl], in_to_replace=ts16[:, h, 0:8],
                                                                  in_values=W[3][:, sl], imm_value=-1e30),
                     reads=["W3", "ts16"], writes=["W0"])
                p.op("dve", lambda e, h=h, sl=sl: e.max(out=ts16[:, h, 8:16], in_=cs2[:, sl]), reads=["W0"], writes=["ts16"])
                p.op("dve", lambda e, h=h, sl=sl: e.max_index(out=j16u[:, h, 8:16], in_max=ts16[:, h, 8:16], in_values=cs2[:, sl]),
                     reads=["W0", "ts16"], writes=["j16u"])
            p.op("dve", lambda e: e.tensor_scalar(out=jhu[:], in0=j16u[:], scalar1=4, scalar2=None, op0=ALU.logical_shift_right),
                 reads=["j16u"], writes=["jhu"])
            p.op("dve", lambda e: e.tensor_scalar(out=jlu[:], in0=j16u[:], scalar1=15, scalar2=None, op0=ALU.bitwise_and),
                 reads=["j16u"], writes=["jlu"])
            p.op("dve", lambda e: e.tensor_copy(out=jhi[:], in_=jhu[:]), reads=["jhu"], writes=["jhi"])
            p.op("dve", lambda e: e.tensor_copy(out=jlo[:], in_=jlu[:]), reads=["jlu"], writes=["jlo"])
            i4 = i16f[:].rearrange("p (h two) k -> p h two k", two=2)
            eq = W[1][:].rearrange("p (h r k) -> p h r k", h=8, r=16)
            for which, (jsel, dsel) in enumerate(((jhi, sel1), (jlo, sel2))):
                for h in range(8):
                    p.op("dve", lambda e, h=h, jsel=jsel: e.tensor_tensor(
                        out=eq[:, h], in0=jsel[:, h, :].unsqueeze(2).to_broadcast([128, 16, 16]),
                        in1=iota16[:].unsqueeze(1).to_broadcast([128, 16, 16]), op=ALU.is_equal),
                        reads=[jsel.name, "iota16"], writes=["W1"])
                    p.op("dve", lambda e, h=h, which=which: e.tensor_tensor(
                        out=eq[:, h], in0=eq[:, h], in1=i4[:, h, which, :].unsqueeze(1).to_broadcast([128, 16, 16]),
                        op=ALU.mult), reads=["i16f"], writes=["W1"])
                p.op("dve", lambda e, dsel=dsel: e.tensor_reduce(out=dsel[:], in_=eq, axis=AX.X, op=ALU.add),
                     reads=["W1"], writes=[dsel.name])
            p.op("dve", lambda e: e.scalar_tensor_tensor(out=eidf[:], in0=sel1[:].rearrange("p h r -> p (h r)"), scalar=128.0,
                                                         in1=sel2[:].rearrange("p h r -> p (h r)"), op0=ALU.mult, op1=ALU.add),
                 reads=["sel1", "sel2"], writes=["eidf"])
            p.op("dve", lambda e: e.tensor_copy(out=eidu[:], in_=eidf[:]), reads=["eidf"], writes=[QK("eidu")])
            p.op("dve", lambda e: e.tensor_tensor(out=gate[:], in0=ts16[:], in1=ts16[:, :, 0:1].to_broadcast([128, 8, 16]),
                                                  op=ALU.subtract), reads=["ts16"], writes=[QK("gate")])
            p.op("act", lambda e: e.activation(out=gate[:], in_=gate[:], func=AF.Exp), reads=[], writes=[QK("gate")])
            p.op("dve", lambda e: e.tensor_reduce(out=small[:, 8:16], in_=gate[:], axis=AX.X, op=ALU.add),
                 reads=[QK("gate")], writes=["small"])
            p.op("dve", lambda e: e.reciprocal(out=small[:, 16:24], in_=small[:, 8:16]), reads=[], writes=["small"])
            p.op("dve", lambda e: e.tensor_tensor(out=gate[:], in0=gate[:],
                                                  in1=small[:, 16:24].unsqueeze(2).to_broadcast([128, 8, 16]), op=ALU.mult),
                 reads=["small"], writes=[QK("gate")])

        def main_part(ti, c_lo, c_hi, state):
            q_ = ti % 2
            QK = lambda nm: f"{nm}{q_}"
            xt, tt, eidu, gate = xt_[q_], tt_[q_], eidu_[q_], gate_[q_]
            s = kinds[ti]
            r0 = ti * 128
            gflat = gate[:].rearrange("p h r -> p (h r)")
            NCK = 64

            def tail_ops(c, gi):
                cs_ = slice(2 * c, 2 * c + 2)
                p.op("dve", lambda e: e.tensor_tensor(out=wval[:, cs_], in0=aval[:, cs_], in1=gflat[:, cs_], op=ALU.mult),
                     reads=[("gl", c % 4), QK("gate")], writes=[("wv", c % 4)])
                for ee in range(2):
                    col = 2 * c + ee
                    if col == 0:
                        p.op("dve", lambda e, ee=ee, col=col: e.tensor_scalar(
                            out=acc[:], in0=gbuf[gi][:, ee, 1024:2048], scalar1=wval[:, col:col + 1], scalar2=None, op0=ALU.mult),
                            reads=[f"gb{gi}", ("wv", c % 4)], writes=["acc"])
                    else:
                        p.op("dve", lambda e, ee=ee, col=col: e.scalar_tensor_tensor(
                            out=acc[:], in0=gbuf[gi][:, ee, 1024:2048], scalar=wval[:, col:col + 1], in1=acc[:],
                            op0=ALU.mult, op1=ALU.add), reads=[f"gb{gi}", ("wv", c % 4)], writes=["acc"])

            for c in range(c_lo, c_hi):
                gi = gring.get()
                cs_ = slice(2 * c, 2 * c + 2)
                for ee in range(2):
                    col = 2 * c + ee
                    p.dma(lambda e, gi=gi, ee=ee, col=col: e.indirect_dma_start(
                        out=gbuf[gi][:, ee, :], out_offset=None, in_=v.UVB,
                        in_offset=bass.IndirectOffsetOnAxis(ap=eidu[:, col:col + 1], axis=0)),
                        reads=[QK("eidu"), "UVB"], writes=[f"gb{gi}"], q="pool")
                p.op("dve", lambda e, gi=gi: e.tensor_tensor(out=gbuf[gi][:, :, 0:1024], in0=gbuf[gi][:, :, 0:1024],
                                                             in1=tt[:].unsqueeze(1).to_broadcast([128, 2, 1024]), op=ALU.mult),
                     reads=[QK("tt")], writes=[f"gb{gi}"])
                p.op("dve", lambda e, gi=gi, cs_=cs_: e.tensor_reduce(out=aval[:, cs_], in_=gbuf[gi][:, :, 0:1024], axis=AX.X, op=ALU.add),
                     reads=[f"gb{gi}"], writes=[("av", c % 4)])
                p.op("act", lambda e, cs_=cs_: e.activation(out=aval[:, cs_], in_=aval[:, cs_], func=AF.Gelu),
                     reads=[("av", c % 4)], writes=[("gl", c % 4)])
                if state[0] is not None:
                    tail_ops(*state[0])
                state[0] = (c, gi)
            if c_hi == NCK:
                tail_ops(*state[0])

        def epilogue(ti):
            q_ = ti % 2
            QK = lambda nm: f"{nm}{q_}"
            xt, tt, eidu, gate = xt_[q_], tt_[q_], eidu_[q_], gate_[q_]
            s = kinds[ti]
            r0 = ti * 128
            p.op("dve", lambda e, s=s: e.tensor_tensor(out=acc[:], in0=acc[:], in1=G2_rep[s][:], op=ALU.mult),
                 reads=[f"G{s}"], writes=["acc"])
            p.op("dve", lambda e: e.tensor_tensor(out=acc[:], in0=acc[:], in1=xt[:], op=ALU.add), reads=[QK("xt")], writes=["acc"])
            if final:
                p.op("act", lambda e: e.activation(out=tt[:], in_=acc[:], func=AF.Square, accum_out=small[:, 32:33]),
                     reads=["acc"], writes=[QK("tt"), "small"])
                p.op("dve", lambda e: e.tensor_scalar(out=small[:, 33:34], in0=small[:, 32:33], scalar1=1.0 / 1024, scalar2=EPS,
                                                      op0=ALU.mult, op1=ALU.add), reads=[], writes=["small"])
                p.op("act", lambda e: e.activation(out=small[:, 34:35], in_=small[:, 33:34], func=AF.Sqrt), reads=[], writes=["small"])
                p.op("dve", lambda e: e.reciprocal(out=small[:, 35:36], in_=small[:, 34:35]), reads=[], writes=["small"])
                p.op("dve", lambda e: e.scalar_tensor_tensor(out=acc[:], in0=acc[:], scalar=small[:, 35:36], in1=gf[:],
                                                             op0=ALU.mult, op1=ALU.mult), reads=["small", "gf"], writes=["acc"])
                p.store(v.yout[r0:r0 + 128, :], acc[:], "acc")
            else:
                for half in range(2):
                    pb = psum[half]
                    for c in range(4):
                        k = half * 4 + c
                        p.op("pe", lambda e, pb=pb, c=c, k=k: e.transpose(out=pb[:, c * 128:(c + 1) * 128],
                                                                          in_=acc[:, k * 128:(k + 1) * 128], identity=ident),
                             reads=["acc", "cs"], writes=[pb.name])
                    p.op("act", lambda e, pb=pb, half=half: e.activation(
                        out=xTo[:, half * 4:(half + 1) * 4, :], in_=pb[:].rearrange("p (c t) -> p c t", c=4), func=AF.Copy),
                        reads=[pb.name], writes=["W2"])
                p.dma(lambda e, r0=r0: e.dma_start(out=dstv[:, :, r0:r0 + 128], in_=xTo), reads=["W2"], writes=["XS"])

        prologue(0)
        for ti in range(NT):
            state = [None]
            main_part(ti, 0, 32, state)
            if ti + 1 < NT:
                prologue(ti + 1)
            main_part(ti, 32, 64, state)
            epilogue(ti)
        p.pop()

    cur = v.xT0
    for l in range(depth):
        i = l // 2
        if l % 2 == 0:
            v.phase_inproj(l, i, cur)
            v.phase_ssd(i)
            v.phase_attn(i)
            v.phase_outproj(l, i, cur, XS[0])
        else:
            v.phase_gmlp(l, i, cur, XS[0])
        final = l == depth - 1
        phase_peer(l, XS[0], None if final else XS[1], final)
        cur = XS[1]
    return p.finish()


def kernel(x, c, ctx, c_ctx, ada_w, ada_b, norm1_g, norm2_g, hyb_w_in, ssd_conv_w, ssd_conv_b,
           ssd_a_log, ssd_dt_bias, ssd_d, ssd_norm_g, mla_q_norm_g, mla_w_qb, mla_kv_norm_g, mla_w_kvb,
           hyb_w_out, gm_w_in, gm_ln_g, gm_ln_b, gm_ws, gm_bs, gm_w_out, peer_wq, peer_k1, peer_k2,
           peer_u, peer_v, final_norm_g):
    f32 = lambda a: np.ascontiguousarray(np.asarray(a, dtype=np.float32))
    x, c, ctx, c_ctx = f32(x), f32(c), f32(ctx), f32(c_ctx)
    B, L, D = x.shape
    LC = ctx.shape[1]
    depth = ada_w.shape[0]
    ne, no = (depth + 1) // 2, depth // 2
    HL, HC = L // 2, LC // 2
    HLB = HL // 512
    TPC = HL + HC
    ada_w, ada_b = f32(ada_w), f32(ada_b)
    shared = {}
    shared["ada_w"] = ada_w
    shared["ada_bc"] = np.ascontiguousarray(ada_b.reshape(depth, 48, 128).transpose(2, 0, 1).reshape(128, depth * 48))
    shared["ada_br"] = np.ascontiguousarray(np.broadcast_to(ada_b[:, None, 3072:6144], (depth, 128, 3072)))
    shared["n1g"] = np.ascontiguousarray(f32(norm1_g).reshape(depth, 8, 128).transpose(2, 0, 1).reshape(128, depth * 8))
    shared["n2g_rep"] = np.ascontiguousarray(np.broadcast_to(f32(norm2_g)[:, None, :], (depth, 128, D)))
    shared["cst"] = _ssd_consts()
    shared["iotad"] = np.ascontiguousarray(np.broadcast_to(np.arange(16, dtype=np.float32)[None], (128, 16)))
    perm = np.concatenate([np.arange(0, 1024), np.arange(2576, 2960),
                           np.arange(1024, 1536), np.arange(2048, 2176), np.arange(2304, 2432),
                           np.arange(1536, 2048), np.arange(2176, 2304), np.arange(2432, 2560),
                           np.arange(2960, 3216), np.arange(3216, 3280), np.arange(2560, 2576)])
    w_in = np.zeros((ne, D, 3328), np.float32)
    w_in[:, :, :perm.size] = f32(hyb_w_in)[:, :, perm]
    shared["w_in"] = w_in
    wqb_, wkvb_ = f32(mla_w_qb), f32(mla_w_kvb)
    cat = lambda w, lo, hi, st: np.ascontiguousarray(np.concatenate([w[:, :, h * st + lo:h * st + hi] for h in range(8)], 2))
    shared["wqn"], shared["wq1"], shared["wq2"] = cat(wqb_, 0, 128, 192), cat(wqb_, 128, 160, 192), cat(wqb_, 160, 192, 192)
    shared["wkn"], shared["wvd"] = cat(wkvb_, 0, 128, 256), cat(wkvb_, 128, 256, 256)
    shared["qg"] = np.stack([_colv(mla_q_norm_g[i], 3) for i in range(ne)])
    shared["kvg"] = np.stack([_colv(mla_kv_norm_g[i], 2) for i in range(ne)])
    cosT, sinT = _rope_tables(L, LC)
    shared["cosK"], shared["sinK"] = cosT, sinT
    shared["w_out"] = f32(hyb_w_out)
    shared["ssd_ng"] = np.stack([_colv(ssd_norm_g[i]) for i in range(ne)])
    shared["gm_win"], shared["gm_wout"] = f32(gm_w_in), f32(gm_w_out)
    shared["gm_wsT"] = np.ascontiguousarray(f32(gm_ws).transpose(0, 3, 1, 2))
    shared["gm_reps"] = np.ascontiguousarray(np.stack([np.broadcast_to(f32(gm_ln_g)[:, None, :], (no, 128, 2048)),
                                                       np.broadcast_to(f32(gm_ln_b)[:, None, :], (no, 128, 2048))], 1))
    shared["gm_bsr"] = np.ascontiguousarray(np.broadcast_to(f32(gm_bs)[:, None], (no, 128, 8, 128)))
    shared["pwq"] = f32(peer_wq)
    shared["pk1T"] = np.ascontiguousarray(f32(peer_k1).transpose(0, 3, 1, 2))
    shared["pk2T"] = np.ascontiguousarray(f32(peer_k2).transpose(0, 3, 1, 2))
    for l_ in range(depth):
        shared[f"putab{l_}"] = f32(peer_u[l_])
        shared[f"pvtab{l_}"] = f32(peer_v[l_])
    shared["gfd"] = _rows(f32(final_norm_g))
    cw, cb = f32(ssd_conv_w), f32(ssd_conv_b)
    ims = []
    for ci in range(NCORES):
        b, r = ci // 2, ci % 2
        im = dict(shared)
        im["xT0"] = np.ascontiguousarray(np.concatenate([x[b, r * HL:(r + 1) * HL], ctx[b, r * HC:(r + 1) * HC]], 0).T)
        cv2 = np.stack([c[b], c_ctx])
        im["c2T"] = np.ascontiguousarray(cv2.T.reshape(8, 128, 2).transpose(1, 0, 2))
        m = np.zeros((128, 2), np.float32)
        m[:, r] = 1.0
        im["m01"] = m
        chsel = np.concatenate([r * 512 + np.arange(512), 1024 + r * 128 + np.arange(128), 1280 + r * 128 + np.arange(128)])
        hs = slice(r * 8, (r + 1) * 8)
        im["convw"] = np.ascontiguousarray(cw[:, chsel])
        im["convb"] = np.ascontiguousarray(cb[:, chsel].reshape(ne, 6, 128).transpose(0, 2, 1))
        im["rep8"] = np.ascontiguousarray(np.broadcast_to(
            np.stack([f32(ssd_dt_bias)[:, 0, hs], f32(ssd_dt_bias)[:, 1, hs], f32(ssd_a_log)[:, 0, hs], f32(ssd_a_log)[:, 1, hs],
                      f32(ssd_d)[:, hs]], 1)[:, None], (ne, 128, 5, 8)))
        im["cosQ"] = np.ascontiguousarray(np.concatenate([cosT[:, LC + r * HL:LC + (r + 1) * HL], cosT[:, r * HC:(r + 1) * HC]], 1))
        im["sinQ"] = np.ascontiguousarray(np.concatenate([sinT[:, LC + r * HL:LC + (r + 1) * HL], sinT[:, r * HC:(r + 1) * HC]], 1))
        ims.append(im)
    res = _run(build_fused(HLB, depth), ims)
    out = np.empty((B, L, D), np.float32)
    for ci in range(NCORES):
        out[ci // 2, (ci % 2) * HL:(ci % 2 + 1) * HL] = res[ci]["y"]
    return out
```
